# Optimizing a Trainium2 kernel written in Bass

```python
import math
import numpy as np
import jax
import jax.numpy as jnp
from jax import lax

D_MODEL = 4096
BATCH = 1
SEQ = 8192
DEPTH = 2

GRID_W = 64
CTX_LEN = 256
NORM_EPS = 1e-6
A_HEADS = 8
A_DH = 64
A_WIDTH = A_HEADS * 2 * A_DH
ROPE_PAIRS = A_DH // 4
ROPE_BASE = 10000.0
Q_BLOCK = 128
B_WIDTH = 1024
B_BLOCKS = 8
B_BW = B_WIDTH // B_BLOCKS
B_CONV = 4
CONV_LEFT = 2
LRU_C = 8.0
C_HEADS = 16
C_DH = 64
C_WIDTH = C_HEADS * C_DH
NA_ROWS = 8
NA_COLS = 16
N_BRANCH = 3
BRANCH_W = 1024
IN_SPLITS = (A_WIDTH, A_WIDTH, A_WIDTH, B_WIDTH, B_WIDTH, C_WIDTH, C_WIDTH, C_WIDTH, N_BRANCH * D_MODEL)
W_IN = 3 * A_WIDTH + 2 * B_WIDTH + 3 * C_WIDTH + N_BRANCH * D_MODEL
N_EXPERTS = 32
TOP_K = 4
F_EXPERT = 512
SWIGLU_ALPHA = 1.702
SWIGLU_LIMIT = 7.0
MOE_BLOCK = 128

kernel_name = "hybrid_gated_diffusion_block"


def rmsnorm(x, g):
    xf = x.astype(jnp.float32)
    y = xf * lax.rsqrt(jnp.mean(xf * xf, axis=-1, keepdims=True) + NORM_EPS)
    return (y * g.astype(jnp.float32)).astype(x.dtype)


def modulate(h, shift, scale):
    return h * (1.0 + scale) + shift


def split_in(z):
    return jnp.split(z, np.cumsum(IN_SPLITS)[:-1].tolist(), axis=-1)


def axial_angles(n):
    t = jnp.arange(n)
    row = (t // GRID_W).astype(jnp.float32)
    col = (t % GRID_W).astype(jnp.float32)
    inv = ROPE_BASE ** (-jnp.arange(ROPE_PAIRS, dtype=jnp.float32) / ROPE_PAIRS)
    return row[:, None] * inv, col[:, None] * inv


def rotate(x, ang):
    cos = jnp.cos(ang)[:, None, None, :].astype(x.dtype)
    sin = jnp.sin(ang)[:, None, None, :].astype(x.dtype)
    x1, x2 = x[..., :ROPE_PAIRS], x[..., ROPE_PAIRS:]
    return jnp.concatenate([x1 * cos - x2 * sin, x2 * cos + x1 * sin], axis=-1)


def axial_rope(x, ang_r, ang_c):
    half = A_DH // 2
    return jnp.concatenate([rotate(x[..., :half], ang_r), rotate(x[..., half:], ang_c)], axis=-1)


def diff_attend(q, k, v, lam):
    s = jnp.einsum('bqhcd,bkhcd->bhcqk', q, k).astype(jnp.float32) * (A_DH ** -0.5)
    p = jax.nn.softmax(s, axis=-1)
    w = (p[:, :, 0] - lam * p[:, :, 1]).astype(v.dtype)
    return jnp.einsum('bhqk,bkhe->bqhe', w, v)


def diff_attention(q, k, v, qc, kc, vc, lam_qk, subln_g, lambda_init):
    B, n = q.shape[0], q.shape[1]
    lq = lam_qk.astype(jnp.float32)
    lam = jnp.exp(jnp.sum(lq[0] * lq[1])) - jnp.exp(jnp.sum(lq[2] * lq[3])) + lambda_init
    k_all = jnp.concatenate([kc, k], axis=1)
    v_all = jnp.concatenate([vc, v], axis=1)
    qb = jnp.moveaxis(q.reshape(B, n // Q_BLOCK, Q_BLOCK, A_HEADS, 2, A_DH), 1, 0)
    o = lax.map(lambda qq: diff_attend(qq, k_all, v_all, lam), qb)
    o = jnp.moveaxis(o, 0, 1).reshape(B, n, A_HEADS, 2 * A_DH)
    oc = diff_attend(qc, kc, vc, lam)

    def post(t):
        return (rmsnorm(t, subln_g) * (1.0 - lambda_init)).reshape(t.shape[0], t.shape[1], A_WIDTH)
    return post(o), post(oc)


def short_conv(x, w, b):
    n = x.shape[1]
    xp = jnp.pad(x, ((0, 0), (CONV_LEFT, B_CONV - 1 - CONV_LEFT), (0, 0)))
    y = b + xp[:, 0:n] * w[0]
    for j in range(1, B_CONV):
        y = y + xp[:, j:j + n] * w[j]
    return y


def rglru_coeffs(u, gw, gb, lam):
    B, L, _ = u.shape
    g = jnp.einsum('blkc,gkcd->gblkd', u.reshape(B, L, B_BLOCKS, B_BW), gw.astype(jnp.float32))
    g = g + gb.astype(jnp.float32)[:, None, None]
    r = jax.nn.sigmoid(g[0]).reshape(B, L, B_WIDTH)
    i = jax.nn.sigmoid(g[1]).reshape(B, L, B_WIDTH)
    log_a = -LRU_C * r * jax.nn.softplus(-lam.astype(jnp.float32))
    a = jnp.exp(log_a)
    b = jnp.sqrt(-jnp.expm1(2.0 * log_a)) * (i * u)
    return a, b


def linear_scan(a, b, h0, reverse):
    if h0 is not None:
        edge = b.shape[1] - 1 if reverse else 0
        b = b.at[:, edge].add(a[:, edge] * h0)

    def combine(left, right):
        a_l, b_l = left
        a_r, b_r = right
        return a_l * a_r, a_r * b_l + b_r
    _, h = lax.associative_scan(combine, (a, b), axis=1, reverse=reverse)
    return h


def rglru_mixer(bx, by, bxc, byc, conv_w, conv_b, gate_w, gate_b, lru_lambda):
    u = short_conv(bx, conv_w, conv_b).astype(jnp.float32)
    uc = short_conv(bxc, conv_w, conv_b).astype(jnp.float32)
    hs, hcs = [], []
    for d, rev in enumerate((False, True)):
        a_c, b_c = rglru_coeffs(uc, gate_w[d], gate_b[d], lru_lambda[d])
        h_c = linear_scan(a_c, b_c, None, rev)
        h_end = h_c[:, 0] if rev else h_c[:, -1]
        a_l, b_l = rglru_coeffs(u, gate_w[d], gate_b[d], lru_lambda[d])
        hs.append(linear_scan(a_l, b_l, h_end, rev))
        hcs.append(h_c)
    y = (hs[0] + hs[1]).astype(bx.dtype) * jax.nn.gelu(by)
    yc = (hcs[0] + hcs[1]).astype(bxc.dtype) * jax.nn.gelu(byc)
    return y, yc


def neighborhood_attention(q, k, v, qc, kc, vc, rpb):
    B, n, H, dh = q.shape
    m = qc.shape[1]
    rows = n // GRID_W
    kr = min(NA_ROWS, rows)
    nk = kr * NA_COLS
    scale = dh ** -0.5
    qg = q.reshape(B, rows, GRID_W, H, dh)
    kg = k.reshape(B, rows, GRID_W, H, dh)
    vg = v.reshape(B, rows, GRID_W, H, dh)
    col = jnp.arange(GRID_W)
    col_idx = jnp.clip(col - NA_COLS // 2, 0, GRID_W - NA_COLS)[:, None] + jnp.arange(NA_COLS)
    col_rel = col_idx - col[:, None] + (NA_COLS - 1)
    rpb_f = rpb.astype(jnp.float32)

    def row_block(r):
        r0 = jnp.clip(r - kr // 2, 0, rows - kr)
        kb = lax.dynamic_slice_in_dim(kg, r0, kr, axis=1)[:, :, col_idx]
        vb = lax.dynamic_slice_in_dim(vg, r0, kr, axis=1)[:, :, col_idx]
        qr = lax.dynamic_index_in_dim(qg, r, axis=1, keepdims=False)
        row_rel = r0 + jnp.arange(kr) - r + (NA_ROWS - 1)
        bias = rpb_f[:, row_rel[None, :, None], col_rel[:, None, :]]
        s_nb = jnp.einsum('bqhd,brqchd->bhqrc', qr, kb).astype(jnp.float32) * scale + bias
        s_cx = jnp.einsum('bqhd,bkhd->bhqk', qr, kc).astype(jnp.float32) * scale
        s = jnp.concatenate([s_nb.reshape(B, H, GRID_W, nk), s_cx], axis=-1)
        p = jax.nn.softmax(s, axis=-1).astype(v.dtype)
        p_nb = p[..., :nk].reshape(B, H, GRID_W, kr, NA_COLS)
        return (jnp.einsum('bhqrc,brqchd->bqhd', p_nb, vb)
                + jnp.einsum('bhqk,bkhd->bqhd', p[..., nk:], vc))

    o = lax.map(row_block, jnp.arange(rows))
    o = jnp.moveaxis(o, 0, 1).reshape(B, n, H * dh)
    sc = jax.nn.softmax(jnp.einsum('bqhd,bkhd->bhqk', qc, kc).astype(jnp.float32) * scale, axis=-1)
    oc = jnp.einsum('bhqk,bkhd->bqhd', sc.astype(vc.dtype), vc).reshape(B, m, H * dh)
    return o, oc


def hybrid_mixer(h, hc, w_in, lam_qk, subln_g, conv_w, conv_b, lru_gate_w, lru_gate_b, lru_lambda,
                 na_rpb, w_branch, w_out, lambda_init):
    B, n, _ = h.shape
    m = hc.shape[1]
    aq, ak, av, bx, by, cq, ck, cv, g = split_in(h @ w_in)
    aqc, akc, avc, bxc, byc, cqc, ckc, cvc, gc = split_in(hc @ w_in)

    ang_r, ang_c = axial_angles(n)
    qa = axial_rope(aq.reshape(B, n, A_HEADS, 2, A_DH), ang_r, ang_c)
    ka = axial_rope(ak.reshape(B, n, A_HEADS, 2, A_DH), ang_r, ang_c)
    o_a, o_ac = diff_attention(qa, ka, av.reshape(B, n, A_HEADS, 2 * A_DH),
                               aqc.reshape(B, m, A_HEADS, 2, A_DH), akc.reshape(B, m, A_HEADS, 2, A_DH),
                               avc.reshape(B, m, A_HEADS, 2 * A_DH), lam_qk, subln_g, lambda_init)

    o_b, o_bc = rglru_mixer(bx, by, bxc, byc, conv_w, conv_b, lru_gate_w, lru_gate_b, lru_lambda)

    o_c, o_cc = neighborhood_attention(
        cq.reshape(B, n, C_HEADS, C_DH), ck.reshape(B, n, C_HEADS, C_DH), cv.reshape(B, n, C_HEADS, C_DH),
        cqc.reshape(B, m, C_HEADS, C_DH), ckc.reshape(B, m, C_HEADS, C_DH), cvc.reshape(B, m, C_HEADS, C_DH),
        na_rpb)

    def merge(outs, gates):
        y = None
        for i, o in enumerate(outs):
            term = jax.nn.sigmoid(gates[..., i * D_MODEL:(i + 1) * D_MODEL]) * (o @ w_branch[i])
            y = term if y is None else y + term
        return y @ w_out
    return merge((o_a, o_b, o_c), g), merge((o_ac, o_bc, o_cc), gc)


def clamped_swiglu(gu):
    glu = jnp.minimum(gu[..., ::2], SWIGLU_LIMIT)
    lin = jnp.clip(gu[..., 1::2], -SWIGLU_LIMIT, SWIGLU_LIMIT)
    return glu * jax.nn.sigmoid(SWIGLU_ALPHA * glu) * (lin + 1.0)


def moe_ffn(h, w_router, b_router, w_gu, b_gu, w_down, b_down):
    T = h.shape[0]
    tk = T * TOP_K
    logits = (h @ w_router).astype(jnp.float32) + b_router.astype(jnp.float32)
    top_val, top_idx = lax.top_k(logits, TOP_K)
    gate = jax.nn.softmax(top_val, axis=-1)
    flat_e = top_idx.reshape(tk)
    flat_tok = jnp.arange(tk, dtype=jnp.int32) // TOP_K
    flat_w = gate.reshape(tk)
    order = jnp.argsort(flat_e)
    sorted_e = flat_e[order]
    counts = jnp.bincount(flat_e, length=N_EXPERTS)
    padded = (counts + MOE_BLOCK - 1) // MOE_BLOCK * MOE_BLOCK
    start = jnp.cumsum(counts) - counts
    pend = jnp.cumsum(padded)
    pstart = pend - padded
    dest = pstart[sorted_e] + jnp.arange(tk) - start[sorted_e]
    n_blocks = -(-(tk + N_EXPERTS * (MOE_BLOCK - 1)) // MOE_BLOCK)
    n_rows = n_blocks * MOE_BLOCK
    row_tok = jnp.zeros((n_rows,), jnp.int32).at[dest].set(flat_tok[order])
    row_w = jnp.zeros((n_rows,), jnp.float32).at[dest].set(flat_w[order])
    block_e = jnp.minimum(jnp.searchsorted(pend, jnp.arange(n_blocks) * MOE_BLOCK, side='right'),
                          N_EXPERTS - 1)

    def body(i, acc):
        e = block_e[i]
        toks = lax.dynamic_slice_in_dim(row_tok, i * MOE_BLOCK, MOE_BLOCK)
        wts = lax.dynamic_slice_in_dim(row_w, i * MOE_BLOCK, MOE_BLOCK)
        gu = h[toks] @ w_gu[e] + b_gu[e]
        y = clamped_swiglu(gu) @ w_down[e] + b_down[e]
        return acc.at[toks].add(y * wts[:, None].astype(y.dtype))
    return lax.fori_loop(0, n_blocks, body, jnp.zeros_like(h))


def trunk_layer(x, xc, c, c_ctx, w_ada, b_ada, norm1_g, norm2_g, w_in, lam_qk, subln_g, conv_w, conv_b,
                lru_gate_w, lru_gate_b, lru_lambda, na_rpb, w_branch, w_out, w_router, b_router,
                w_gu, b_gu, w_down, b_down, lambda_init):
    B, n, D = x.shape
    m = xc.shape[1]
    mod = jax.nn.silu(c) @ w_ada + b_ada
    mod_c = jax.nn.silu(c_ctx) @ w_ada + b_ada
    sh1, sc1, g1, sh2, sc2, g2 = jnp.split(mod[:, None, :], 6, axis=-1)
    sh1c, sc1c, g1c, sh2c, sc2c, g2c = jnp.split(mod_c, 6)
    mix, mix_c = hybrid_mixer(modulate(rmsnorm(x, norm1_g), sh1, sc1),
                              modulate(rmsnorm(xc, norm1_g), sh1c, sc1c),
                              w_in, lam_qk, subln_g, conv_w, conv_b, lru_gate_w, lru_gate_b, lru_lambda,
                              na_rpb, w_branch, w_out, lambda_init)
    x = x + g1 * mix
    xc = xc + g1c * mix_c
    tokens = jnp.concatenate([modulate(rmsnorm(xc, norm2_g), sh2c, sc2c),
                              modulate(rmsnorm(x, norm2_g), sh2, sc2)], axis=1)
    f = moe_ffn(tokens.reshape(B * (m + n), D), w_router, b_router, w_gu, b_gu, w_down, b_down)
    f = f.reshape(B, m + n, D)
    return x + g2 * f[:, m:], xc + g2c * f[:, :m]


def setup_inputs(seed: int = 0) -> dict:
    key = jax.random.key(seed)
    ks = jax.random.split(key, 28)
    f32 = jnp.float32
    D, L, E, F = D_MODEL, DEPTH, N_EXPERTS, F_EXPERT

    def nrm(k, shape, scale):
        return jax.random.normal(k, shape, f32) * scale

    a0 = jax.random.uniform(ks[16], (L, 2, B_WIDTH), f32, minval=0.9, maxval=0.999)
    s0 = a0 ** (1.0 / LRU_C)
    return {
        "x": nrm(ks[0], (BATCH, SEQ, D), 1.0),
        "c": nrm(ks[1], (BATCH, D), 1.0),
        "ctx": nrm(ks[2], (BATCH, CTX_LEN, D), 1.0),
        "c_ctx": nrm(ks[3], (D,), 1.0),
        "w_ada": nrm(ks[4], (L, D, 6 * D), 0.5 * D ** -0.5),
        "b_ada": nrm(ks[5], (L, 6 * D), 0.02),
        "norm1_g": 1.0 + nrm(ks[6], (L, D), 0.05),
        "norm2_g": 1.0 + nrm(ks[7], (L, D), 0.05),
        "w_in": nrm(ks[8], (L, D, W_IN), D ** -0.5),
        "lam_qk": nrm(ks[9], (L, 4, A_DH), 0.1),
        "subln_g": 1.0 + nrm(ks[10], (L, 2 * A_DH), 0.05),
        "conv_w": nrm(ks[11], (L, B_CONV, B_WIDTH), B_CONV ** -0.5),
        "conv_b": nrm(ks[12], (L, B_WIDTH), 0.02),
        "lru_gate_w": nrm(ks[13], (L, 2, 2, B_BLOCKS, B_BW, B_BW), B_BW ** -0.5),
        "lru_gate_b": nrm(ks[14], (L, 2, 2, B_BLOCKS, B_BW), 0.02),
        "lru_lambda": jnp.log(s0) - jnp.log1p(-s0),
        "na_rpb": nrm(ks[15], (L, C_HEADS, 2 * NA_ROWS - 1, 2 * NA_COLS - 1), 0.1),
        "w_branch": nrm(ks[17], (L, N_BRANCH, BRANCH_W, D), BRANCH_W ** -0.5),
        "w_out": nrm(ks[18], (L, D, D), D ** -0.5),
        "w_router": nrm(ks[19], (L, D, E), D ** -0.5),
        "b_router": nrm(ks[20], (L, E), 0.01),
        "w_gu": nrm(ks[21], (L, E, D, 2 * F), D ** -0.5),
        "b_gu": nrm(ks[22], (L, E, 2 * F), 0.02),
        "w_down": nrm(ks[23], (L, E, F, D), F ** -0.5),
        "b_down": nrm(ks[24], (L, E, D), 0.02),
        "final_g": 1.0 + nrm(ks[25], (D,), 0.05),
    }


def reference(x, c, ctx, c_ctx, w_ada, b_ada, norm1_g, norm2_g, w_in, lam_qk, subln_g, conv_w, conv_b,
              lru_gate_w, lru_gate_b, lru_lambda, na_rpb, w_branch, w_out, w_router, b_router,
              w_gu, b_gu, w_down, b_down, final_g):
    x_lat, x_ctx = x, ctx
    for l in range(DEPTH):
        lambda_init = 0.8 - 0.6 * math.exp(-0.3 * l)
        x_lat, x_ctx = trunk_layer(x_lat, x_ctx, c, c_ctx, w_ada[l], b_ada[l], norm1_g[l], norm2_g[l],
                                   w_in[l], lam_qk[l], subln_g[l], conv_w[l], conv_b[l], lru_gate_w[l],
                                   lru_gate_b[l], lru_lambda[l], na_rpb[l], w_branch[l], w_out[l],
                                   w_router[l], b_router[l], w_gu[l], b_gu[l], w_down[l], b_down[l],
                                   lambda_init)
    return rmsnorm(x_lat, final_g)
```

```python
import contextlib
import numpy as np
import concourse.bass as bass
import concourse.mybir as mybir

F32 = mybir.dt.float32
BF16 = mybir.dt.bfloat16
I32 = mybir.dt.int32
U32 = mybir.dt.uint32
AF = mybir.ActivationFunctionType
ALU = mybir.AluOpType
AX = mybir.AxisListType

ENGS = ("pe", "act", "dve", "pool", "sp")


class Buf:
    __slots__ = ("name", "w", "r", "sem", "semval")

    def __init__(self, name):
        self.name = name
        self.w = None
        self.r = []
        self.sem = None
        self.semval = 0


class Sched:
    def __init__(self, nc):
        self.nc = nc
        self.stream = {e: [] for e in ENGS}
        self.seen = {e: {} for e in ENGS}
        self.ndma = 0
        self._allbufs = []

    def buf(self, name="b"):
        b = Buf(name)
        self._allbufs.append(b)
        return b

    def bufs(self, n, name="b"):
        return [self.buf(f"{name}{i}") for i in range(n)]

    def _need(self, eng, ev, waits, same_engine_ok):
        if ev is None:
            return
        kind = ev[0]
        if kind == "E":
            _, src, idx = ev
            if src == eng and same_engine_ok:
                return
            key = ("E", src)
            if self.seen[eng].get(key, -1) >= idx:
                return
            waits[key] = max(waits.get(key, -1), idx)
        else:
            _, semid, val = ev
            key = ("D", semid)
            if self.seen[eng].get(key, -1) >= val:
                return
            waits[key] = max(waits.get(key, -1), val)

    def _collect(self, eng, reads, writes):
        waits = {}
        for b in reads:
            self._need(eng, b.w, waits, False)
        for b in writes:
            self._need(eng, b.w, waits, True)
            for r in b.r:
                self._need(eng, r, waits, True)
        wl = []
        for key, v in waits.items():
            self.seen[eng][key] = v
            if key[0] == "E":
                self.stream[key[1]][v]["sig"] = True
            wl.append((key, v))
        return wl

    def op(self, eng, fn, reads=(), writes=()):
        wl = self._collect(eng, reads, writes)
        idx = len(self.stream[eng])
        self.stream[eng].append(dict(fn=fn, waits=wl, sig=False, dma=None))
        ev = ("E", eng, idx)
        for b in reads:
            b.r.append(ev)
        for b in writes:
            b.w = ev
            b.r = []
        return ev

    def dma(self, q, fn, reads=(), writes=(), sembuf=None):
        wl = self._collect(q, reads, writes)
        if sembuf is None:
            sembuf = writes[0] if writes else reads[0]
        if sembuf.sem is None:
            sembuf.sem = self.ndma
            self.ndma += 1
        sembuf.semval += 16
        ev = ("D", sembuf.sem, sembuf.semval)
        self.stream[q].append(dict(fn=fn, waits=wl, sig=False, dma=sembuf.sem))
        for b in reads:
            b.r.append(ev)
        for b in writes:
            b.w = ev
            b.r = []
        return ev

    def wait_all(self, eng, bufs):
        wl = self._collect(eng, [], bufs)
        self.stream[eng].append(dict(fn=None, waits=wl, sig=False, dma=None))

    def emit(self, stack):
        nc = self.nc
        esem = {e: stack.enter_context(nc.semaphore(f"es_{e}")) for e in ENGS}
        dsem = [stack.enter_context(nc.semaphore(f"ds_{i}")) for i in range(self.ndma)]
        vals = {}
        for e in ENGS:
            c = 0
            v = []
            for ent in self.stream[e]:
                if ent["sig"]:
                    c += 1
                v.append(c)
            vals[e] = v
        self.maxval = {e: (vals[e][-1] if vals[e] else 0) for e in ENGS}
        engobj = {"pe": "tensor", "act": "scalar", "dve": "vector", "pool": "gpsimd", "sp": "sync"}

        def replay(ename, eng):
            for ent in self.stream[ename]:
                for key, v in ent["waits"]:
                    if key[0] == "E":
                        eng.wait_ge(esem[key[1]], vals[key[1]][v])
                    else:
                        eng.wait_ge(dsem[key[1]], v)
                if ent["fn"] is None:
                    continue
                ins = ent["fn"](eng)
                if ent["dma"] is not None:
                    ins.then_inc(dsem[ent["dma"]], 16)
                elif ent["sig"]:
                    ins.then_inc(esem[ename], 1)

        import os
        if os.environ.get("MK_CLEAR", "0") == "1":
          with nc.Block() as blk0:
            @blk0.vector
            def _(v):
                for e in ENGS:
                    v.sem_clear(esem[e])
                for s in dsem:
                    v.sem_clear(s)
            del _
        with nc.Block() as blk:
            @blk.tensor
            def _(e):
                replay("pe", e)

            @blk.scalar
            def _(e):
                replay("act", e)

            @blk.vector
            def _(e):
                replay("dve", e)

            @blk.gpsimd
            def _(e):
                replay("pool", e)

            @blk.sync
            def _(e):
                replay("sp", e)


def sched_fence(S, eng, bufs, out, scratch_ap):
    S.wait_all(eng, bufs)
    S.op(eng, lambda e: e.memset(scratch_ap, 0.0), writes=[out])


def sched_barrier(S, scratch_ap):
    allb = list(S._allbufs)
    S.wait_all("dve", allb)
    bar = Buf("bar")
    S.op("dve", lambda e: e.memset(scratch_ap, 0.0), writes=[bar])
    for e in ENGS:
        if e != "dve":
            S.wait_all(e, [bar])
    S._allbufs.append(bar)


NT = 1056
NC_CTX = 32
D = 4096
KC = 32
TT = [(0, 352), (352, 352), (704, 352)]
EPS = 1e-6


class Ring:
    def __init__(self, S, nc, st, name, shape, dtype, n):
        self.t = [st.enter_context(nc.sbuf_tensor(f"{name}{i}", shape, dtype)) for i in range(n)]
        self.b = S.bufs(n, name)
        self.i = 0
        self.n = n

    def next(self):
        k = self.i % self.n
        self.i += 1
        return self.t[k], self.b[k]


class PsRing:
    def __init__(self, S, ps, idxs):
        self.t = [ps[i] for i in idxs]
        self.b = S.bufs(len(idxs), "psr")
        self.i = 0

    def next(self):
        k = self.i % len(self.t)
        self.i += 1
        return self.t[k], self.b[k]


def alloc_psum(nc, st):
    return [st.enter_context(nc.psum_tensor(f"psb{i}", [128, 512], F32)) for i in range(8)]


def emit_p0(S, nc, st, ps, cT_d, wada_d, bada_d, modv_d):
    cT = st.enter_context(nc.sbuf_tensor("p0_cT", [128, 32, 2], F32))
    sT = st.enter_context(nc.sbuf_tensor("p0_sT", [128, 32, 2], F32))
    bada = st.enter_context(nc.sbuf_tensor("p0_bada", [128, 48], F32))
    modv = st.enter_context(nc.sbuf_tensor("p0_modv", [128, 48, 2], F32))
    b_c, b_s, b_b, b_m = S.bufs(4, "p0")
    wr = Ring(S, nc, st, "p0_w", [128, 32, 512], F32, 2)
    pr = PsRing(S, ps, [0, 1, 2, 3])
    S.dma("sp", lambda e: e.dma_start(out=cT[:], in_=cT_d), writes=[b_c])
    S.dma("sp", lambda e: e.dma_start(out=bada[:], in_=bada_d), writes=[b_b])
    S.op("act", lambda e: e.activation(out=sT[:], in_=cT[:], func=AF.Silu), reads=[b_c], writes=[b_s])
    wv = wada_d.rearrange("(k p) f -> p k f", p=128)
    for g in range(12):
        wt, wb = wr.next()
        for h in range(2):
            S.dma("sp" if h == 0 else "act",
                  lambda e, wt=wt, g=g, h=h: e.dma_start(out=wt[:, h * 16:(h + 1) * 16, :],
                                                         in_=wv[:, h * 16:(h + 1) * 16, g * 512:(g + 1) * 512]),
                  writes=[wb])
        for c in range(4):
            pt, pb = pr.next()
            for k in range(32):
                S.op("pe", lambda e, pt=pt, wt=wt, c=c, k=k: e.matmul(
                    pt[:, 0:2], lhsT=wt[:, k, c * 128:(c + 1) * 128], rhs=sT[:, k, :],
                    start=(k == 0), stop=(k == 31)), reads=[wb, b_s], writes=[pb])
            ci = g * 4 + c
            S.op("dve", lambda e, pt=pt, ci=ci: e.tensor_scalar(
                out=modv[:, ci, :], in0=pt[:, 0:2], scalar1=bada[:, ci:ci + 1], scalar2=None, op0=ALU.add),
                reads=[pb, b_b], writes=[b_m])
    S.dma("sp", lambda e: e.dma_start(out=modv_d, in_=modv[:]), reads=[b_m], sembuf=b_m)
    return [b_m]


def emit_norm_mod(S, nc, st, ps, xT_d, b_xd, modv, b_modv, gvec, b_gvec, mod_base, hT, b_h, tag,
                  h32_cb=None):
    xr = Ring(S, nc, st, f"{tag}_x", [128, NT], F32, 3)
    sq = Ring(S, nc, st, f"{tag}_sq", [128, NT], F32, 2)
    tm = Ring(S, nc, st, f"{tag}_tm", [128, NT], F32, 2)
    ones = st.enter_context(nc.sbuf_tensor(f"{tag}_ones", [128, 128], F32))
    rstd = st.enter_context(nc.sbuf_tensor(f"{tag}_rstd", [128, NT], F32))
    Ab = st.enter_context(nc.sbuf_tensor(f"{tag}_A", [128, 32, 2], F32))
    b_ones, b_rstd, b_A = S.bufs(3, tag)
    S.op("pool", lambda e: e.memset(ones[:], 1.0), writes=[b_ones])
    S.op("dve", lambda e: e.tensor_scalar(out=Ab[:], in0=modv[:, mod_base + 32:mod_base + 64, :], scalar1=1.0,
                                          scalar2=None, op0=ALU.add), reads=[b_modv], writes=[b_A])
    for t in range(2):
        S.op("dve", lambda e, t=t: e.tensor_tensor(out=Ab[:, :, t], in0=Ab[:, :, t], in1=gvec[:], op=ALU.mult),
             reads=[b_A, b_gvec], writes=[b_A])
    xv = xT_d.rearrange("(k p) t -> k p t", p=128)
    pss = [(ps[i], S.buf(f"{tag}_pss{i}")) for i in range(3)]
    for k in range(32):
        xt, xb = xr.next()
        S.dma("sp", lambda e, xt=xt, k=k: e.dma_start(out=xt[:], in_=xv[k]), reads=[b_xd], writes=[xb])
        qt, qb = sq.next()
        S.op("act", lambda e, xt=xt, qt=qt: e.activation(out=qt[:], in_=xt[:], func=AF.Square),
             reads=[xb], writes=[qb])
        for i, (o, n) in enumerate(TT):
            S.op("pe", lambda e, i=i, o=o, n=n, qt=qt, k=k: e.matmul(
                pss[i][0][:, 0:n], lhsT=ones[:], rhs=qt[:, o:o + n], start=(k == 0), stop=(k == 31)),
                reads=[qb, b_ones], writes=[pss[i][1]])
    for i, (o, n) in enumerate(TT):
        S.op("dve", lambda e, i=i, o=o, n=n: e.tensor_scalar(
            out=rstd[:, o:o + n], in0=pss[i][0][:, 0:n], scalar1=1.0 / D, scalar2=EPS, op0=ALU.mult, op1=ALU.add),
            reads=[pss[i][1]], writes=[b_rstd])
    S.op("act", lambda e: e.activation(out=rstd[:], in_=rstd[:], func=AF.Sqrt), reads=[b_rstd], writes=[b_rstd])
    S.op("dve", lambda e: e.reciprocal(out=rstd[:], in_=rstd[:]), reads=[b_rstd], writes=[b_rstd])
    for k in range(32):
        xt, xb = xr.next()
        S.dma("sp", lambda e, xt=xt, k=k: e.dma_start(out=xt[:], in_=xv[k]), reads=[b_xd], writes=[xb])
        tt, tb = tm.next()
        S.op("dve", lambda e, xt=xt, tt=tt: e.tensor_tensor(out=tt[:], in0=xt[:], in1=rstd[:], op=ALU.mult),
             reads=[xb, b_rstd], writes=[tb])
        if h32_cb is None:
            S.op("act", lambda e, tt=tt, k=k: e.activation(
                out=hT[:, k, 0:NC_CTX], in_=tt[:, 0:NC_CTX], func=AF.Identity,
                scale=Ab[:, k, 1:2], bias=modv[:, mod_base + k, 1:2]), reads=[tb, b_A, b_modv], writes=[b_h])
            S.op("act", lambda e, tt=tt, k=k: e.activation(
                out=hT[:, k, NC_CTX:NT], in_=tt[:, NC_CTX:NT], func=AF.Identity,
                scale=Ab[:, k, 0:1], bias=modv[:, mod_base + k, 0:1]), reads=[tb, b_A, b_modv], writes=[b_h])
        else:
            S.op("act", lambda e, tt=tt, k=k: e.activation(
                out=tt[:, 0:NC_CTX], in_=tt[:, 0:NC_CTX], func=AF.Identity,
                scale=Ab[:, k, 1:2], bias=modv[:, mod_base + k, 1:2]), reads=[tb, b_A, b_modv], writes=[tb])
            S.op("act", lambda e, tt=tt, k=k: e.activation(
                out=tt[:, NC_CTX:NT], in_=tt[:, NC_CTX:NT], func=AF.Identity,
                scale=Ab[:, k, 0:1], bias=modv[:, mod_base + k, 0:1]), reads=[tb, b_A, b_modv], writes=[tb])
            S.op("dve", lambda e, tt=tt, k=k: e.tensor_copy(out=hT[:, k, :], in_=tt[:]), reads=[tb], writes=[b_h])
            h32_cb(k, tt, tb)


def emit_p1(S, nc, st, ps, layer, xT_d, b_xd, modv_d, n1g_d, win_d, zsend_d, b_zd, gsig_d, b_gd):
    modv = st.enter_context(nc.sbuf_tensor("p1_modv", [128, 384, 2], F32))
    gvec = st.enter_context(nc.sbuf_tensor("p1_g", [128, 32], F32))
    hT = st.enter_context(nc.sbuf_tensor("p1_hT", [128, 32, NT], BF16))
    b_modv, b_gvec, b_h = S.bufs(3, "p1")
    S.dma("sp", lambda e: e.dma_start(out=modv[:], in_=modv_d), writes=[b_modv])
    S.dma("sp", lambda e: e.dma_start(out=gvec[:], in_=n1g_d), writes=[b_gvec])
    emit_norm_mod(S, nc, st, ps, xT_d, b_xd, modv, b_modv, gvec, b_gvec, layer * 192 + 0, hT, b_h, "p1n")
    WCOL = 256
    wr = Ring(S, nc, st, "p1_w", [128, 32, WCOL], BF16, 3)
    osr = Ring(S, nc, st, "p1_os", [128, NT], F32, 3)
    pr = PsRing(S, ps, [0, 1, 2, 3, 4, 5])
    wv = win_d.rearrange("(k p) f -> p k f", p=128)
    nfc = 160
    for wi in range(nfc * 128 // WCOL):
        wt, wb = wr.next()
        S.dma("pool", lambda e, wt=wt, wi=wi: e.dma_start(out=wt[:], in_=wv[:, :, wi * WCOL:(wi + 1) * WCOL]),
              writes=[wb])
        for c in range(WCOL // 128):
            fc = wi * (WCOL // 128) + c
            ot, ob = osr.next()
            for i, (o, n) in enumerate(TT):
                pt, pb = pr.next()
                for k in range(32):
                    S.op("pe", lambda e, pt=pt, wt=wt, c=c, k=k, o=o, n=n: e.matmul(
                        pt[:, 0:n], lhsT=wt[:, k, c * 128:(c + 1) * 128], rhs=hT[:, k, o:o + n],
                        start=(k == 0), stop=(k == 31)), reads=[wb, b_h], writes=[pb])
                if fc < 64:
                    S.op("dve", lambda e, pt=pt, ot=ot, o=o, n=n: e.tensor_copy(out=ot[:, o:o + n], in_=pt[:, 0:n]),
                         reads=[pb], writes=[ob])
                else:
                    S.op("act", lambda e, pt=pt, ot=ot, o=o, n=n: e.activation(
                        out=ot[:, o:o + n], in_=pt[:, 0:n], func=AF.Sigmoid), reads=[pb], writes=[ob])
            if fc < 64:
                S.dma("sp", lambda e, ot=ot, fc=fc: e.dma_start(out=zsend_d[fc % 8, fc // 8], in_=ot[:]),
                      reads=[ob], sembuf=ob)
            else:
                S.dma("sp", lambda e, ot=ot, fc=fc: e.dma_start(out=gsig_d[fc - 64], in_=ot[:]),
                      reads=[ob], sembuf=ob)
    return osr.b


NG = 8448
import os as _os
_QW = int(_os.environ.get("MK_QW", "512"))
QT = [(0, 256)] + [(256 + _QW * m, _QW) for m in range(8192 // _QW)]


def g_load_pieces(zr_d, grp):
    out = []
    for s in range(8):
        out.append((32 * s, 32, zr_d[s, grp, :, 0:32]))
        out.append((256 + 1024 * s, 1024, zr_d[s, grp, :, 32:1056]))
    return out


def g_store_pieces(os_d, br, g0, n):
    out = []
    g = g0
    while g < g0 + n:
        if g < 256:
            dest, col, m = g // 32, g % 32, min(32 - g % 32, g0 + n - g)
        else:
            l = g - 256
            dest, col = l // 1024, 32 + l % 1024
            m = min(1024 - l % 1024, g0 + n - g)
        out.append((g - g0, m, os_d[dest, br, :, col:col + m]))
        g += m
    return out


def load_group(S, nc, q, zr_d, b_zr, grp, tile, buf):
    for (off, n, src) in g_load_pieces(zr_d, grp):
        S.dma(q, lambda e, off=off, n=n, src=src: e.dma_start(out=tile[:, off:off + n], in_=src),
              reads=[b_zr], writes=[buf])


def store_group(S, q, os_d, br, tile, buf, g0, n, t0=0):
    for (off, m, dst) in g_store_pieces(os_d, br, g0, n):
        S.dma(q, lambda e, off=off, m=m, dst=dst: e.dma_start(out=dst, in_=tile[:, t0 + off:t0 + off + m]),
              reads=[buf], sembuf=buf)


def build_vtok(S, nc, ps_ring, vs, b_vs, ident, b_id, vtok, b_vtok, shift, nchunk):
    c = 0
    while c < nchunk:
        nb = min(4, nchunk - c)
        pt, pb = ps_ring.next()
        for j in range(nb):
            S.op("pe", lambda e, pt=pt, j=j, c=c: e.transpose(
                out=pt[:, j * 128:(j + 1) * 128], in_=vs[:, shift + (c + j) * 128: shift + (c + j + 1) * 128],
                identity=ident[:]), reads=[b_vs, b_id], writes=[pb])
        eng = "dve" if (c // 4) % 2 == 0 else "act"
        if eng == "dve":
            S.op("dve", lambda e, pt=pt, c=c, nb=nb: e.tensor_copy(
                out=vtok[:, c:c + nb, :], in_=pt[:, 0:nb * 128].rearrange("p (c e) -> p c e", e=128)),
                reads=[pb], writes=[b_vtok])
        else:
            S.op("act", lambda e, pt=pt, c=c, nb=nb: e.activation(
                out=vtok[:, c:c + nb, :], in_=pt[:, 0:nb * 128].rearrange("p (c e) -> p c e", e=128),
                func=AF.Copy), reads=[pb], writes=[b_vtok])
        c += nb


def emit_p2a(S, nc, st, ps, zr_d, b_zr, os_d, consts, lambda_init):
    stg = Ring(S, nc, st, "a_stg", [128, NG], F32, 2)
    qTb = st.enter_context(nc.sbuf_tensor("a_q", [128, 2, NG], BF16))
    kTb = st.enter_context(nc.sbuf_tensor("a_k", [128, NG], BF16))
    vtok = st.enter_context(nc.sbuf_tensor("a_v", [128, 66, 128], BF16))
    rmat = st.enter_context(nc.sbuf_tensor("a_rmat", [128, 128], F32))
    ident = st.enter_context(nc.sbuf_tensor("a_ident", [128, 128], F32))
    ones_b = st.enter_context(nc.sbuf_tensor("a_onesb", [128, 128], BF16))
    ones_f = st.enter_context(nc.sbuf_tensor("a_onesf", [128, 128], F32))
    lamqk = st.enter_context(nc.sbuf_tensor("a_lamqk", [128, 256], F32))
    lamt = st.enter_context(nc.sbuf_tensor("a_lamt", [128, 128], F32))
    lamv = st.enter_context(nc.sbuf_tensor("a_lamv", [128, 4], F32))
    subg = st.enter_context(nc.sbuf_tensor("a_subg", [128, 1], F32))
    b_q, b_k, b_v, b_rm, b_id, b_ob, b_of, b_lq, b_lt, b_lv, b_sg = S.bufs(11, "a")
    S.dma("sp", lambda e: e.dma_start(out=rmat[:], in_=consts["rmat"]), writes=[b_rm])
    S.dma("sp", lambda e: e.dma_start(out=ident[:], in_=consts["ident"]), writes=[b_id])
    S.dma("sp", lambda e: e.dma_start(out=lamqk[:], in_=consts["lamqk"]), writes=[b_lq])
    S.dma("sp", lambda e: e.dma_start(out=subg[:], in_=consts["subg"]), writes=[b_sg])
    S.op("pool", lambda e: e.memset(ones_b[:], 1.0), writes=[b_ob])
    S.op("pool", lambda e: e.memset(ones_f[:], 1.0), writes=[b_of])
    S.op("pool", lambda e: e.memset(qTb[:, 0, :], 0.0), writes=[b_q])
    S.op("pool", lambda e: e.memset(qTb[:, 1, :], 0.0), writes=[b_q])
    S.op("dve", lambda e: e.tensor_tensor(out=lamt[:, 0:64], in0=lamqk[:, 0:64], in1=lamqk[:, 64:128], op=ALU.mult),
         reads=[b_lq], writes=[b_lt])
    S.op("dve", lambda e: e.tensor_tensor(out=lamt[:, 64:128], in0=lamqk[:, 128:192], in1=lamqk[:, 192:256],
                                          op=ALU.mult), reads=[b_lq], writes=[b_lt])
    S.op("dve", lambda e: e.tensor_reduce(out=lamv[:, 0:2], in_=lamt[:].rearrange("p (a b) -> p a b", b=64),
                                          axis=AX.X, op=ALU.add), reads=[b_lt], writes=[b_lv])
    S.op("act", lambda e: e.activation(out=lamv[:, 0:2], in_=lamv[:, 0:2], func=AF.Exp), reads=[b_lv], writes=[b_lv])
    S.op("dve", lambda e: e.tensor_tensor(out=lamv[:, 2:3], in0=lamv[:, 1:2], in1=lamv[:, 0:1], op=ALU.subtract),
         reads=[b_lv], writes=[b_lv])
    S.op("dve", lambda e: e.tensor_scalar(out=lamv[:, 3:4], in0=lamv[:, 2:3], scalar1=-float(lambda_init),
                                          scalar2=None, op0=ALU.add), reads=[b_lv], writes=[b_lv])
    S.op("dve", lambda e: e.tensor_scalar(out=subg[:], in0=subg[:], scalar1=float(1.0 - lambda_init), scalar2=None,
                                          op0=ALU.mult), reads=[b_sg], writes=[b_sg])
    pr = PsRing(S, ps, [0, 1, 2, 3, 4, 5, 6, 7])
    cs = Ring(S, nc, st, "a_cs", [128, 2, 352], F32, 3)
    t1r = Ring(S, nc, st, "a_t1", [128, 352], F32, 2)
    t2r = Ring(S, nc, st, "a_t2", [128, 352], F32, 2)
    for grp, dstT, b_dst in ((0, qTb, b_q), (1, kTb, b_k)):
        xs, xb = stg.next()
        load_group(S, nc, "sp", zr_d, b_zr, grp, xs, xb)
        for i in range(NG // 352):
            o = i * 352
            ct, cb = cs.next()
            S.dma("act", lambda e, ct=ct, o=o: e.dma_start(out=ct[:, 0, :], in_=consts["ropeC"][:, o:o + 352]),
                  writes=[cb])
            S.dma("act", lambda e, ct=ct, o=o: e.dma_start(out=ct[:, 1, :], in_=consts["ropeS"][:, o:o + 352]),
                  writes=[cb])
            pt, pb = pr.next()
            S.op("pe", lambda e, pt=pt, xs=xs, o=o: e.matmul(pt[:, 0:352], lhsT=rmat[:], rhs=xs[:, o:o + 352],
                                                            start=True, stop=True), reads=[xb, b_rm], writes=[pb])
            t1, t1b = t1r.next()
            t2, t2b = t2r.next()
            S.op("pool", lambda e, t1=t1, xs=xs, ct=ct, o=o: e.tensor_tensor(
                out=t1[:], in0=xs[:, o:o + 352], in1=ct[:, 0, :], op=ALU.mult), reads=[xb, cb], writes=[t1b])
            S.op("dve", lambda e, t2=t2, pt=pt, ct=ct: e.tensor_tensor(
                out=t2[:], in0=pt[:, 0:352], in1=ct[:, 1, :], op=ALU.mult), reads=[pb, cb], writes=[t2b])
            if grp == 0:
                for c in range(2):
                    S.op("dve", lambda e, t1=t1, t2=t2, o=o, c=c: e.tensor_tensor(
                        out=qTb[64 * c:64 * c + 64, c, o:o + 352], in0=t1[64 * c:64 * c + 64, :],
                        in1=t2[64 * c:64 * c + 64, :], op=ALU.add), reads=[t1b, t2b], writes=[b_dst])
            else:
                S.op("dve", lambda e, t1=t1, t2=t2, dstT=dstT, o=o: e.tensor_tensor(
                    out=dstT[:, o:o + 352], in0=t1[:], in1=t2[:], op=ALU.add), reads=[t1b, t2b], writes=[b_dst])
    import os as _os
    if _os.environ.get("MK_DEBUG") == "rope":
        outs = []
        for br, (srcT, b_src) in enumerate(((qTb, b_q), (kTb, b_k))):
            xs, xb = stg.next()
            S.op("dve", lambda e, xs=xs, srcT=srcT: e.tensor_copy(out=xs[:], in_=srcT[:]), reads=[b_src], writes=[xb])
            for i in range(8):
                S.dma("sp", lambda e, xs=xs, i=i, br=br: e.dma_start(out=os_d[i, br], in_=xs[:, 1056 * i:1056 * (i + 1)]),
                      reads=[xb], sembuf=xb)
            outs.append(xb)
        return outs
    xs, xb = stg.next()
    load_group(S, nc, "sp", zr_d, b_zr, 2, xs, xb)
    build_vtok(S, nc, pr, xs, xb, ident, b_id, vtok, b_v, 0, 66)
    scr = st.enter_context(nc.sbuf_tensor("a_scr", [128, 1], F32))
    sched_barrier(S, scr[:])
    pT = Ring(S, nc, st, "a_pT", [128, 512], BF16, 8)
    epi = Ring(S, nc, st, "a_epi", [128, 4, 512], F32, 2)
    ost = Ring(S, nc, st, "a_ost", [128, 512], F32, 2)
    ps_s = PsRing(S, ps, [0, 1, 4, 5, 7])
    ps_nd = [(ps[2], ps[3]), (ps[2], ps[3])]
    _bn, _bd = S.buf("n0"), S.buf("d0")
    b_nd = [(_bn, _bd), (_bn, _bd)]
    ps_ss, b_ss = ps[6], S.buf("ss")
    if _os.environ.get("MK_DEBUG") == "att64":
        dbg = st.enter_context(nc.sbuf_tensor("a_dbg", [128, 3, 512], F32))
        b_dbg = S.buf("dbg")
        sp_, sb_ = ps_s.next()
        S.op("pe", lambda e: e.matmul(sp_[:, 0:512], lhsT=kTb[0:64, 8192:8320], rhs=qTb[0:64, 256:768],
                                      start=True, stop=True), reads=[b_k, b_q], writes=[sb_])
        pt_, ptb = pT.next()
        S.op("act", lambda e: e.activation(out=pt_[:, 0:512], in_=sp_[:, 0:512], func=AF.Exp, scale=0.125),
             reads=[sb_], writes=[ptb])
        S.op("dve", lambda e: e.tensor_copy(out=dbg[:, 0, :], in_=pt_[:, 0:512]), reads=[ptb], writes=[b_dbg])
        S.op("dve", lambda e: e.tensor_copy(out=dbg[:, 1, 0:128], in_=vtok[:, 64, :]), reads=[b_v], writes=[b_dbg])
        S.op("dve", lambda e: e.tensor_copy(out=dbg[:, 1, 128:256], in_=vtok[:, 65, :]), reads=[b_v], writes=[b_dbg])
        S.op("dve", lambda e: e.tensor_copy(out=dbg[:, 2, 0:256], in_=kTb[:, 8192:8448]), reads=[b_k], writes=[b_dbg])
        for i in range(3):
            S.dma("sp", lambda e, i=i: e.dma_start(out=os_d[i, 0, :, 0:512], in_=dbg[:, i, :]), reads=[b_dbg], sembuf=b_dbg)
        return [b_dbg]
    HALF = int(_os.environ.get("MK_HALF", "66"))
    acc = Ring(S, nc, st, "a_acc", [128, 4, 512], F32, 2)
    for (q0, qn) in QT:
        nkc = 2 if q0 == 0 else int(_os.environ.get('MK_NKC', '66'))
        at, ab = acc.next()
        kc0 = 0 if q0 == 0 else int(_os.environ.get('MK_KC0', '0'))
        groups = [(g0, min(g0 + HALF, nkc)) for g0 in range(kc0, nkc, HALF)]
        steps = [(c, gi, g0, g1, kc) for c in range(2) for gi, (g0, g1) in enumerate(groups) for kc in range(g0, g1)]
        pending = []

        def emit_qk(step, q0=q0, qn=qn):
            c, gi, g0, g1, kc = step
            sp_, sb_ = ps_s.next()
            S.op("pe", lambda e, sp_=sp_, c=c, kc=kc, q0=q0, qn=qn: e.matmul(
                sp_[:, 0:qn], lhsT=kTb[:, kc * 128:(kc + 1) * 128],
                rhs=qTb[:, c, q0:q0 + qn], start=True, stop=True),
                reads=[b_k, b_q], writes=[sb_])
            pt_, ptb = pT.next()
            S.op("act", lambda e, sp_=sp_, pt_=pt_, qn=qn: e.activation(
                out=pt_[:, 0:qn], in_=sp_[:, 0:qn], func=AF.Exp, scale=0.125), reads=[sb_], writes=[ptb])
            return (step, pt_, ptb)

        def emit_pv(item, qn=qn, at=at, ab=ab):
            (c, gi, g0, g1, kc), pt_, ptb = item
            (pn, pd), (bn, bd) = ps_nd[c], b_nd[c]
            S.op("pe", lambda e, pn=pn, pt_=pt_, kc=kc, qn=qn, g0=g0, g1=g1: e.matmul(
                pn[:, 0:qn], lhsT=vtok[:, kc, :], rhs=pt_[:, 0:qn], start=(kc == g0), stop=(kc == g1 - 1)),
                reads=[b_v, ptb], writes=[bn])
            S.op("pe", lambda e, pd=pd, pt_=pt_, kc=kc, qn=qn, g0=g0, g1=g1: e.matmul(
                pd[:, 0:qn], lhsT=ones_b[:], rhs=pt_[:, 0:qn], start=(kc == g0), stop=(kc == g1 - 1)),
                reads=[b_ob, ptb], writes=[bd])
            if kc == g1 - 1:
                if gi == 0:
                    S.op("dve", lambda e, pn=pn, c=c: e.tensor_copy(out=at[:, c, 0:qn], in_=pn[:, 0:qn]),
                         reads=[bn], writes=[ab])
                    S.op("dve", lambda e, pd=pd, c=c: e.tensor_copy(out=at[:, 2 + c, 0:qn], in_=pd[:, 0:qn]),
                         reads=[bd], writes=[ab])
                else:
                    S.op("dve", lambda e, pn=pn, c=c: e.tensor_tensor(
                        out=at[:, c, 0:qn], in0=pn[:, 0:qn], in1=at[:, c, 0:qn], op=ALU.add), reads=[bn, ab], writes=[ab])
                    S.op("dve", lambda e, pd=pd, c=c: e.tensor_tensor(
                        out=at[:, 2 + c, 0:qn], in0=pd[:, 0:qn], in1=at[:, 2 + c, 0:qn], op=ALU.add),
                        reads=[bd, ab], writes=[ab])

        for step in steps:
            pending.append(emit_qk(step))
            if len(pending) > 4:
                emit_pv(pending.pop(0))
        while pending:
            emit_pv(pending.pop(0))
        et, eb = epi.next()
        for c in range(2):
            S.op("dve", lambda e, et=et, at=at, c=c, qn=qn: e.reciprocal(out=et[:, 2 + c, 0:qn], in_=at[:, 2 + c, 0:qn]),
                 reads=[ab], writes=[eb])
            S.op("dve", lambda e, et=et, at=at, c=c, qn=qn: e.tensor_tensor(
                out=et[:, c, 0:qn], in0=at[:, c, 0:qn], in1=et[:, 2 + c, 0:qn], op=ALU.mult), reads=[ab, eb], writes=[eb])
        S.op("dve", lambda e, et=et, qn=qn: e.scalar_tensor_tensor(
            out=et[:, 0, 0:qn], in0=et[:, 1, 0:qn], scalar=lamv[:, 3:4], in1=et[:, 0, 0:qn], op0=ALU.mult, op1=ALU.add),
            reads=[eb, b_lv], writes=[eb])
        S.op("act", lambda e, et=et, qn=qn: e.activation(out=et[:, 1, 0:qn], in_=et[:, 0, 0:qn], func=AF.Square),
             reads=[eb], writes=[eb])
        S.op("pe", lambda e, et=et, qn=qn: e.matmul(ps_ss[:, 0:qn], lhsT=ones_f[:], rhs=et[:, 1, 0:qn],
                                                    start=True, stop=True), reads=[eb, b_of], writes=[b_ss])
        S.op("dve", lambda e, et=et, qn=qn: e.tensor_scalar(
            out=et[:, 2, 0:qn], in0=ps_ss[:, 0:qn], scalar1=1.0 / 128, scalar2=EPS, op0=ALU.mult, op1=ALU.add),
            reads=[b_ss], writes=[eb])
        S.op("act", lambda e, et=et, qn=qn: e.activation(out=et[:, 2, 0:qn], in_=et[:, 2, 0:qn], func=AF.Sqrt),
             reads=[eb], writes=[eb])
        S.op("dve", lambda e, et=et, qn=qn: e.reciprocal(out=et[:, 2, 0:qn], in_=et[:, 2, 0:qn]),
             reads=[eb], writes=[eb])
        ot, ob = ost.next()
        S.op("dve", lambda e, et=et, ot=ot, qn=qn: e.scalar_tensor_tensor(
            out=ot[:, 0:qn], in0=et[:, 0, 0:qn], scalar=subg[:, 0:1], in1=et[:, 2, 0:qn], op0=ALU.mult, op1=ALU.mult),
            reads=[eb, b_sg], writes=[ob])
        store_group(S, "sp", os_d, 0, ot, ob, q0, qn)
    return ost.b


def emit_p2b(S, nc, st, ps, zr_d, b_zr, os_d, consts):
    xs = st.enter_context(nc.sbuf_tensor("b_xs", [128, NG], F32))
    u = st.enter_context(nc.sbuf_tensor("b_u", [128, NG], F32))
    aa = st.enter_context(nc.sbuf_tensor("b_a", [128, NG], F32))
    bb = st.enter_context(nc.sbuf_tensor("b_b", [128, NG], F32))
    hf = st.enter_context(nc.sbuf_tensor("b_hf", [128, NG], F32))
    convw = st.enter_context(nc.sbuf_tensor("b_cw", [128, 4], F32))
    convb = st.enter_context(nc.sbuf_tensor("b_cb", [128, 1], F32))
    gatew = st.enter_context(nc.sbuf_tensor("b_gw", [128, 4, 128], F32))
    gateb = st.enter_context(nc.sbuf_tensor("b_gb", [128, 4], F32))
    lrul = st.enter_context(nc.sbuf_tensor("b_ll", [128, 2], F32))
    cneg = st.enter_context(nc.sbuf_tensor("b_cn", [128, 2], F32))
    b_xs, b_u, b_a, b_b, b_hf, b_cw, b_cb, b_gw, b_gb, b_ll, b_cn = S.bufs(11, "bb")
    for t, d_, b_ in ((convw, "convw", b_cw), (convb, "convb", b_cb), (gatew, "gatew", b_gw), (gateb, "gateb", b_gb),
                      (lrul, "lrul", b_ll)):
        S.dma("sp", lambda e, t=t, d_=d_: e.dma_start(out=t[:], in_=consts[d_]), writes=[b_])
    S.op("act", lambda e: e.activation(out=cneg[:], in_=lrul[:], func=AF.Exp, scale=-1.0), reads=[b_ll], writes=[b_cn])
    S.op("act", lambda e: e.activation(out=cneg[:], in_=cneg[:], func=AF.Ln, bias=1.0), reads=[b_cn], writes=[b_cn])
    S.op("dve", lambda e: e.tensor_scalar(out=cneg[:], in0=cneg[:], scalar1=-8.0, scalar2=None, op0=ALU.mult),
         reads=[b_cn], writes=[b_cn])
    load_group(S, nc, "sp", zr_d, b_zr, 3, xs, b_xs)
    SEG = [(0, 256), (256, 8192)]
    for (o, n) in SEG:
        S.op("dve", lambda e, o=o, n=n: e.tensor_scalar(out=u[:, o:o + n], in0=xs[:, o:o + n], scalar1=convw[:, 2:3],
                                                        scalar2=convb[:, 0:1], op0=ALU.mult, op1=ALU.add),
             reads=[b_xs, b_cw, b_cb], writes=[b_u])
        for (j, sh) in ((0, -2), (1, -1), (3, 1)):
            if sh < 0:
                oo, io, m = o - sh, o, n + sh
            else:
                oo, io, m = o, o + sh, n - sh
            S.op("dve", lambda e, oo=oo, io=io, m=m, j=j: e.scalar_tensor_tensor(
                out=u[:, oo:oo + m], in0=xs[:, io:io + m], scalar=convw[:, j:j + 1], in1=u[:, oo:oo + m],
                op0=ALU.mult, op1=ALU.add), reads=[b_xs, b_cw, b_u], writes=[b_u])
    ri = Ring(S, nc, st, "b_ri", [128, 3, 512], F32, 3)
    pr = PsRing(S, ps, [0, 1, 2, 3])
    TL = [(0, 256)] + [(256 + 512 * m, 512) for m in range(16)]
    for d in range(2):
        for (o, n) in TL:
            rt, rb = ri.next()
            for g in range(2):
                pt, pb = pr.next()
                S.op("pe", lambda e, pt=pt, d=d, g=g, o=o, n=n: e.matmul(
                    pt[:, 0:n], lhsT=gatew[:, d * 2 + g, :], rhs=u[:, o:o + n], start=True, stop=True),
                    reads=[b_gw, b_u], writes=[pb])
                S.op("act", lambda e, pt=pt, rt=rt, d=d, g=g, n=n: e.activation(
                    out=rt[:, g, 0:n], in_=pt[:, 0:n], func=AF.Sigmoid, bias=gateb[:, d * 2 + g:d * 2 + g + 1]),
                    reads=[pb, b_gb], writes=[rb])
            S.op("act", lambda e, rt=rt, d=d, o=o, n=n: e.activation(
                out=aa[:, o:o + n], in_=rt[:, 0, 0:n], func=AF.Exp, scale=cneg[:, d:d + 1]),
                reads=[rb, b_cn], writes=[b_a])
            S.op("dve", lambda e, rt=rt, o=o, n=n: e.tensor_tensor(
                out=rt[:, 2, 0:n], in0=aa[:, o:o + n], in1=aa[:, o:o + n], op=ALU.mult), reads=[b_a], writes=[rb])
            S.op("dve", lambda e, rt=rt, n=n: e.tensor_scalar(
                out=rt[:, 2, 0:n], in0=rt[:, 2, 0:n], scalar1=-1.0, scalar2=1.0, op0=ALU.mult, op1=ALU.add),
                reads=[rb], writes=[rb])
            S.op("act", lambda e, rt=rt, n=n: e.activation(out=rt[:, 2, 0:n], in_=rt[:, 2, 0:n], func=AF.Sqrt),
                 reads=[rb], writes=[rb])
            S.op("pool", lambda e, rt=rt, o=o, n=n: e.tensor_tensor(
                out=rt[:, 1, 0:n], in0=rt[:, 1, 0:n], in1=u[:, o:o + n], op=ALU.mult), reads=[rb, b_u], writes=[rb])
            S.op("dve", lambda e, rt=rt, o=o, n=n: e.tensor_tensor(
                out=bb[:, o:o + n], in0=rt[:, 2, 0:n], in1=rt[:, 1, 0:n], op=ALU.mult), reads=[rb], writes=[b_b])
        if d == 0:
            S.op("dve", lambda e: e.tensor_tensor_scan(out=hf[:, 0:256], data0=aa[:, 0:256], data1=bb[:, 0:256],
                                                       initial=0.0, op0=ALU.mult, op1=ALU.add),
                 reads=[b_a, b_b], writes=[b_hf])
            S.op("dve", lambda e: e.tensor_tensor_scan(out=hf[:, 256:NG], data0=aa[:, 256:NG], data1=bb[:, 256:NG],
                                                       initial=hf[:, 255:256], op0=ALU.mult, op1=ALU.add),
                 reads=[b_a, b_b, b_hf], writes=[b_hf])
        else:
            S.op("dve", lambda e: e.tensor_tensor_scan(out=xs[:, 255::-1], data0=aa[:, 255::-1], data1=bb[:, 255::-1],
                                                       initial=0.0, op0=ALU.mult, op1=ALU.add),
                 reads=[b_a, b_b, b_u], writes=[b_xs])
            S.op("dve", lambda e: e.tensor_tensor_scan(out=xs[:, NG - 1:255:-1], data0=aa[:, NG - 1:255:-1],
                                                       data1=bb[:, NG - 1:255:-1], initial=xs[:, 0:1],
                                                       op0=ALU.mult, op1=ALU.add),
                 reads=[b_a, b_b, b_xs], writes=[b_xs])
    S.op("pool", lambda e: e.tensor_tensor(out=hf[:], in0=hf[:], in1=xs[:], op=ALU.add), reads=[b_hf, b_xs],
         writes=[b_hf])
    load_group(S, nc, "sp", zr_d, b_zr, 4, u, b_u)
    for (o, n) in TL:
        S.op("dve", lambda e, o=o, n=n: e.tensor_tensor(out=aa[:, o:o + n], in0=u[:, o:o + n], in1=u[:, o:o + n],
                                                        op=ALU.mult), reads=[b_u], writes=[b_a])
        S.op("dve", lambda e, o=o, n=n: e.tensor_scalar(out=aa[:, o:o + n], in0=aa[:, o:o + n], scalar1=0.044715,
                                                        scalar2=1.0, op0=ALU.mult, op1=ALU.add),
             reads=[b_a], writes=[b_a])
        S.op("pool", lambda e, o=o, n=n: e.tensor_tensor(out=aa[:, o:o + n], in0=aa[:, o:o + n], in1=u[:, o:o + n],
                                                         op=ALU.mult), reads=[b_a, b_u], writes=[b_a])
        S.op("act", lambda e, o=o, n=n: e.activation(out=aa[:, o:o + n], in_=aa[:, o:o + n], func=AF.Sigmoid,
                                                     scale=1.5957691216057308), reads=[b_a], writes=[b_a])
        S.op("pool", lambda e, o=o, n=n: e.tensor_tensor(out=aa[:, o:o + n], in0=aa[:, o:o + n], in1=u[:, o:o + n],
                                                         op=ALU.mult), reads=[b_a, b_u], writes=[b_a])
        S.op("dve", lambda e, o=o, n=n: e.tensor_tensor(out=bb[:, o:o + n], in0=aa[:, o:o + n], in1=hf[:, o:o + n],
                                                        op=ALU.mult), reads=[b_a, b_hf], writes=[b_b])
    store_group(S, "sp", os_d, 1, bb, b_b, 0, NG)
    return [b_b]


def na_case(r):
    if r < 4:
        return r, 0
    if r <= 124:
        return 4, r - 4
    return 5 + (r - 125), 120


def emit_p2c(S, nc, st, ps, zr_d, b_zr, os_d, consts):
    stg = Ring(S, nc, st, "c_stg", [128, NG], F32, 2)
    qb = st.enter_context(nc.sbuf_tensor("c_q", [128, 2, NG], BF16))
    kb = st.enter_context(nc.sbuf_tensor("c_k", [128, NG], BF16))
    vte = st.enter_context(nc.sbuf_tensor("c_ve", [128, 66, 128], BF16))
    vto = st.enter_context(nc.sbuf_tensor("c_vo", [128, 65, 128], BF16))
    oc = st.enter_context(nc.sbuf_tensor("c_o", [128, NG], F32))
    nabt = st.enter_context(nc.sbuf_tensor("c_bt", [128, 2, 8, 4, 64], F32))
    ident = st.enter_context(nc.sbuf_tensor("c_ident", [128, 128], F32))
    ones_b = st.enter_context(nc.sbuf_tensor("c_onesb", [128, 128], BF16))
    b_q, b_k, b_ve, b_vo, b_oc, b_bt, b_id, b_ob = S.bufs(8, "cc")
    S.dma("sp", lambda e: e.dma_start(out=nabt[:], in_=consts["nabt"]), writes=[b_bt])
    S.dma("sp", lambda e: e.dma_start(out=ident[:], in_=consts["ident"]), writes=[b_id])
    S.op("pool", lambda e: e.memset(ones_b[:], 1.0), writes=[b_ob])
    pr = PsRing(S, ps, [0, 1, 2, 3, 4, 5, 6, 7])
    S.op("pool", lambda e: e.memset(qb[:, 0, :], 0.0), writes=[b_q])
    S.op("pool", lambda e: e.memset(qb[:, 1, :], 0.0), writes=[b_q])
    for grp, dst, b_dst in ((5, qb, b_q), (6, kb, b_k)):
        xs, xb = stg.next()
        load_group(S, nc, "sp", zr_d, b_zr, grp, xs, xb)
        for i in range(4):
            o = i * (NG // 4)
            eng = "dve" if i % 2 == 0 else "pool"
            if grp == 5:
                for hl in range(2):
                    S.op(eng, lambda e, xs=xs, o=o, hl=hl: e.tensor_copy(
                        out=qb[64 * hl:64 * hl + 64, hl, o:o + NG // 4], in_=xs[64 * hl:64 * hl + 64, o:o + NG // 4]),
                        reads=[xb], writes=[b_q])
            else:
                S.op(eng, lambda e, xs=xs, dst=dst, o=o: e.tensor_copy(out=dst[:, o:o + NG // 4], in_=xs[:, o:o + NG // 4]),
                     reads=[xb], writes=[b_dst])
    xs, xb = stg.next()
    load_group(S, nc, "sp", zr_d, b_zr, 7, xs, xb)
    build_vtok(S, nc, pr, xs, xb, ident, b_id, vte, b_ve, 0, 66)
    build_vtok(S, nc, pr, xs, xb, ident, b_id, vto, b_vo, 64, 65)
    scr = st.enter_context(nc.sbuf_tensor("c_scr", [128, 1], F32))
    sched_barrier(S, scr[:])
    sbias = Ring(S, nc, st, "c_sb", [128, 256], F32, 3)
    pT = Ring(S, nc, st, "c_pT", [128, 384], BF16, 3)
    rc = Ring(S, nc, st, "c_rc", [128, 256], F32, 3)
    ps_s = PsRing(S, ps, [0, 1])
    ps_n = PsRing(S, ps, [2, 3])
    ps_d = PsRing(S, ps, [4, 5])
    for hl in range(2):
        P0 = 64 * hl
        sp_, sb_ = ps_s.next()
        pt_, ptb = pT.next()
        pn, bn = ps_n.next()
        pd, bd = ps_d.next()
        for kc in range(2):
            if kc == 1:
                sp_, sb_ = ps_s.next()
                pt_, ptb = pT.next()
            S.op("pe", lambda e, sp_=sp_, P0=P0, kc=kc, hl=hl: e.matmul(
                sp_[:, 0:256], lhsT=kb[:, kc * 128:(kc + 1) * 128], rhs=qb[:, hl, 0:256],
                start=True, stop=True), reads=[b_k, b_q], writes=[sb_])
            S.op("act", lambda e, sp_=sp_, pt_=pt_: e.activation(out=pt_[:, 0:256], in_=sp_[:, 0:256], func=AF.Exp,
                                                                 scale=0.125), reads=[sb_], writes=[ptb])
            S.op("pe", lambda e, pn=pn, pt_=pt_, kc=kc: e.matmul(pn[:, 0:256], lhsT=vte[:, kc, :], rhs=pt_[:, 0:256],
                                                                 start=(kc == 0), stop=(kc == 1)),
                 reads=[b_ve, ptb], writes=[bn])
            S.op("pe", lambda e, pd=pd, pt_=pt_, kc=kc: e.matmul(pd[:, 0:256], lhsT=ones_b[:], rhs=pt_[:, 0:256],
                                                                 start=(kc == 0), stop=(kc == 1)),
                 reads=[b_ob, ptb], writes=[bd])
        rt, rb = rc.next()
        S.op("dve", lambda e, rt=rt, pd=pd, P0=P0: e.reciprocal(out=rt[P0:P0 + 64, 0:256], in_=pd[P0:P0 + 64, 0:256]),
             reads=[bd], writes=[rb])
        S.op("dve", lambda e, rt=rt, pn=pn, P0=P0: e.tensor_tensor(
            out=oc[P0:P0 + 64, 0:256], in0=pn[P0:P0 + 64, 0:256], in1=rt[P0:P0 + 64, 0:256], op=ALU.mult),
            reads=[bn, rb], writes=[b_oc])
    for r in range(128):
        case, r0 = na_case(r)
        q0 = 256 + 64 * r
        if r0 % 2 == 0:
            vt, bv, cbase = vte, b_ve, (256 + 64 * r0) // 128
        else:
            vt, bv, cbase = vto, b_vo, (256 + 64 * r0 - 64) // 128
        k0 = 256 + 64 * r0
        for hl in range(2):
            P0 = 64 * hl
            sp_, sb_ = ps_s.next()
            for c in range(6):
                ks = k0 + 128 * c if c < 4 else 128 * (c - 4)
                S.op("pe", lambda e, sp_=sp_, P0=P0, ks=ks, c=c, q0=q0, hl=hl: e.matmul(
                    sp_[:, c * 64:(c + 1) * 64], lhsT=kb[:, ks:ks + 128], rhs=qb[:, hl, q0:q0 + 64],
                    start=True, stop=True), reads=[b_k, b_q], writes=[sb_])
            bt_, btb = sbias.next()
            S.op("dve", lambda e, sp_=sp_, bt_=bt_, hl=hl, case=case: e.scalar_tensor_tensor(
                out=bt_[:], in0=sp_[:, 0:256], scalar=0.125,
                in1=nabt[:, hl, case, :, :].rearrange("p c q -> p (c q)"), op0=ALU.mult, op1=ALU.add),
                reads=[sb_, b_bt], writes=[btb])
            pt_, ptb = pT.next()
            S.op("act", lambda e, pt_=pt_, bt_=bt_: e.activation(out=pt_[:, 0:256], in_=bt_[:], func=AF.Exp),
                 reads=[btb], writes=[ptb])
            S.op("act", lambda e, pt_=pt_, sp_=sp_: e.activation(out=pt_[:, 256:384], in_=sp_[:, 256:384], func=AF.Exp,
                                                                 scale=0.125), reads=[sb_], writes=[ptb])
            pn, bn = ps_n.next()
            pd, bd = ps_d.next()
            for c in range(6):
                if c < 4:
                    lhs, bl = vt[:, cbase + c, :], bv
                else:
                    lhs, bl = vte[:, c - 4, :], b_ve
                S.op("pe", lambda e, pn=pn, pt_=pt_, c=c, lhs=lhs: e.matmul(
                    pn[:, 0:64], lhsT=lhs, rhs=pt_[:, c * 64:(c + 1) * 64], start=(c == 0), stop=(c == 5)),
                    reads=[bl, ptb], writes=[bn])
                S.op("pe", lambda e, pd=pd, pt_=pt_, c=c: e.matmul(
                    pd[:, 0:64], lhsT=ones_b[:], rhs=pt_[:, c * 64:(c + 1) * 64], start=(c == 0), stop=(c == 5)),
                    reads=[b_ob, ptb], writes=[bd])
            rt, rb = rc.next()
            S.op("dve", lambda e, rt=rt, pd=pd, P0=P0: e.reciprocal(out=rt[P0:P0 + 64, 0:64], in_=pd[P0:P0 + 64, 0:64]),
                 reads=[bd], writes=[rb])
            S.op("dve", lambda e, rt=rt, pn=pn, P0=P0, q0=q0: e.tensor_tensor(
                out=oc[P0:P0 + 64, q0:q0 + 64], in0=pn[P0:P0 + 64, 0:64], in1=rt[P0:P0 + 64, 0:64], op=ALU.mult),
                reads=[bn, rb], writes=[b_oc])
    store_group(S, "sp", os_d, 2, oc, b_oc, 0, NG)
    return [b_oc]


def emit_final_norm(S, nc, st, ps, xT_d, b_xd, gvec, b_gvec, out_d, tag):
    xr = Ring(S, nc, st, f"{tag}_x", [128, NT], F32, 3)
    sq = Ring(S, nc, st, f"{tag}_sq", [128, NT], F32, 2)
    tm = Ring(S, nc, st, f"{tag}_tm", [128, NT], F32, 3)
    ones = st.enter_context(nc.sbuf_tensor(f"{tag}_ones", [128, 128], F32))
    rstd = st.enter_context(nc.sbuf_tensor(f"{tag}_rstd", [128, NT], F32))
    b_ones, b_rstd = S.bufs(2, tag)
    S.op("pool", lambda e: e.memset(ones[:], 1.0), writes=[b_ones])
    xv = xT_d.rearrange("(k p) t -> k p t", p=128)
    pss = [(ps[i], S.buf(f"{tag}_pss{i}")) for i in range(3)]
    for k in range(32):
        xt, xb = xr.next()
        S.dma("sp", lambda e, xt=xt, k=k: e.dma_start(out=xt[:], in_=xv[k]), reads=[b_xd], writes=[xb])
        qt, qb = sq.next()
        S.op("act", lambda e, xt=xt, qt=qt: e.activation(out=qt[:], in_=xt[:], func=AF.Square), reads=[xb], writes=[qb])
        for i, (o, n) in enumerate(TT):
            S.op("pe", lambda e, i=i, o=o, n=n, qt=qt, k=k: e.matmul(
                pss[i][0][:, 0:n], lhsT=ones[:], rhs=qt[:, o:o + n], start=(k == 0), stop=(k == 31)),
                reads=[qb, b_ones], writes=[pss[i][1]])
    for i, (o, n) in enumerate(TT):
        S.op("dve", lambda e, i=i, o=o, n=n: e.tensor_scalar(
            out=rstd[:, o:o + n], in0=pss[i][0][:, 0:n], scalar1=1.0 / D, scalar2=EPS, op0=ALU.mult, op1=ALU.add),
            reads=[pss[i][1]], writes=[b_rstd])
    S.op("act", lambda e: e.activation(out=rstd[:], in_=rstd[:], func=AF.Sqrt), reads=[b_rstd], writes=[b_rstd])
    S.op("dve", lambda e: e.reciprocal(out=rstd[:], in_=rstd[:]), reads=[b_rstd], writes=[b_rstd])
    for k in range(32):
        xt, xb = xr.next()
        S.dma("sp", lambda e, xt=xt, k=k: e.dma_start(out=xt[:], in_=xv[k]), reads=[b_xd], writes=[xb])
        tt, tb = tm.next()
        S.op("dve", lambda e, xt=xt, tt=tt, k=k: e.scalar_tensor_tensor(
            out=tt[:], in0=xt[:], scalar=gvec[:, k:k + 1], in1=rstd[:], op0=ALU.mult, op1=ALU.mult),
            reads=[xb, b_rstd, b_gvec], writes=[tb])
        S.dma("sp", lambda e, tt=tt, k=k: e.dma_start(out=out_d[k], in_=tt[:, NC_CTX:NT]), reads=[tb], sembuf=tb)
    return tm.b


def emit_p3(S, nc, ps, scr, layer, last, d):
    MB = layer * 192
    with contextlib.ExitStack() as st0:
        modv = st0.enter_context(nc.sbuf_tensor("p3_modv", [128, 384, 2], F32))
        b_modv = S.buf("p3modv")
        S.dma("sp", lambda e: e.dma_start(out=modv[:], in_=d["modv"]), writes=[b_modv])
        b_x1d, b_h2d, b_x2d = S.bufs(3, "p3d")
        with contextlib.ExitStack() as st:
            yT = st.enter_context(nc.sbuf_tensor("p3_yT", [128, 32, NT], BF16))
            b_y = S.buf("p3y")
            with contextlib.ExitStack() as sm:
                oT = sm.enter_context(nc.sbuf_tensor("p3_oT", [128, 3, 8, NT], BF16))
                b_o = S.buf("p3o")
                for j in range(8):
                    for br in range(3):
                        S.dma("pool", lambda e, j=j, br=br: e.dma_start(out=oT[:, br, j, :], in_=d["orecv"][j, br]),
                              writes=[b_o])
                wr = Ring(S, nc, sm, "p3_wbr", [128, 3, 8, 256], BF16, 2)
                gr = Ring(S, nc, sm, "p3_gs", [128, NT], F32, 4)
                ya = Ring(S, nc, sm, "p3_ya", [128, NT], F32, 2)
                tr = Ring(S, nc, sm, "p3_tm", [128, 352], F32, 3)
                pr = PsRing(S, ps, [0, 1, 2, 3, 4, 5])
                for wi in range(16):
                    wt, wb = wr.next()
                    for br in range(3):
                        S.dma("pool", lambda e, wt=wt, br=br, wi=wi: e.dma_start(
                            out=wt[:, br, :, :],
                            in_=d["wbr"][br].rearrange("(k p) f -> p k f", p=128)[:, :, wi * 256:(wi + 1) * 256]),
                            writes=[wb])
                    for c in range(2):
                        dc = wi * 2 + c
                        yt, yb = ya.next()
                        for br in range(3):
                            gt, gb = gr.next()
                            S.dma("sp", lambda e, gt=gt, br=br, dc=dc: e.dma_start(out=gt[:], in_=d["gsig"][br * 32 + dc]),
                                  writes=[gb])
                            for (o, n) in TT:
                                pt, pb = pr.next()
                                for k in range(8):
                                    S.op("pe", lambda e, pt=pt, wt=wt, br=br, k=k, c=c, o=o, n=n: e.matmul(
                                        pt[:, 0:n], lhsT=wt[:, br, k, c * 128:(c + 1) * 128], rhs=oT[:, br, k, o:o + n],
                                        start=(k == 0), stop=(k == 7)), reads=[wb, b_o], writes=[pb])
                                if br == 0:
                                    S.op("dve", lambda e, pt=pt, yt=yt, gt=gt, o=o, n=n: e.tensor_tensor(
                                        out=yt[:, o:o + n], in0=pt[:, 0:n], in1=gt[:, o:o + n], op=ALU.mult),
                                        reads=[pb, gb], writes=[yb])
                                else:
                                    t_, tb_ = tr.next()
                                    S.op("dve", lambda e, pt=pt, t_=t_, gt=gt, o=o, n=n: e.tensor_tensor(
                                        out=t_[:, 0:n], in0=pt[:, 0:n], in1=gt[:, o:o + n], op=ALU.mult),
                                        reads=[pb, gb], writes=[tb_])
                                    S.op("pool", lambda e, t_=t_, yt=yt, o=o, n=n: e.tensor_tensor(
                                        out=yt[:, o:o + n], in0=yt[:, o:o + n], in1=t_[:, 0:n], op=ALU.add),
                                        reads=[tb_, yb], writes=[yb])
                        S.op("act", lambda e, yt=yt, dc=dc: e.activation(out=yT[:, dc, :], in_=yt[:], func=AF.Copy),
                             reads=[yb], writes=[b_y])
                sched_barrier(S, scr)
            with contextlib.ExitStack() as so:
                wr = Ring(S, nc, so, "p3_wo", [128, 32, 256], BF16, 3)
                xr = Ring(S, nc, so, "p3_xi", [128, NT], F32, 3)
                xo = Ring(S, nc, so, "p3_xo", [128, NT], F32, 3)
                pr = PsRing(S, ps, [0, 1, 2, 3, 4, 5])
                wv = d["wout"].rearrange("(k p) f -> p k f", p=128)
                xv = d["xT"].rearrange("(k p) t -> k p t", p=128)
                x1v = d["x1T"].rearrange("(k p) t -> k p t", p=128)
                for wi in range(16):
                    wt, wb = wr.next()
                    S.dma("pool", lambda e, wt=wt, wi=wi: e.dma_start(out=wt[:], in_=wv[:, :, wi * 256:(wi + 1) * 256]),
                          writes=[wb])
                    for c in range(2):
                        dc = wi * 2 + c
                        xt, xb = xr.next()
                        S.dma("sp", lambda e, xt=xt, dc=dc: e.dma_start(out=xt[:], in_=xv[dc]), writes=[xb])
                        ot, ob = xo.next()
                        for (o, n) in TT:
                            pt, pb = pr.next()
                            for k in range(32):
                                S.op("pe", lambda e, pt=pt, wt=wt, k=k, c=c, o=o, n=n: e.matmul(
                                    pt[:, 0:n], lhsT=wt[:, k, c * 128:(c + 1) * 128], rhs=yT[:, k, o:o + n],
                                    start=(k == 0), stop=(k == 31)), reads=[wb, b_y], writes=[pb])
                            segs = [(0, NC_CTX, 1), (NC_CTX, n, 0)] if o == 0 else [(0, n, 0)]
                            for (a, b, col) in segs:
                                S.op("dve", lambda e, pt=pt, ot=ot, xt=xt, o=o, a=a, b=b, col=col, dc=dc: e.scalar_tensor_tensor(
                                    out=ot[:, o + a:o + b], in0=pt[:, a:b], scalar=modv[:, MB + 64 + dc, col:col + 1],
                                    in1=xt[:, o + a:o + b], op0=ALU.mult, op1=ALU.add),
                                    reads=[pb, xb, b_modv], writes=[ob])
                        S.dma("sp", lambda e, ot=ot, dc=dc: e.dma_start(out=x1v[dc], in_=ot[:]), reads=[ob], sembuf=ob)
                sched_barrier(S, scr)
        gT = st0.enter_context(nc.sbuf_tensor("p3_gT", [32, NT], F32))
        b_gT = S.buf("p3gT")
        with contextlib.ExitStack() as sn:
            gvec = sn.enter_context(nc.sbuf_tensor("p3_n2g", [128, 32], F32))
            h2T = sn.enter_context(nc.sbuf_tensor("p3_h2T", [128, 32, NT], BF16))
            wrt = sn.enter_context(nc.sbuf_tensor("p3_wrt", [128, 32, 32], BF16))
            brt = sn.enter_context(nc.sbuf_tensor("p3_brt", [128, 32], F32))
            ident = sn.enter_context(nc.sbuf_tensor("p3_ident", [128, 128], F32))
            b_gv, b_h2, b_wrt, b_brt, b_id = S.bufs(5, "p3n")
            S.dma("sp", lambda e: e.dma_start(out=gvec[:], in_=d["n2g"]), writes=[b_gv])
            S.dma("sp", lambda e: e.dma_start(out=brt[:], in_=d["brt"]), writes=[b_brt])
            S.dma("sp", lambda e: e.dma_start(out=ident[:], in_=d["ident"]), writes=[b_id])
            S.dma("pool", lambda e: e.dma_start(out=wrt[:], in_=d["wrt"].rearrange("(k p) f -> p k f", p=128)),
                  writes=[b_wrt])
            emit_norm_mod(S, nc, sn, ps, d["x1T"], b_x1d, modv, b_modv, gvec, b_gv, MB + 96, h2T, b_h2, "p3nm")
            S.dma("sp", lambda e: e.dma_start(out=d["h2d"], in_=h2T[:]), reads=[b_h2], sembuf=b_h2)
            lg = Ring(S, nc, sn, "p3_lg", [128, 3, 32], F32, 3)
            m8 = Ring(S, nc, sn, "p3_m8", [128, 12], F32, 3)
            pr = PsRing(S, ps, [3, 4, 5, 6])
            for i in range(9):
                t0 = i * 128
                nt_ = min(128, NT - t0)
                pt, pb = pr.next()
                for k in range(32):
                    S.op("pe", lambda e, pt=pt, k=k, t0=t0, nt_=nt_: e.matmul(
                        pt[0:nt_, 0:32], lhsT=h2T[:, k, t0:t0 + nt_], rhs=wrt[:, k, :], start=(k == 0), stop=(k == 31)),
                        reads=[b_h2, b_wrt], writes=[pb])
                lt, lb = lg.next()
                mt, mb = m8.next()
                S.op("dve", lambda e, pt=pt, lt=lt, nt_=nt_: e.tensor_tensor(
                    out=lt[0:nt_, 0, :], in0=pt[0:nt_, 0:32], in1=brt[0:nt_, :], op=ALU.add), reads=[pb, b_brt], writes=[lb])
                S.op("dve", lambda e, lt=lt, mt=mt, nt_=nt_: e.max(out=mt[0:nt_, 0:8], in_=lt[0:nt_, 0, :]),
                     reads=[lb], writes=[mb])
                S.op("dve", lambda e, mt=mt, nt_=nt_: e.tensor_scalar(
                    out=mt[0:nt_, 8:9], in0=mt[0:nt_, 0:1], scalar1=-1.0, scalar2=None, op0=ALU.mult),
                    reads=[mb], writes=[mb])
                S.op("act", lambda e, lt=lt, mt=mt, nt_=nt_: e.activation(
                    out=lt[0:nt_, 1, :], in_=lt[0:nt_, 0, :], func=AF.Exp, bias=mt[0:nt_, 8:9]), reads=[lb, mb], writes=[lb])
                S.op("dve", lambda e, lt=lt, mt=mt, nt_=nt_: e.tensor_scalar(
                    out=lt[0:nt_, 2, :], in0=lt[0:nt_, 0, :], scalar1=mt[0:nt_, 3:4], scalar2=None, op0=ALU.is_ge),
                    reads=[lb, mb], writes=[lb])
                S.op("dve", lambda e, lt=lt, nt_=nt_: e.tensor_tensor(
                    out=lt[0:nt_, 1, :], in0=lt[0:nt_, 1, :], in1=lt[0:nt_, 2, :], op=ALU.mult), reads=[lb], writes=[lb])
                S.op("dve", lambda e, lt=lt, mt=mt, nt_=nt_: e.tensor_reduce(
                    out=mt[0:nt_, 9:10], in_=lt[0:nt_, 1, :], axis=AX.X, op=ALU.add), reads=[lb], writes=[mb])
                S.op("dve", lambda e, mt=mt, nt_=nt_: e.reciprocal(out=mt[0:nt_, 10:11], in_=mt[0:nt_, 9:10]),
                     reads=[mb], writes=[mb])
                S.op("dve", lambda e, lt=lt, mt=mt, nt_=nt_: e.tensor_scalar(
                    out=lt[0:nt_, 2, :], in0=lt[0:nt_, 1, :], scalar1=mt[0:nt_, 10:11], scalar2=None, op0=ALU.mult),
                    reads=[lb, mb], writes=[lb])
                pt2, pb2 = pr.next()
                S.op("pe", lambda e, pt2=pt2, lt=lt, nt_=nt_: e.transpose(
                    out=pt2[0:32, 0:nt_], in_=lt[0:nt_, 2, :], identity=ident[0:nt_, 0:nt_]),
                    reads=[lb, b_id], writes=[pb2])
                S.op("act", lambda e, pt2=pt2, t0=t0, nt_=nt_: e.activation(
                    out=gT[:, t0:t0 + nt_], in_=pt2[0:32, 0:nt_], func=AF.Copy), reads=[pb2], writes=[b_gT])
            sched_barrier(S, scr)
        with contextlib.ExitStack() as se:
            sel = se.enter_context(nc.sbuf_tensor("p3_sel", [32, 32, 128], F32))
            bdn = se.enter_context(nc.sbuf_tensor("p3_bdn", [32, 4096], F32))
            bgu = se.enter_context(nc.sbuf_tensor("p3_bgu", [128, 32, 8], F32))
            b_sel, b_bdn, b_bgu = S.bufs(3, "p3e")
            S.dma("sp", lambda e: e.dma_start(out=sel[:], in_=d["sel"]), writes=[b_sel])
            S.dma("sp", lambda e: e.dma_start(out=bdn[:], in_=d["bdn"]), writes=[b_bdn])
            S.dma("sp", lambda e: e.dma_start(out=bgu[:], in_=d["bgu"]), writes=[b_bgu])
            h2r = Ring(S, nc, se, "p3_h2", [128, 32, 352], BF16, 1)
            accr = Ring(S, nc, se, "p3_acc", [128, 32, 352], F32, 1)
            wgr = Ring(S, nc, se, "p3_wg", [128, 32, 256], BF16, 2)
            wdr = Ring(S, nc, se, "p3_wd", [128, 4, 2048], BF16, 2)
            gbr = Ring(S, nc, se, "p3_gb", [128, 352], F32, 2)
            glr = Ring(S, nc, se, "p3_gl", [128, 4, 352], F32, 2)
            tmr = Ring(S, nc, se, "p3_t", [128, 352], F32, 3)
            acr = Ring(S, nc, se, "p3_ac", [128, 4, 352], BF16, 2)
            xir = Ring(S, nc, se, "p3_x1", [128, 352], F32, 3)
            xor_ = Ring(S, nc, se, "p3_x2", [128, 352], F32, 3)
            pr = PsRing(S, ps, [0, 1, 2, 3, 4, 5, 6, 7])
            x1v = d["x1T"].rearrange("(k p) t -> k p t", p=128)
            x2v = d["x2T"].rearrange("(k p) t -> k p t", p=128)
            for (o, n) in TT:
                ht, hb = h2r.next()
                S.dma("sp", lambda e, ht=ht, o=o, n=n: e.dma_start(out=ht[:], in_=d["h2d"][:, :, o:o + n]),
                      reads=[b_h2d], writes=[hb])
                at, ab = accr.next()
                for dc in range(32):
                    pt, pb = pr.next()
                    S.op("pe", lambda e, pt=pt, dc=dc, o=o, n=n: e.matmul(
                        pt[:, 0:n], lhsT=bdn[:, dc * 128:(dc + 1) * 128], rhs=gT[:, o:o + n], start=True, stop=True),
                        reads=[b_bdn, b_gT], writes=[pb])
                    S.op("act", lambda e, pt=pt, at=at, dc=dc, n=n: e.activation(out=at[:, dc, :], in_=pt[:, 0:n],
                                                                                func=AF.Copy), reads=[pb], writes=[ab])
                for ex in range(32):
                    pt, pb = pr.next()
                    S.op("pe", lambda e, pt=pt, ex=ex, o=o, n=n: e.matmul(
                        pt[:, 0:n], lhsT=sel[:, ex, :], rhs=gT[:, o:o + n], start=True, stop=True),
                        reads=[b_sel, b_gT], writes=[pb])
                    gbt, gbb = gbr.next()
                    S.op("act", lambda e, pt=pt, gbt=gbt, n=n: e.activation(out=gbt[:], in_=pt[:, 0:n], func=AF.Copy),
                         reads=[pb], writes=[gbb])
                    glt, glb = glr.next()
                    act_, actb = acr.next()
                    for wi in range(4):
                        wt, wb = wgr.next()
                        S.dma("sp" if wi % 2 == 0 else "act", lambda e, wt=wt, ex=ex, wi=wi: e.dma_start(
                            out=wt[:], in_=d["wgu"][ex].rearrange("(k p) f -> p k f", p=128)[:, :, wi * 256:(wi + 1) * 256]),
                            writes=[wb])
                        for c2 in range(2):
                            c = wi * 2 + c2
                            pt, pb = pr.next()
                            for k in range(32):
                                S.op("pe", lambda e, pt=pt, wt=wt, k=k, c2=c2, ht=ht, n=n: e.matmul(
                                    pt[:, 0:n], lhsT=wt[:, k, c2 * 128:(c2 + 1) * 128], rhs=ht[:, k, :],
                                    start=(k == 0), stop=(k == 31)), reads=[wb, hb], writes=[pb])
                            if c < 4:
                                S.op("dve", lambda e, pt=pt, glt=glt, c=c, ex=ex, n=n: e.tensor_scalar(
                                    out=glt[:, c, :], in0=pt[:, 0:n], scalar1=bgu[:, ex, c:c + 1], scalar2=7.0,
                                    op0=ALU.add, op1=ALU.min), reads=[pb, b_bgu], writes=[glb])
                                t_, tb_ = tmr.next()
                                S.op("act", lambda e, t_=t_, glt=glt, c=c: e.activation(
                                    out=t_[:], in_=glt[:, c, :], func=AF.Sigmoid, scale=1.702), reads=[glb], writes=[tb_])
                                S.op("pool", lambda e, t_=t_, glt=glt, c=c: e.tensor_tensor(
                                    out=glt[:, c, :], in0=glt[:, c, :], in1=t_[:], op=ALU.mult), reads=[tb_, glb], writes=[glb])
                            else:
                                t_, tb_ = tmr.next()
                                S.op("dve", lambda e, pt=pt, t_=t_, c=c, ex=ex, n=n: e.tensor_scalar(
                                    out=t_[:], in0=pt[:, 0:n], scalar1=bgu[:, ex, c:c + 1], scalar2=7.0,
                                    op0=ALU.add, op1=ALU.min), reads=[pb, b_bgu], writes=[tb_])
                                S.op("dve", lambda e, t_=t_: e.tensor_scalar(
                                    out=t_[:], in0=t_[:], scalar1=-7.0, scalar2=1.0, op0=ALU.max, op1=ALU.add),
                                    reads=[tb_], writes=[tb_])
                                S.op("pool", lambda e, t_=t_, glt=glt, c=c: e.tensor_tensor(
                                    out=t_[:], in0=t_[:], in1=glt[:, c - 4, :], op=ALU.mult), reads=[tb_, glb], writes=[tb_])
                                S.op("dve", lambda e, t_=t_, act_=act_, gbt=gbt, c=c: e.tensor_tensor(
                                    out=act_[:, c - 4, :], in0=t_[:], in1=gbt[:], op=ALU.mult),
                                    reads=[tb_, gbb], writes=[actb])
                    for wj in range(2):
                        wt, wb = wdr.next()
                        S.dma("sp" if wj % 2 == 0 else "act", lambda e, wt=wt, ex=ex, wj=wj: e.dma_start(
                            out=wt[:], in_=d["wdn"][ex].rearrange("(k p) f -> p k f", p=128)[:, :, wj * 2048:(wj + 1) * 2048]),
                            writes=[wb])
                        for dcl in range(16):
                            dc = wj * 16 + dcl
                            pt, pb = pr.next()
                            for k in range(4):
                                S.op("pe", lambda e, pt=pt, wt=wt, k=k, dcl=dcl, act_=act_, n=n: e.matmul(
                                    pt[:, 0:n], lhsT=wt[:, k, dcl * 128:(dcl + 1) * 128], rhs=act_[:, k, :],
                                    start=(k == 0), stop=(k == 3)), reads=[wb, actb], writes=[pb])
                            S.op("dve", lambda e, pt=pt, at=at, dc=dc, n=n: e.tensor_tensor(
                                out=at[:, dc, :], in0=pt[:, 0:n], in1=at[:, dc, :], op=ALU.add), reads=[pb, ab], writes=[ab])
                for dc in range(32):
                    xt, xb = xir.next()
                    S.dma("sp", lambda e, xt=xt, dc=dc, o=o, n=n: e.dma_start(out=xt[:], in_=x1v[dc][:, o:o + n]),
                          reads=[b_x1d], writes=[xb])
                    ot, ob = xor_.next()
                    segs = [(0, NC_CTX, 1), (NC_CTX, n, 0)] if o == 0 else [(0, n, 0)]
                    for (a, b, col) in segs:
                        S.op("dve", lambda e, at=at, ot=ot, xt=xt, a=a, b=b, col=col, dc=dc: e.scalar_tensor_tensor(
                            out=ot[:, a:b], in0=at[:, dc, a:b], scalar=modv[:, MB + 160 + dc, col:col + 1],
                            in1=xt[:, a:b], op0=ALU.mult, op1=ALU.add), reads=[ab, xb, b_modv], writes=[ob])
                    S.dma("sp", lambda e, ot=ot, dc=dc, o=o, n=n: e.dma_start(out=x2v[dc][:, o:o + n], in_=ot[:]),
                          reads=[ob], sembuf=ob)
            sched_barrier(S, scr)
        if last:
            with contextlib.ExitStack() as sf:
                fg = sf.enter_context(nc.sbuf_tensor("p3_fg", [128, 32], F32))
                b_fg = S.buf("p3fg")
                S.dma("sp", lambda e: e.dma_start(out=fg[:], in_=d["fing"]), writes=[b_fg])
                emit_final_norm(S, nc, sf, ps, d["x2T"], b_x2d, fg, b_fg, d["outT"], "p3f")
                sched_barrier(S, scr)
        sched_barrier(S, scr)


def emit_pw(S, nc, st, wgu_d, wdn_d, wgu_o, wdn_o):
    ring = Ring(S, nc, st, "pw_t", [128, 8, 1024], BF16, 6)
    gi = wgu_d.rearrange("e (k p) c -> p (e k) c", p=128)
    go = wgu_o.rearrange("e (k p) c -> p (e k) c", p=128)
    outs = []
    for r in range(32):
        t, b = ring.next()
        S.dma("pool", lambda e, t=t, r=r: e.dma_start(out=t[:], in_=gi[:, r * 8:(r + 1) * 8, :]), writes=[b])
        S.dma("sp", lambda e, t=t, r=r: e.dma_start(out=go[:, r * 8:(r + 1) * 8, :], in_=t[:]), reads=[b], sembuf=b)
    di = wdn_d.rearrange("e (k p) (h c) -> p (e k) h c", p=128, h=4)
    do = wdn_o.rearrange("e (k p) (h c) -> p (e k) h c", p=128, h=4)
    for r in range(16):
        t, b = ring.next()
        tv = t[:].rearrange("p (a h) c -> p a h c", h=4)
        S.dma("pool", lambda e, tv=tv, r=r: e.dma_start(out=tv, in_=di[:, r * 2:(r + 1) * 2, :, :]), writes=[b])
        S.dma("sp", lambda e, tv=tv, r=r: e.dma_start(out=do[:, r * 2:(r + 1) * 2, :, :], in_=tv), reads=[b], sembuf=b)
    return ring.b


GRID_W = 64
def fm(v):
    return np.ascontiguousarray(np.asarray(v, np.float32).reshape(-1, 128).T)

def rope_tables():
    t = np.arange(8192)
    row = (t // GRID_W).astype(np.float32); col = (t % GRID_W).astype(np.float32)
    inv = (np.float32(10000.0) ** (-np.arange(16, dtype=np.float32) / np.float32(16))).astype(np.float32)
    ang_r = (row[:, None] * inv).astype(np.float32); ang_c = (col[:, None] * inv).astype(np.float32)
    C = np.ones((128, 8448), np.float32); Sn = np.zeros((128, 8448), np.float32)
    for p in range(128):
        d = p % 64
        ang = ang_r if d < 32 else ang_c
        f = d % 16
        C[p, 256:] = np.cos(ang[:, f]); Sn[p, 256:] = np.sin(ang[:, f])
    return C, Sn

def rot_matrix_T():
    R = np.zeros((128, 128), np.float32)
    for m in range(128):
        if m % 32 < 16:
            R[m, m + 16] = -1.0
        else:
            R[m, m - 16] = 1.0
    return np.ascontiguousarray(R.T)

def na_bias_tables(rpb2):
    out = np.full((2, 8, 4, 128, 64), -30000.0, np.float32)
    qc = np.arange(64)
    c0 = np.clip(qc - 8, 0, 48)
    for case in range(8):
        for kr in range(8):
            if case < 4:
                row_rel = kr - case + 7
            elif case == 4:
                row_rel = kr + 3
            else:
                r = 125 + (case - 5)
                row_rel = 120 + kr - r + 7
            for kcol in range(64):
                valid = (kcol >= c0) & (kcol < c0 + 16)
                col_rel = kcol - qc + 15
                c, k128 = kr // 2, (kr % 2) * 64 + kcol
                for hl in range(2):
                    vals = rpb2[hl, row_rel, np.clip(col_rel, 0, 30)]
                    out[hl, case, c, k128, :] = np.where(valid, vals, np.float32(-30000.0))
    return np.ascontiguousarray(out.transpose(3, 0, 1, 2, 4))

def p2_consts(inp, l, j, ropeC, ropeS):
    gw = inp["lru_gate_w"][l][:, :, j]
    gb = inp["lru_gate_b"][l][:, :, j]
    return {
        "ropeC": ropeC, "ropeS": ropeS, "rmat": rot_matrix_T(), "ident": np.eye(128, dtype=np.float32),
        "lamqk": np.ascontiguousarray(np.broadcast_to(inp["lam_qk"][l].reshape(1, 256), (128, 256))).astype(np.float32),
        "subg": np.ascontiguousarray(inp["subln_g"][l].reshape(128, 1)).astype(np.float32),
        "convw": np.ascontiguousarray(inp["conv_w"][l][:, 128 * j:128 * j + 128].T).astype(np.float32),
        "convb": np.ascontiguousarray(inp["conv_b"][l][128 * j:128 * j + 128].reshape(128, 1)).astype(np.float32),
        "gatew": np.ascontiguousarray(gw.reshape(4, 128, 128).transpose(1, 0, 2)).astype(np.float32),
        "gateb": np.ascontiguousarray(gb.reshape(4, 128).T).astype(np.float32),
        "lrul": np.ascontiguousarray(inp["lru_lambda"][l][:, 128 * j:128 * j + 128].T).astype(np.float32),
        "nabt": na_bias_tables(np.asarray(inp["na_rpb"][l][2 * j:2 * j + 2], np.float32)),
    }

P2_CONST_SHAPES = {"ropeC": [128, 8448], "ropeS": [128, 8448], "rmat": [128, 128], "ident": [128, 128],
                   "lamqk": [128, 256], "subg": [128, 1], "convw": [128, 4], "convb": [128, 1],
                   "gatew": [128, 4, 128], "gateb": [128, 4], "lrul": [128, 2], "nabt": [128, 2, 8, 4, 64]}

GU_PERM = np.concatenate([np.arange(0, 1024, 2), np.arange(1, 1024, 2)])

def p3_consts(inp, l):
    wgu = np.ascontiguousarray(np.asarray(inp["w_gu"][l], np.float32)[:, :, GU_PERM])
    bgu = np.asarray(inp["b_gu"][l], np.float32)[:, GU_PERM].reshape(32, 8, 128).transpose(2, 0, 1)
    sel = np.zeros((32, 32, 128), np.float32)
    for e in range(32):
        sel[e, e, :] = 1.0
    return {
        "n2g": fm(inp["norm2_g"][l]), "wbr": np.ascontiguousarray(inp["w_branch"][l], dtype=np.float32),
        "wout": np.ascontiguousarray(inp["w_out"][l], dtype=np.float32),
        "wrt": np.ascontiguousarray(inp["w_router"][l], dtype=np.float32),
        "brt": np.ascontiguousarray(np.broadcast_to(np.asarray(inp["b_router"][l], np.float32).reshape(1, 32), (128, 32))),
        "wgu": wgu, "bgu": np.ascontiguousarray(bgu),
        "wdn": np.ascontiguousarray(inp["w_down"][l], dtype=np.float32),
        "bdn": np.ascontiguousarray(inp["b_down"][l], dtype=np.float32),
        "sel": sel, "ident": np.eye(128, dtype=np.float32), "fing": fm(inp["final_g"]),
    }

P3_IN_SHAPES = {"orecv": [8, 3, 128, 1056], "gsig": [96, 128, 1056], "xT": [4096, 1056], "modv": [128, 384, 2],
                "n2g": [128, 32], "wbr": [3, 1024, 4096], "wout": [4096, 4096], "wrt": [4096, 32], "brt": [128, 32],
                "wgu": [32, 4096, 1024], "bgu": [128, 32, 8], "wdn": [32, 512, 4096], "bdn": [32, 4096],
                "sel": [32, 32, 128], "ident": [128, 128], "fing": [128, 32]}
def _run(nc, in_maps):
    from concourse.bass_utils import run_bass_kernel_spmd
    return run_bass_kernel_spmd(nc, in_maps, core_ids=list(range(len(in_maps)))).results


def _build_p0():
    nc = bass.Bass("TRN2", target_bir_lowering=False)
    cT_d = nc.dram_tensor("cT", [128, 32, 2], F32, kind="ExternalInput").ap()
    wada_d = nc.dram_tensor("wada", [4096, 6144], F32, kind="ExternalInput").ap()
    bada_d = nc.dram_tensor("bada", [128, 48], F32, kind="ExternalInput").ap()
    modv_d = nc.dram_tensor("modv", [128, 48, 2], F32, kind="ExternalOutput").ap()
    S = Sched(nc)
    with contextlib.ExitStack() as st:
        ps = alloc_psum(nc, st)
        outs = emit_p0(S, nc, st, ps, cT_d, wada_d, bada_d, modv_d)
        S.wait_all("sp", outs)
        S.emit(st)
    return nc


def _build_p1(layer):
    nc = bass.Bass("TRN2", target_bir_lowering=False)
    xT_d = nc.dram_tensor("xT", [4096, NT], F32, kind="ExternalInput").ap()
    modv_d = nc.dram_tensor("modv", [128, 384, 2], F32, kind="ExternalInput").ap()
    n1g_d = nc.dram_tensor("n1g", [128, 32], F32, kind="ExternalInput").ap()
    win_d = nc.dram_tensor("win", [4096, 20480], F32, kind="ExternalInput").ap()
    zs_d = nc.dram_tensor("zsend", [8, 8, 128, NT], F32, kind="ExternalOutput").ap()
    gs_d = nc.dram_tensor("gsig", [96, 128, NT], F32, kind="ExternalOutput").ap()
    S = Sched(nc)
    with contextlib.ExitStack() as st:
        ps = alloc_psum(nc, st)
        outs = emit_p1(S, nc, st, ps, layer, xT_d, S.buf("xd"), modv_d, n1g_d, win_d, zs_d, None, gs_d, None)
        S.wait_all("sp", outs)
        S.emit(st)
    return nc


def _build_p2(lambda_init):
    nc = bass.Bass("TRN2", target_bir_lowering=False)
    zr_d = nc.dram_tensor("zr", [8, 8, 128, NT], F32, kind="ExternalInput").ap()
    os_d = nc.dram_tensor("os", [8, 3, 128, NT], F32, kind="ExternalOutput").ap()
    cd = {k: nc.dram_tensor(k, v, F32, kind="ExternalInput").ap() for k, v in P2_CONST_SHAPES.items()}
    S = Sched(nc)
    with contextlib.ExitStack() as st:
        ps = alloc_psum(nc, st)
        scr = st.enter_context(nc.sbuf_tensor("p2_scr", [128, 1], F32))
        b_zr = S.buf("zr")
        with contextlib.ExitStack() as sa:
            emit_p2a(S, nc, sa, ps, zr_d, b_zr, os_d, cd, lambda_init)
            sched_barrier(S, scr[:])
        with contextlib.ExitStack() as sb:
            emit_p2b(S, nc, sb, ps, zr_d, b_zr, os_d, cd)
            sched_barrier(S, scr[:])
        with contextlib.ExitStack() as sc:
            emit_p2c(S, nc, sc, ps, zr_d, b_zr, os_d, cd)
            sched_barrier(S, scr[:])
        S.emit(st)
    return nc


def _build_pw():
    nc = bass.Bass("TRN2", target_bir_lowering=False)
    a = nc.dram_tensor("wgu32", [8, 4096, 1024], F32, kind="ExternalInput").ap()
    b = nc.dram_tensor("wdn32", [8, 512, 4096], F32, kind="ExternalInput").ap()
    ao = nc.dram_tensor("wgub", [8, 4096, 1024], BF16, kind="ExternalOutput").ap()
    bo = nc.dram_tensor("wdnb", [8, 512, 4096], BF16, kind="ExternalOutput").ap()
    S = Sched(nc)
    with contextlib.ExitStack() as st:
        outs = emit_pw(S, nc, st, a, b, ao, bo)
        S.wait_all("sp", outs)
        S.emit(st)
    return nc


def _build_p3(layer, last):
    nc = bass.Bass("TRN2", target_bir_lowering=False)
    dd = {k: nc.dram_tensor(k, v, BF16 if k in ("wgu", "wdn") else F32, kind="ExternalInput").ap()
          for k, v in P3_IN_SHAPES.items()}
    dd["x1T"] = nc.dram_tensor("x1T", [4096, NT], F32, kind="Internal").ap()
    dd["h2d"] = nc.dram_tensor("h2d", [128, 32, NT], BF16, kind="Internal").ap()
    dd["x2T"] = nc.dram_tensor("x2T", [4096, NT], F32, kind="ExternalOutput").ap()
    if last:
        dd["outT"] = nc.dram_tensor("outT", [32, 128, 1024], F32, kind="ExternalOutput").ap()
    S = Sched(nc)
    with contextlib.ExitStack() as st:
        ps = alloc_psum(nc, st)
        scr = st.enter_context(nc.sbuf_tensor("p3_scr", [128, 1], F32))
        emit_p3(S, nc, ps, scr[:], layer, last, dd)
        S.emit(st)
    return nc


def kernel(x, c, ctx, c_ctx, w_ada, b_ada, norm1_g, norm2_g, w_in, lam_qk, subln_g, conv_w, conv_b,
           lru_gate_w, lru_gate_b, lru_lambda, na_rpb, w_branch, w_out, w_router, b_router,
           w_gu, b_gu, w_down, b_down, final_g):
    import math
    A = lambda v: np.asarray(v, dtype=np.float32)
    x = A(x); ctx = A(ctx)
    inp = dict(lam_qk=A(lam_qk), subln_g=A(subln_g), conv_w=A(conv_w), conv_b=A(conv_b), lru_gate_w=A(lru_gate_w),
               lru_gate_b=A(lru_gate_b), lru_lambda=A(lru_lambda), na_rpb=A(na_rpb), norm2_g=A(norm2_g),
               w_branch=A(w_branch), w_out=A(w_out), w_router=A(w_router), b_router=A(b_router), w_gu=A(w_gu),
               b_gu=A(b_gu), w_down=A(w_down), b_down=A(b_down), final_g=A(final_g))
    w_ada = A(w_ada); b_ada = A(b_ada); w_in = A(w_in); norm1_g = A(norm1_g)
    cT = np.stack([fm(A(c)[0]), fm(A(c_ctx))], axis=-1)
    ims = []
    for j in range(8):
        l, q = j // 4, j % 4
        ims.append({"cT": cT, "wada": np.ascontiguousarray(w_ada[l][:, q * 6144:(q + 1) * 6144]),
                    "bada": fm(b_ada[l][q * 6144:(q + 1) * 6144])})
    res = _run(_build_p0(), ims)
    modv = np.ascontiguousarray(np.concatenate([r["modv"] for r in res], axis=1))
    del ims
    xTs = []
    for i in range(8):
        xc = np.concatenate([ctx[0, 32 * i:32 * i + 32], x[0, 1024 * i:1024 * i + 1024]], 0)
        xTs.append(np.ascontiguousarray(xc.T))
    ropeC, ropeS = rope_tables()
    csts = [p3_consts(inp, l) for l in range(2)]
    ims = [{"wgu32": np.ascontiguousarray(np.concatenate([csts[0]["wgu"][4 * j:4 * j + 4], csts[1]["wgu"][4 * j:4 * j + 4]])),
            "wdn32": np.ascontiguousarray(np.concatenate([csts[0]["wdn"][4 * j:4 * j + 4], csts[1]["wdn"][4 * j:4 * j + 4]]))}
           for j in range(8)]
    res = _run(_build_pw(), ims)
    for l in range(2):
        csts[l]["wgu"] = np.ascontiguousarray(np.concatenate([res[j]["wgub"][4 * l:4 * l + 4] for j in range(8)]))
        csts[l]["wdn"] = np.ascontiguousarray(np.concatenate([res[j]["wdnb"][4 * l:4 * l + 4] for j in range(8)]))
    del ims, res
    outT = None
    for l in range(2):
        last = (l == 1)
        n1g = fm(norm1_g[l])
        ims = [{"xT": xTs[i], "modv": modv, "n1g": n1g, "win": w_in[l]} for i in range(8)]
        res = _run(_build_p1(l), ims)
        zs = [r["zsend"] for r in res]
        gs = [r["gsig"] for r in res]
        del ims, res
        lambda_init = 0.8 - 0.6 * math.exp(-0.3 * l)
        ims = []
        for j in range(8):
            im = {"zr": np.ascontiguousarray(np.stack([zs[s][j] for s in range(8)]))}
            im.update(p2_consts(inp, l, j, ropeC, ropeS))
            ims.append(im)
        del zs
        res = _run(_build_p2(lambda_init), ims)
        osr = [r["os"] for r in res]
        del ims, res
        cst = csts[l]
        ims = []
        for i in range(8):
            im = {"orecv": np.ascontiguousarray(np.stack([osr[j][i] for j in range(8)])), "gsig": gs[i],
                  "xT": xTs[i], "modv": modv}
            im.update(cst)
            ims.append(im)
        del osr, gs
        res = _run(_build_p3(l, last), ims)
        xTs = [r["x2T"] for r in res]
        if last:
            outT = [r["outT"] for r in res]
        del ims, res, cst
        csts[l] = None
    out = np.empty((1, 8192, 4096), np.float32)
    for i in range(8):
        out[0, 1024 * i:1024 * (i + 1), :] = outT[i].reshape(4096, 1024).T
    return out
```

```python
import contextlib
import numpy as np
import concourse.bass as bass
import concourse.mybir as mybir

F32 = mybir.dt.float32
BF16 = mybir.dt.bfloat16
I32 = mybir.dt.int32
U32 = mybir.dt.uint32
AF = mybir.ActivationFunctionType
ALU = mybir.AluOpType
AX = mybir.AxisListType

ENGS = ("pe", "act", "dve", "pool", "sp")


class Buf:
    __slots__ = ("name", "w", "r", "sem", "semval")

    def __init__(self, name):
        self.name = name
        self.w = None
        self.r = []
        self.sem = None
        self.semval = 0


class Sched:
    def __init__(self, nc):
        self.nc = nc
        self.stream = {e: [] for e in ENGS}
        self.seen = {e: {} for e in ENGS}
        self.ndma = 0
        self._allbufs = []

    def buf(self, name="b"):
        b = Buf(name)
        self._allbufs.append(b)
        return b

    def bufs(self, n, name="b"):
        return [self.buf(f"{name}{i}") for i in range(n)]

    def _need(self, eng, ev, waits, same_engine_ok):
        if ev is None:
            return
        kind = ev[0]
        if kind == "E":
            _, src, idx = ev
            if src == eng and same_engine_ok:
                return
            key = ("E", src)
            if self.seen[eng].get(key, -1) >= idx:
                return
            waits[key] = max(waits.get(key, -1), idx)
        else:
            _, semid, val = ev
            key = ("D", semid)
            if self.seen[eng].get(key, -1) >= val:
                return
            waits[key] = max(waits.get(key, -1), val)

    def _collect(self, eng, reads, writes):
        waits = {}
        for b in reads:
            self._need(eng, b.w, waits, False)
        for b in writes:
            self._need(eng, b.w, waits, True)
            for r in b.r:
                self._need(eng, r, waits, True)
        wl = []
        for key, v in waits.items():
            self.seen[eng][key] = v
            if key[0] == "E":
                self.stream[key[1]][v]["sig"] = True
            wl.append((key, v))
        return wl

    def op(self, eng, fn, reads=(), writes=()):
        wl = self._collect(eng, reads, writes)
        idx = len(self.stream[eng])
        self.stream[eng].append(dict(fn=fn, waits=wl, sig=False, dma=None))
        ev = ("E", eng, idx)
        for b in reads:
            b.r.append(ev)
        for b in writes:
            b.w = ev
            b.r = []
        return ev

    def dma(self, q, fn, reads=(), writes=(), sembuf=None):
        wl = self._collect(q, reads, writes)
        if sembuf is None:
            sembuf = writes[0] if writes else reads[0]
        if sembuf.sem is None:
            sembuf.sem = self.ndma
            self.ndma += 1
        sembuf.semval += 16
        ev = ("D", sembuf.sem, sembuf.semval)
        self.stream[q].append(dict(fn=fn, waits=wl, sig=False, dma=sembuf.sem))
        for b in reads:
            b.r.append(ev)
        for b in writes:
            b.w = ev
            b.r = []
        return ev

    def wait_all(self, eng, bufs):
        wl = self._collect(eng, [], bufs)
        self.stream[eng].append(dict(fn=None, waits=wl, sig=False, dma=None))

    def emit(self, stack):
        nc = self.nc
        esem = {e: stack.enter_context(nc.semaphore(f"es_{e}")) for e in ENGS}
        dsem = [stack.enter_context(nc.semaphore(f"ds_{i}")) for i in range(self.ndma)]
        vals = {}
        for e in ENGS:
            c = 0
            v = []
            for ent in self.stream[e]:
                if ent["sig"]:
                    c += 1
                v.append(c)
            vals[e] = v
        self.maxval = {e: (vals[e][-1] if vals[e] else 0) for e in ENGS}
        engobj = {"pe": "tensor", "act": "scalar", "dve": "vector", "pool": "gpsimd", "sp": "sync"}

        def replay(ename, eng):
            for ent in self.stream[ename]:
                for key, v in ent["waits"]:
                    if key[0] == "E":
                        eng.wait_ge(esem[key[1]], vals[key[1]][v])
                    else:
                        eng.wait_ge(dsem[key[1]], v)
                if ent["fn"] is None:
                    continue
                ins = ent["fn"](eng)
                if ent["dma"] is not None:
                    ins.then_inc(dsem[ent["dma"]], 16)
                elif ent["sig"]:
                    ins.then_inc(esem[ename], 1)

        import os
        if os.environ.get("MK_CLEAR", "0") == "1":
          with nc.Block() as blk0:
            @blk0.vector
            def _(v):
                for e in ENGS:
                    v.sem_clear(esem[e])
                for s in dsem:
                    v.sem_clear(s)
            del _
        with nc.Block() as blk:
            @blk.tensor
            def _(e):
                replay("pe", e)

            @blk.scalar
            def _(e):
                replay("act", e)

            @blk.vector
            def _(e):
                replay("dve", e)

            @blk.gpsimd
            def _(e):
                replay("pool", e)

            @blk.sync
            def _(e):
                replay("sp", e)


def sched_fence(S, eng, bufs, out, scratch_ap):
    S.wait_all(eng, bufs)
    S.op(eng, lambda e: e.memset(scratch_ap, 0.0), writes=[out])


def sched_barrier(S, scratch_ap):
    allb = list(S._allbufs)
    S.wait_all("dve", allb)
    bar = Buf("bar")
    S.op("dve", lambda e: e.memset(scratch_ap, 0.0), writes=[bar])
    for e in ENGS:
        if e != "dve":
            S.wait_all(e, [bar])
    S._allbufs.append(bar)


NT = 1056
NC_CTX = 32
D = 4096
KC = 32
TT = [(0, 352), (352, 352), (704, 352)]
EPS = 1e-6


class Ring:
    def __init__(self, S, nc, st, name, shape, dtype, n):
        self.t = [st.enter_context(nc.sbuf_tensor(f"{name}{i}", shape, dtype)) for i in range(n)]
        self.b = S.bufs(n, name)
        self.i = 0
        self.n = n

    def next(self):
        k = self.i % self.n
        self.i += 1
        return self.t[k], self.b[k]


class PsRing:
    def __init__(self, S, ps, idxs):
        self.t = [ps[i] for i in idxs]
        self.b = S.bufs(len(idxs), "psr")
        self.i = 0

    def next(self):
        k = self.i % len(self.t)
        self.i += 1
        return self.t[k], self.b[k]


def alloc_psum(nc, st):
    return [st.enter_context(nc.psum_tensor(f"psb{i}", [128, 512], F32)) for i in range(8)]


def emit_p0(S, nc, st, ps, cT_d, wada_d, bada_d, modv_d):
    cT = st.enter_context(nc.sbuf_tensor("p0_cT", [128, 32, 2], F32))
    sT = st.enter_context(nc.sbuf_tensor("p0_sT", [128, 32, 2], F32))
    bada = st.enter_context(nc.sbuf_tensor("p0_bada", [128, 48], F32))
    modv = st.enter_context(nc.sbuf_tensor("p0_modv", [128, 48, 2], F32))
    b_c, b_s, b_b, b_m = S.bufs(4, "p0")
    wr = Ring(S, nc, st, "p0_w", [128, 32, 512], F32, 2)
    pr = PsRing(S, ps, [0, 1, 2, 3])
    S.dma("sp", lambda e: e.dma_start(out=cT[:], in_=cT_d), writes=[b_c])
    S.dma("sp", lambda e: e.dma_start(out=bada[:], in_=bada_d), writes=[b_b])
    S.op("act", lambda e: e.activation(out=sT[:], in_=cT[:], func=AF.Silu), reads=[b_c], writes=[b_s])
    wv = wada_d.rearrange("(k p) f -> p k f", p=128)
    for g in range(12):
        wt, wb = wr.next()
        for h in range(2):
            S.dma("sp" if h == 0 else "act",
                  lambda e, wt=wt, g=g, h=h: e.dma_start(out=wt[:, h * 16:(h + 1) * 16, :],
                                                         in_=wv[:, h * 16:(h + 1) * 16, g * 512:(g + 1) * 512]),
                  writes=[wb])
        for c in range(4):
            pt, pb = pr.next()
            for k in range(32):
                S.op("pe", lambda e, pt=pt, wt=wt, c=c, k=k: e.matmul(
                    pt[:, 0:2], lhsT=wt[:, k, c * 128:(c + 1) * 128], rhs=sT[:, k, :],
                    start=(k == 0), stop=(k == 31)), reads=[wb, b_s], writes=[pb])
            ci = g * 4 + c
            S.op("dve", lambda e, pt=pt, ci=ci: e.tensor_scalar(
                out=modv[:, ci, :], in0=pt[:, 0:2], scalar1=bada[:, ci:ci + 1], scalar2=None, op0=ALU.add),
                reads=[pb, b_b], writes=[b_m])
    S.dma("sp", lambda e: e.dma_start(out=modv_d, in_=modv[:]), reads=[b_m], sembuf=b_m)
    return [b_m]


def emit_norm_mod(S, nc, st, ps, xT_d, b_xd, modv, b_modv, gvec, b_gvec, mod_base, hT, b_h, tag,
                  h32_cb=None):
    xr = Ring(S, nc, st, f"{tag}_x", [128, NT], F32, 3)
    sq = Ring(S, nc, st, f"{tag}_sq", [128, NT], F32, 2)
    tm = Ring(S, nc, st, f"{tag}_tm", [128, NT], F32, 2)
    ones = st.enter_context(nc.sbuf_tensor(f"{tag}_ones", [128, 128], F32))
    rstd = st.enter_context(nc.sbuf_tensor(f"{tag}_rstd", [128, NT], F32))
    Ab = st.enter_context(nc.sbuf_tensor(f"{tag}_A", [128, 32, 2], F32))
    b_ones, b_rstd, b_A = S.bufs(3, tag)
    S.op("pool", lambda e: e.memset(ones[:], 1.0), writes=[b_ones])
    S.op("dve", lambda e: e.tensor_scalar(out=Ab[:], in0=modv[:, mod_base + 32:mod_base + 64, :], scalar1=1.0,
                                          scalar2=None, op0=ALU.add), reads=[b_modv], writes=[b_A])
    for t in range(2):
        S.op("dve", lambda e, t=t: e.tensor_tensor(out=Ab[:, :, t], in0=Ab[:, :, t], in1=gvec[:], op=ALU.mult),
             reads=[b_A, b_gvec], writes=[b_A])
    xv = xT_d.rearrange("(k p) t -> k p t", p=128)
    pss = [(ps[i], S.buf(f"{tag}_pss{i}")) for i in range(3)]
    for k in range(32):
        xt, xb = xr.next()
        S.dma("sp", lambda e, xt=xt, k=k: e.dma_start(out=xt[:], in_=xv[k]), reads=[b_xd], writes=[xb])
        qt, qb = sq.next()
        S.op("act", lambda e, xt=xt, qt=qt: e.activation(out=qt[:], in_=xt[:], func=AF.Square),
             reads=[xb], writes=[qb])
        for i, (o, n) in enumerate(TT):
            S.op("pe", lambda e, i=i, o=o, n=n, qt=qt, k=k: e.matmul(
                pss[i][0][:, 0:n], lhsT=ones[:], rhs=qt[:, o:o + n], start=(k == 0), stop=(k == 31)),
                reads=[qb, b_ones], writes=[pss[i][1]])
    for i, (o, n) in enumerate(TT):
        S.op("dve", lambda e, i=i, o=o, n=n: e.tensor_scalar(
            out=rstd[:, o:o + n], in0=pss[i][0][:, 0:n], scalar1=1.0 / D, scalar2=EPS, op0=ALU.mult, op1=ALU.add),
            reads=[pss[i][1]], writes=[b_rstd])
    S.op("act", lambda e: e.activation(out=rstd[:], in_=rstd[:], func=AF.Sqrt), reads=[b_rstd], writes=[b_rstd])
    S.op("dve", lambda e: e.reciprocal(out=rstd[:], in_=rstd[:]), reads=[b_rstd], writes=[b_rstd])
    for k in range(32):
        xt, xb = xr.next()
        S.dma("sp", lambda e, xt=xt, k=k: e.dma_start(out=xt[:], in_=xv[k]), reads=[b_xd], writes=[xb])
        tt, tb = tm.next()
        S.op("dve", lambda e, xt=xt, tt=tt: e.tensor_tensor(out=tt[:], in0=xt[:], in1=rstd[:], op=ALU.mult),
             reads=[xb, b_rstd], writes=[tb])
        if h32_cb is None:
            S.op("act", lambda e, tt=tt, k=k: e.activation(
                out=hT[:, k, 0:NC_CTX], in_=tt[:, 0:NC_CTX], func=AF.Identity,
                scale=Ab[:, k, 1:2], bias=modv[:, mod_base + k, 1:2]), reads=[tb, b_A, b_modv], writes=[b_h])
            S.op("act", lambda e, tt=tt, k=k: e.activation(
                out=hT[:, k, NC_CTX:NT], in_=tt[:, NC_CTX:NT], func=AF.Identity,
                scale=Ab[:, k, 0:1], bias=modv[:, mod_base + k, 0:1]), reads=[tb, b_A, b_modv], writes=[b_h])
        else:
            S.op("act", lambda e, tt=tt, k=k: e.activation(
                out=tt[:, 0:NC_CTX], in_=tt[:, 0:NC_CTX], func=AF.Identity,
                scale=Ab[:, k, 1:2], bias=modv[:, mod_base + k, 1:2]), reads=[tb, b_A, b_modv], writes=[tb])
            S.op("act", lambda e, tt=tt, k=k: e.activation(
                out=tt[:, NC_CTX:NT], in_=tt[:, NC_CTX:NT], func=AF.Identity,
                scale=Ab[:, k, 0:1], bias=modv[:, mod_base + k, 0:1]), reads=[tb, b_A, b_modv], writes=[tb])
            S.op("dve", lambda e, tt=tt, k=k: e.tensor_copy(out=hT[:, k, :], in_=tt[:]), reads=[tb], writes=[b_h])
            h32_cb(k, tt, tb)


def emit_p1(S, nc, st, ps, layer, xT_d, b_xd, modv_d, n1g_d, win_d, zsend_d, b_zd, gsig_d, b_gd):
    modv = st.enter_context(nc.sbuf_tensor("p1_modv", [128, 384, 2], F32))
    gvec = st.enter_context(nc.sbuf_tensor("p1_g", [128, 32], F32))
    hT = st.enter_context(nc.sbuf_tensor("p1_hT", [128, 32, NT], BF16))
    b_modv, b_gvec, b_h = S.bufs(3, "p1")
    S.dma("sp", lambda e: e.dma_start(out=modv[:], in_=modv_d), writes=[b_modv])
    S.dma("sp", lambda e: e.dma_start(out=gvec[:], in_=n1g_d), writes=[b_gvec])
    emit_norm_mod(S, nc, st, ps, xT_d, b_xd, modv, b_modv, gvec, b_gvec, layer * 192 + 0, hT, b_h, "p1n")
    WCOL = 256
    wr = Ring(S, nc, st, "p1_w", [128, 32, WCOL], BF16, 3)
    osr = Ring(S, nc, st, "p1_os", [128, NT], F32, 3)
    pr = PsRing(S, ps, [0, 1, 2, 3, 4, 5])
    wv = win_d.rearrange("(k p) f -> p k f", p=128)
    nfc = 160
    for wi in range(nfc * 128 // WCOL):
        wt, wb = wr.next()
        S.dma("pool", lambda e, wt=wt, wi=wi: e.dma_start(out=wt[:], in_=wv[:, :, wi * WCOL:(wi + 1) * WCOL]),
              writes=[wb])
        for c in range(WCOL // 128):
            fc = wi * (WCOL // 128) + c
            ot, ob = osr.next()
            for i, (o, n) in enumerate(TT):
                pt, pb = pr.next()
                for k in range(32):
                    S.op("pe", lambda e, pt=pt, wt=wt, c=c, k=k, o=o, n=n: e.matmul(
                        pt[:, 0:n], lhsT=wt[:, k, c * 128:(c + 1) * 128], rhs=hT[:, k, o:o + n],
                        start=(k == 0), stop=(k == 31)), reads=[wb, b_h], writes=[pb])
                if fc < 64:
                    S.op("dve", lambda e, pt=pt, ot=ot, o=o, n=n: e.tensor_copy(out=ot[:, o:o + n], in_=pt[:, 0:n]),
                         reads=[pb], writes=[ob])
                else:
                    S.op("act", lambda e, pt=pt, ot=ot, o=o, n=n: e.activation(
                        out=ot[:, o:o + n], in_=pt[:, 0:n], func=AF.Sigmoid), reads=[pb], writes=[ob])
            if fc < 64:
                S.dma("sp", lambda e, ot=ot, fc=fc: e.dma_start(out=zsend_d[fc % 8, fc // 8], in_=ot[:]),
                      reads=[ob], sembuf=ob)
            else:
                S.dma("sp", lambda e, ot=ot, fc=fc: e.dma_start(out=gsig_d[fc - 64], in_=ot[:]),
                      reads=[ob], sembuf=ob)
    return osr.b


NG = 8448
import os as _os
_QW = int(_os.environ.get("MK_QW", "512"))
QT = [(0, 256)] + [(256 + _QW * m, _QW) for m in range(8192 // _QW)]


def g_load_pieces(zr_d, grp):
    out = []
    for s in range(8):
        out.append((32 * s, 32, zr_d[s, grp, :, 0:32]))
        out.append((256 + 1024 * s, 1024, zr_d[s, grp, :, 32:1056]))
    return out


def g_store_pieces(os_d, br, g0, n):
    out = []
    g = g0
    while g < g0 + n:
        if g < 256:
            dest, col, m = g // 32, g % 32, min(32 - g % 32, g0 + n - g)
        else:
            l = g - 256
            dest, col = l // 1024, 32 + l % 1024
            m = min(1024 - l % 1024, g0 + n - g)
        out.append((g - g0, m, os_d[dest, br, :, col:col + m]))
        g += m
    return out


def load_group(S, nc, q, zr_d, b_zr, grp, tile, buf):
    for (off, n, src) in g_load_pieces(zr_d, grp):
        S.dma(q, lambda e, off=off, n=n, src=src: e.dma_start(out=tile[:, off:off + n], in_=src),
              reads=[b_zr], writes=[buf])


def store_group(S, q, os_d, br, tile, buf, g0, n, t0=0):
    for (off, m, dst) in g_store_pieces(os_d, br, g0, n):
        S.dma(q, lambda e, off=off, m=m, dst=dst: e.dma_start(out=dst, in_=tile[:, t0 + off:t0 + off + m]),
              reads=[buf], sembuf=buf)


def build_vtok(S, nc, ps_ring, vs, b_vs, ident, b_id, vtok, b_vtok, shift, nchunk):
    c = 0
    while c < nchunk:
        nb = min(4, nchunk - c)
        pt, pb = ps_ring.next()
        for j in range(nb):
            S.op("pe", lambda e, pt=pt, j=j, c=c: e.transpose(
                out=pt[:, j * 128:(j + 1) * 128], in_=vs[:, shift + (c + j) * 128: shift + (c + j + 1) * 128],
                identity=ident[:]), reads=[b_vs, b_id], writes=[pb])
        eng = "dve" if (c // 4) % 2 == 0 else "act"
        if eng == "dve":
            S.op("dve", lambda e, pt=pt, c=c, nb=nb: e.tensor_copy(
                out=vtok[:, c:c + nb, :], in_=pt[:, 0:nb * 128].rearrange("p (c e) -> p c e", e=128)),
                reads=[pb], writes=[b_vtok])
        else:
            S.op("act", lambda e, pt=pt, c=c, nb=nb: e.activation(
                out=vtok[:, c:c + nb, :], in_=pt[:, 0:nb * 128].rearrange("p (c e) -> p c e", e=128),
                func=AF.Copy), reads=[pb], writes=[b_vtok])
        c += nb


def emit_p2a(S, nc, st, ps, zr_d, b_zr, os_d, consts, lambda_init):
    stg = Ring(S, nc, st, "a_stg", [128, NG], F32, 2)
    qTb = st.enter_context(nc.sbuf_tensor("a_q", [128, 2, NG], BF16))
    kTb = st.enter_context(nc.sbuf_tensor("a_k", [128, NG], BF16))
    vtok = st.enter_context(nc.sbuf_tensor("a_v", [128, 66, 128], BF16))
    rmat = st.enter_context(nc.sbuf_tensor("a_rmat", [128, 128], F32))
    ident = st.enter_context(nc.sbuf_tensor("a_ident", [128, 128], F32))
    ones_b = st.enter_context(nc.sbuf_tensor("a_onesb", [128, 128], BF16))
    ones_f = st.enter_context(nc.sbuf_tensor("a_onesf", [128, 128], F32))
    lamqk = st.enter_context(nc.sbuf_tensor("a_lamqk", [128, 256], F32))
    lamt = st.enter_context(nc.sbuf_tensor("a_lamt", [128, 128], F32))
    lamv = st.enter_context(nc.sbuf_tensor("a_lamv", [128, 4], F32))
    subg = st.enter_context(nc.sbuf_tensor("a_subg", [128, 1], F32))
    b_q, b_k, b_v, b_rm, b_id, b_ob, b_of, b_lq, b_lt, b_lv, b_sg = S.bufs(11, "a")
    S.dma("sp", lambda e: e.dma_start(out=rmat[:], in_=consts["rmat"]), writes=[b_rm])
    S.dma("sp", lambda e: e.dma_start(out=ident[:], in_=consts["ident"]), writes=[b_id])
    S.dma("sp", lambda e: e.dma_start(out=lamqk[:], in_=consts["lamqk"]), writes=[b_lq])
    S.dma("sp", lambda e: e.dma_start(out=subg[:], in_=consts["subg"]), writes=[b_sg])
    S.op("pool", lambda e: e.memset(ones_b[:], 1.0), writes=[b_ob])
    S.op("pool", lambda e: e.memset(ones_f[:], 1.0), writes=[b_of])
    S.op("pool", lambda e: e.memset(qTb[:, 0, :], 0.0), writes=[b_q])
    S.op("pool", lambda e: e.memset(qTb[:, 1, :], 0.0), writes=[b_q])
    S.op("dve", lambda e: e.tensor_tensor(out=lamt[:, 0:64], in0=lamqk[:, 0:64], in1=lamqk[:, 64:128], op=ALU.mult),
         reads=[b_lq], writes=[b_lt])
    S.op("dve", lambda e: e.tensor_tensor(out=lamt[:, 64:128], in0=lamqk[:, 128:192], in1=lamqk[:, 192:256],
                                          op=ALU.mult), reads=[b_lq], writes=[b_lt])
    S.op("dve", lambda e: e.tensor_reduce(out=lamv[:, 0:2], in_=lamt[:].rearrange("p (a b) -> p a b", b=64),
                                          axis=AX.X, op=ALU.add), reads=[b_lt], writes=[b_lv])
    S.op("act", lambda e: e.activation(out=lamv[:, 0:2], in_=lamv[:, 0:2], func=AF.Exp), reads=[b_lv], writes=[b_lv])
    S.op("dve", lambda e: e.tensor_tensor(out=lamv[:, 2:3], in0=lamv[:, 1:2], in1=lamv[:, 0:1], op=ALU.subtract),
         reads=[b_lv], writes=[b_lv])
    S.op("dve", lambda e: e.tensor_scalar(out=lamv[:, 3:4], in0=lamv[:, 2:3], scalar1=-float(lambda_init),
                                          scalar2=None, op0=ALU.add), reads=[b_lv], writes=[b_lv])
    S.op("dve", lambda e: e.tensor_scalar(out=subg[:], in0=subg[:], scalar1=float(1.0 - lambda_init), scalar2=None,
                                          op0=ALU.mult), reads=[b_sg], writes=[b_sg])
    pr = PsRing(S, ps, [0, 1, 2, 3, 4, 5, 6, 7])
    cs = Ring(S, nc, st, "a_cs", [128, 2, 352], F32, 3)
    t1r = Ring(S, nc, st, "a_t1", [128, 352], F32, 2)
    t2r = Ring(S, nc, st, "a_t2", [128, 352], F32, 2)
    for grp, dstT, b_dst in ((0, qTb, b_q), (1, kTb, b_k)):
        xs, xb = stg.next()
        load_group(S, nc, "sp", zr_d, b_zr, grp, xs, xb)
        for i in range(NG // 352):
            o = i * 352
            ct, cb = cs.next()
            S.dma("act", lambda e, ct=ct, o=o: e.dma_start(out=ct[:, 0, :], in_=consts["ropeC"][:, o:o + 352]),
                  writes=[cb])
            S.dma("act", lambda e, ct=ct, o=o: e.dma_start(out=ct[:, 1, :], in_=consts["ropeS"][:, o:o + 352]),
                  writes=[cb])
            pt, pb = pr.next()
            S.op("pe", lambda e, pt=pt, xs=xs, o=o: e.matmul(pt[:, 0:352], lhsT=rmat[:], rhs=xs[:, o:o + 352],
                                                            start=True, stop=True), reads=[xb, b_rm], writes=[pb])
            t1, t1b = t1r.next()
            t2, t2b = t2r.next()
            S.op("pool", lambda e, t1=t1, xs=xs, ct=ct, o=o: e.tensor_tensor(
                out=t1[:], in0=xs[:, o:o + 352], in1=ct[:, 0, :], op=ALU.mult), reads=[xb, cb], writes=[t1b])
            S.op("dve", lambda e, t2=t2, pt=pt, ct=ct: e.tensor_tensor(
                out=t2[:], in0=pt[:, 0:352], in1=ct[:, 1, :], op=ALU.mult), reads=[pb, cb], writes=[t2b])
            if grp == 0:
                for c in range(2):
                    S.op("dve", lambda e, t1=t1, t2=t2, o=o, c=c: e.tensor_tensor(
                        out=qTb[64 * c:64 * c + 64, c, o:o + 352], in0=t1[64 * c:64 * c + 64, :],
                        in1=t2[64 * c:64 * c + 64, :], op=ALU.add), reads=[t1b, t2b], writes=[b_dst])
            else:
                S.op("dve", lambda e, t1=t1, t2=t2, dstT=dstT, o=o: e.tensor_tensor(
                    out=dstT[:, o:o + 352], in0=t1[:], in1=t2[:], op=ALU.add), reads=[t1b, t2b], writes=[b_dst])
    import os as _os
    if _os.environ.get("MK_DEBUG") == "rope":
        outs = []
        for br, (srcT, b_src) in enumerate(((qTb, b_q), (kTb, b_k))):
            xs, xb = stg.next()
            S.op("dve", lambda e, xs=xs, srcT=srcT: e.tensor_copy(out=xs[:], in_=srcT[:]), reads=[b_src], writes=[xb])
            for i in range(8):
                S.dma("sp", lambda e, xs=xs, i=i, br=br: e.dma_start(out=os_d[i, br], in_=xs[:, 1056 * i:1056 * (i + 1)]),
                      reads=[xb], sembuf=xb)
            outs.append(xb)
        return outs
    xs, xb = stg.next()
    load_group(S, nc, "sp", zr_d, b_zr, 2, xs, xb)
    build_vtok(S, nc, pr, xs, xb, ident, b_id, vtok, b_v, 0, 66)
    scr = st.enter_context(nc.sbuf_tensor("a_scr", [128, 1], F32))
    sched_barrier(S, scr[:])
    pT = Ring(S, nc, st, "a_pT", [128, 512], BF16, 8)
    epi = Ring(S, nc, st, "a_epi", [128, 4, 512], F32, 2)
    ost = Ring(S, nc, st, "a_ost", [128, 512], F32, 2)
    ps_s = PsRing(S, ps, [0, 1, 4, 5, 7])
    ps_nd = [(ps[2], ps[3]), (ps[2], ps[3])]
    _bn, _bd = S.buf("n0"), S.buf("d0")
    b_nd = [(_bn, _bd), (_bn, _bd)]
    ps_ss, b_ss = ps[6], S.buf("ss")
    if _os.environ.get("MK_DEBUG") == "att64":
        dbg = st.enter_context(nc.sbuf_tensor("a_dbg", [128, 3, 512], F32))
        b_dbg = S.buf("dbg")
        sp_, sb_ = ps_s.next()
        S.op("pe", lambda e: e.matmul(sp_[:, 0:512], lhsT=kTb[0:64, 8192:8320], rhs=qTb[0:64, 256:768],
                                      start=True, stop=True), reads=[b_k, b_q], writes=[sb_])
        pt_, ptb = pT.next()
        S.op("act", lambda e: e.activation(out=pt_[:, 0:512], in_=sp_[:, 0:512], func=AF.Exp, scale=0.125),
             reads=[sb_], writes=[ptb])
        S.op("dve", lambda e: e.tensor_copy(out=dbg[:, 0, :], in_=pt_[:, 0:512]), reads=[ptb], writes=[b_dbg])
        S.op("dve", lambda e: e.tensor_copy(out=dbg[:, 1, 0:128], in_=vtok[:, 64, :]), reads=[b_v], writes=[b_dbg])
        S.op("dve", lambda e: e.tensor_copy(out=dbg[:, 1, 128:256], in_=vtok[:, 65, :]), reads=[b_v], writes=[b_dbg])
        S.op("dve", lambda e: e.tensor_copy(out=dbg[:, 2, 0:256], in_=kTb[:, 8192:8448]), reads=[b_k], writes=[b_dbg])
        for i in range(3):
            S.dma("sp", lambda e, i=i: e.dma_start(out=os_d[i, 0, :, 0:512], in_=dbg[:, i, :]), reads=[b_dbg], sembuf=b_dbg)
        return [b_dbg]
    HALF = int(_os.environ.get("MK_HALF", "66"))
    acc = Ring(S, nc, st, "a_acc", [128, 4, 512], F32, 2)
    for (q0, qn) in QT:
        nkc = 2 if q0 == 0 else int(_os.environ.get('MK_NKC', '66'))
        at, ab = acc.next()
        kc0 = 0 if q0 == 0 else int(_os.environ.get('MK_KC0', '0'))
        groups = [(g0, min(g0 + HALF, nkc)) for g0 in range(kc0, nkc, HALF)]
        steps = [(c, gi, g0, g1, kc) for c in range(2) for gi, (g0, g1) in enumerate(groups) for kc in range(g0, g1)]
        pending = []

        def emit_qk(step, q0=q0, qn=qn):
            c, gi, g0, g1, kc = step
            sp_, sb_ = ps_s.next()
            S.op("pe", lambda e, sp_=sp_, c=c, kc=kc, q0=q0, qn=qn: e.matmul(
                sp_[:, 0:qn], lhsT=kTb[:, kc * 128:(kc + 1) * 128],
                rhs=qTb[:, c, q0:q0 + qn], start=True, stop=True),
                reads=[b_k, b_q], writes=[sb_])
            pt_, ptb = pT.next()
            S.op("act", lambda e, sp_=sp_, pt_=pt_, qn=qn: e.activation(
                out=pt_[:, 0:qn], in_=sp_[:, 0:qn], func=AF.Exp, scale=0.125), reads=[sb_], writes=[ptb])
            return (step, pt_, ptb)

        def emit_pv(item, qn=qn, at=at, ab=ab):
            (c, gi, g0, g1, kc), pt_, ptb = item
            (pn, pd), (bn, bd) = ps_nd[c], b_nd[c]
            S.op("pe", lambda e, pn=pn, pt_=pt_, kc=kc, qn=qn, g0=g0, g1=g1: e.matmul(
                pn[:, 0:qn], lhsT=vtok[:, kc, :], rhs=pt_[:, 0:qn], start=(kc == g0), stop=(kc == g1 - 1)),
                reads=[b_v, ptb], writes=[bn])
            S.op("pe", lambda e, pd=pd, pt_=pt_, kc=kc, qn=qn, g0=g0, g1=g1: e.matmul(
                pd[:, 0:qn], lhsT=ones_b[:], rhs=pt_[:, 0:qn], start=(kc == g0), stop=(kc == g1 - 1)),
                reads=[b_ob, ptb], writes=[bd])
            if kc == g1 - 1:
                if gi == 0:
                    S.op("dve", lambda e, pn=pn, c=c: e.tensor_copy(out=at[:, c, 0:qn], in_=pn[:, 0:qn]),
                         reads=[bn], writes=[ab])
                    S.op("dve", lambda e, pd=pd, c=c: e.tensor_copy(out=at[:, 2 + c, 0:qn], in_=pd[:, 0:qn]),
                         reads=[bd], writes=[ab])
                else:
                    S.op("dve", lambda e, pn=pn, c=c: e.tensor_tensor(
                        out=at[:, c, 0:qn], in0=pn[:, 0:qn], in1=at[:, c, 0:qn], op=ALU.add), reads=[bn, ab], writes=[ab])
                    S.op("dve", lambda e, pd=pd, c=c: e.tensor_tensor(
                        out=at[:, 2 + c, 0:qn], in0=pd[:, 0:qn], in1=at[:, 2 + c, 0:qn], op=ALU.add),
                        reads=[bd, ab], writes=[ab])

        for step in steps:
            pending.append(emit_qk(step))
            if len(pending) > 4:
                emit_pv(pending.pop(0))
        while pending:
            emit_pv(pending.pop(0))
        et, eb = epi.next()
        for c in range(2):
            S.op("dve", lambda e, et=et, at=at, c=c, qn=qn: e.reciprocal(out=et[:, 2 + c, 0:qn], in_=at[:, 2 + c, 0:qn]),
                 reads=[ab], writes=[eb])
            S.op("dve", lambda e, et=et, at=at, c=c, qn=qn: e.tensor_tensor(
                out=et[:, c, 0:qn], in0=at[:, c, 0:qn], in1=et[:, 2 + c, 0:qn], op=ALU.mult), reads=[ab, eb], writes=[eb])
        S.op("dve", lambda e, et=et, qn=qn: e.scalar_tensor_tensor(
            out=et[:, 0, 0:qn], in0=et[:, 1, 0:qn], scalar=lamv[:, 3:4], in1=et[:, 0, 0:qn], op0=ALU.mult, op1=ALU.add),
            reads=[eb, b_lv], writes=[eb])
        S.op("act", lambda e, et=et, qn=qn: e.activation(out=et[:, 1, 0:qn], in_=et[:, 0, 0:qn], func=AF.Square),
             reads=[eb], writes=[eb])
        S.op("pe", lambda e, et=et, qn=qn: e.matmul(ps_ss[:, 0:qn], lhsT=ones_f[:], rhs=et[:, 1, 0:qn],
                                                    start=True, stop=True), reads=[eb, b_of], writes=[b_ss])
        S.op("dve", lambda e, et=et, qn=qn: e.tensor_scalar(
            out=et[:, 2, 0:qn], in0=ps_ss[:, 0:qn], scalar1=1.0 / 128, scalar2=EPS, op0=ALU.mult, op1=ALU.add),
            reads=[b_ss], writes=[eb])
        S.op("act", lambda e, et=et, qn=qn: e.activation(out=et[:, 2, 0:qn], in_=et[:, 2, 0:qn], func=AF.Sqrt),
             reads=[eb], writes=[eb])
        S.op("dve", lambda e, et=et, qn=qn: e.reciprocal(out=et[:, 2, 0:qn], in_=et[:, 2, 0:qn]),
             reads=[eb], writes=[eb])
        ot, ob = ost.next()
        S.op("dve", lambda e, et=et, ot=ot, qn=qn: e.scalar_tensor_tensor(
            out=ot[:, 0:qn], in0=et[:, 0, 0:qn], scalar=subg[:, 0:1], in1=et[:, 2, 0:qn], op0=ALU.mult, op1=ALU.mult),
            reads=[eb, b_sg], writes=[ob])
        store_group(S, "sp", os_d, 0, ot, ob, q0, qn)
    return ost.b


def emit_p2b(S, nc, st, ps, zr_d, b_zr, os_d, consts):
    xs = st.enter_context(nc.sbuf_tensor("b_xs", [128, NG], F32))
    u = st.enter_context(nc.sbuf_tensor("b_u", [128, NG], F32))
    aa = st.enter_context(nc.sbuf_tensor("b_a", [128, NG], F32))
    bb = st.enter_context(nc.sbuf_tensor("b_b", [128, NG], F32))
    hf = st.enter_context(nc.sbuf_tensor("b_hf", [128, NG], F32))
    convw = st.enter_context(nc.sbuf_tensor("b_cw", [128, 4], F32))
    convb = st.enter_context(nc.sbuf_tensor("b_cb", [128, 1], F32))
    gatew = st.enter_context(nc.sbuf_tensor("b_gw", [128, 4, 128], F32))
    gateb = st.enter_context(nc.sbuf_tensor("b_gb", [128, 4], F32))
    lrul = st.enter_context(nc.sbuf_tensor("b_ll", [128, 2], F32))
    cneg = st.enter_context(nc.sbuf_tensor("b_cn", [128, 2], F32))
    b_xs, b_u, b_a, b_b, b_hf, b_cw, b_cb, b_gw, b_gb, b_ll, b_cn = S.bufs(11, "bb")
    for t, d_, b_ in ((convw, "convw", b_cw), (convb, "convb", b_cb), (gatew, "gatew", b_gw), (gateb, "gateb", b_gb),
                      (lrul, "lrul", b_ll)):
        S.dma("sp", lambda e, t=t, d_=d_: e.dma_start(out=t[:], in_=consts[d_]), writes=[b_])
    S.op("act", lambda e: e.activation(out=cneg[:], in_=lrul[:], func=AF.Exp, scale=-1.0), reads=[b_ll], writes=[b_cn])
    S.op("act", lambda e: e.activation(out=cneg[:], in_=cneg[:], func=AF.Ln, bias=1.0), reads=[b_cn], writes=[b_cn])
    S.op("dve", lambda e: e.tensor_scalar(out=cneg[:], in0=cneg[:], scalar1=-8.0, scalar2=None, op0=ALU.mult),
         reads=[b_cn], writes=[b_cn])
    load_group(S, nc, "sp", zr_d, b_zr, 3, xs, b_xs)
    SEG = [(0, 256), (256, 8192)]
    for (o, n) in SEG:
        S.op("dve", lambda e, o=o, n=n: e.tensor_scalar(out=u[:, o:o + n], in0=xs[:, o:o + n], scalar1=convw[:, 2:3],
                                                        scalar2=convb[:, 0:1], op0=ALU.mult, op1=ALU.add),
             reads=[b_xs, b_cw, b_cb], writes=[b_u])
        for (j, sh) in ((0, -2), (1, -1), (3, 1)):
            if sh < 0:
                oo, io, m = o - sh, o, n + sh
            else:
                oo, io, m = o, o + sh, n - sh
            S.op("dve", lambda e, oo=oo, io=io, m=m, j=j: e.scalar_tensor_tensor(
                out=u[:, oo:oo + m], in0=xs[:, io:io + m], scalar=convw[:, j:j + 1], in1=u[:, oo:oo + m],
                op0=ALU.mult, op1=ALU.add), reads=[b_xs, b_cw, b_u], writes=[b_u])
    ri = Ring(S, nc, st, "b_ri", [128, 3, 512], F32, 3)
    pr = PsRing(S, ps, [0, 1, 2, 3])
    TL = [(0, 256)] + [(256 + 512 * m, 512) for m in range(16)]
    for d in range(2):
        for (o, n) in TL:
            rt, rb = ri.next()
            for g in range(2):
                pt, pb = pr.next()
                S.op("pe", lambda e, pt=pt, d=d, g=g, o=o, n=n: e.matmul(
                    pt[:, 0:n], lhsT=gatew[:, d * 2 + g, :], rhs=u[:, o:o + n], start=True, stop=True),
                    reads=[b_gw, b_u], writes=[pb])
                S.op("act", lambda e, pt=pt, rt=rt, d=d, g=g, n=n: e.activation(
                    out=rt[:, g, 0:n], in_=pt[:, 0:n], func=AF.Sigmoid, bias=gateb[:, d * 2 + g:d * 2 + g + 1]),
                    reads=[pb, b_gb], writes=[rb])
            S.op("act", lambda e, rt=rt, d=d, o=o, n=n: e.activation(
                out=aa[:, o:o + n], in_=rt[:, 0, 0:n], func=AF.Exp, scale=cneg[:, d:d + 1]),
                reads=[rb, b_cn], writes=[b_a])
            S.op("dve", lambda e, rt=rt, o=o, n=n: e.tensor_tensor(
                out=rt[:, 2, 0:n], in0=aa[:, o:o + n], in1=aa[:, o:o + n], op=ALU.mult), reads=[b_a], writes=[rb])
            S.op("dve", lambda e, rt=rt, n=n: e.tensor_scalar(
                out=rt[:, 2, 0:n], in0=rt[:, 2, 0:n], scalar1=-1.0, scalar2=1.0, op0=ALU.mult, op1=ALU.add),
                reads=[rb], writes=[rb])
            S.op("act", lambda e, rt=rt, n=n: e.activation(out=rt[:, 2, 0:n], in_=rt[:, 2, 0:n], func=AF.Sqrt),
                 reads=[rb], writes=[rb])
            S.op("pool", lambda e, rt=rt, o=o, n=n: e.tensor_tensor(
                out=rt[:, 1, 0:n], in0=rt[:, 1, 0:n], in1=u[:, o:o + n], op=ALU.mult), reads=[rb, b_u], writes=[rb])
            S.op("dve", lambda e, rt=rt, o=o, n=n: e.tensor_tensor(
                out=bb[:, o:o + n], in0=rt[:, 2, 0:n], in1=rt[:, 1, 0:n], op=ALU.mult), reads=[rb], writes=[b_b])
        if d == 0:
            S.op("dve", lambda e: e.tensor_tensor_scan(out=hf[:, 0:256], data0=aa[:, 0:256], data1=bb[:, 0:256],
                                                       initial=0.0, op0=ALU.mult, op1=ALU.add),
                 reads=[b_a, b_b], writes=[b_hf])
            S.op("dve", lambda e: e.tensor_tensor_scan(out=hf[:, 256:NG], data0=aa[:, 256:NG], data1=bb[:, 256:NG],
                                                       initial=hf[:, 255:256], op0=ALU.mult, op1=ALU.add),
                 reads=[b_a, b_b, b_hf], writes=[b_hf])
        else:
            S.op("dve", lambda e: e.tensor_tensor_scan(out=xs[:, 255::-1], data0=aa[:, 255::-1], data1=bb[:, 255::-1],
                                                       initial=0.0, op0=ALU.mult, op1=ALU.add),
                 reads=[b_a, b_b, b_u], writes=[b_xs])
            S.op("dve", lambda e: e.tensor_tensor_scan(out=xs[:, NG - 1:255:-1], data0=aa[:, NG - 1:255:-1],
                                                       data1=bb[:, NG - 1:255:-1], initial=xs[:, 0:1],
                                                       op0=ALU.mult, op1=ALU.add),
                 reads=[b_a, b_b, b_xs], writes=[b_xs])
    S.op("pool", lambda e: e.tensor_tensor(out=hf[:], in0=hf[:], in1=xs[:], op=ALU.add), reads=[b_hf, b_xs],
         writes=[b_hf])
    load_group(S, nc, "sp", zr_d, b_zr, 4, u, b_u)
    for (o, n) in TL:
        S.op("dve", lambda e, o=o, n=n: e.tensor_tensor(out=aa[:, o:o + n], in0=u[:, o:o + n], in1=u[:, o:o + n],
                                                        op=ALU.mult), reads=[b_u], writes=[b_a])
        S.op("dve", lambda e, o=o, n=n: e.tensor_scalar(out=aa[:, o:o + n], in0=aa[:, o:o + n], scalar1=0.044715,
                                                        scalar2=1.0, op0=ALU.mult, op1=ALU.add),
             reads=[b_a], writes=[b_a])
        S.op("pool", lambda e, o=o, n=n: e.tensor_tensor(out=aa[:, o:o + n], in0=aa[:, o:o + n], in1=u[:, o:o + n],
                                                         op=ALU.mult), reads=[b_a, b_u], writes=[b_a])
        S.op("act", lambda e, o=o, n=n: e.activation(out=aa[:, o:o + n], in_=aa[:, o:o + n], func=AF.Sigmoid,
                                                     scale=1.5957691216057308), reads=[b_a], writes=[b_a])
        S.op("pool", lambda e, o=o, n=n: e.tensor_tensor(out=aa[:, o:o + n], in0=aa[:, o:o + n], in1=u[:, o:o + n],
                                                         op=ALU.mult), reads=[b_a, b_u], writes=[b_a])
        S.op("dve", lambda e, o=o, n=n: e.tensor_tensor(out=bb[:, o:o + n], in0=aa[:, o:o + n], in1=hf[:, o:o + n],
                                                        op=ALU.mult), reads=[b_a, b_hf], writes=[b_b])
    store_group(S, "sp", os_d, 1, bb, b_b, 0, NG)
    return [b_b]


def na_case(r):
    if r < 4:
        return r, 0
    if r <= 124:
        return 4, r - 4
    return 5 + (r - 125), 120


def emit_p2c(S, nc, st, ps, zr_d, b_zr, os_d, consts):
    stg = Ring(S, nc, st, "c_stg", [128, NG], F32, 2)
    qb = st.enter_context(nc.sbuf_tensor("c_q", [128, 2, NG], BF16))
    kb = st.enter_context(nc.sbuf_tensor("c_k", [128, NG], BF16))
    vte = st.enter_context(nc.sbuf_tensor("c_ve", [128, 66, 128], BF16))
    vto = st.enter_context(nc.sbuf_tensor("c_vo", [128, 65, 128], BF16))
    oc = st.enter_context(nc.sbuf_tensor("c_o", [128, NG], F32))
    nabt = st.enter_context(nc.sbuf_tensor("c_bt", [128, 2, 8, 4, 64], F32))
    ident = st.enter_context(nc.sbuf_tensor("c_ident", [128, 128], F32))
    ones_b = st.enter_context(nc.sbuf_tensor("c_onesb", [128, 128], BF16))
    b_q, b_k, b_ve, b_vo, b_oc, b_bt, b_id, b_ob = S.bufs(8, "cc")
    S.dma("sp", lambda e: e.dma_start(out=nabt[:], in_=consts["nabt"]), writes=[b_bt])
    S.dma("sp", lambda e: e.dma_start(out=ident[:], in_=consts["ident"]), writes=[b_id])
    S.op("pool", lambda e: e.memset(ones_b[:], 1.0), writes=[b_ob])
    pr = PsRing(S, ps, [0, 1, 2, 3, 4, 5, 6, 7])
    S.op("pool", lambda e: e.memset(qb[:, 0, :], 0.0), writes=[b_q])
    S.op("pool", lambda e: e.memset(qb[:, 1, :], 0.0), writes=[b_q])
    for grp, dst, b_dst in ((5, qb, b_q), (6, kb, b_k)):
        xs, xb = stg.next()
        load_group(S, nc, "sp", zr_d, b_zr, grp, xs, xb)
        for i in range(4):
            o = i * (NG // 4)
            eng = "dve" if i % 2 == 0 else "pool"
            if grp == 5:
                for hl in range(2):
                    S.op(eng, lambda e, xs=xs, o=o, hl=hl: e.tensor_copy(
                        out=qb[64 * hl:64 * hl + 64, hl, o:o + NG // 4], in_=xs[64 * hl:64 * hl + 64, o:o + NG // 4]),
                        reads=[xb], writes=[b_q])
            else:
                S.op(eng, lambda e, xs=xs, dst=dst, o=o: e.tensor_copy(out=dst[:, o:o + NG // 4], in_=xs[:, o:o + NG // 4]),
                     reads=[xb], writes=[b_dst])
    xs, xb = stg.next()
    load_group(S, nc, "sp", zr_d, b_zr, 7, xs, xb)
    build_vtok(S, nc, pr, xs, xb, ident, b_id, vte, b_ve, 0, 66)
    build_vtok(S, nc, pr, xs, xb, ident, b_id, vto, b_vo, 64, 65)
    scr = st.enter_context(nc.sbuf_tensor("c_scr", [128, 1], F32))
    sched_barrier(S, scr[:])
    sbias = Ring(S, nc, st, "c_sb", [128, 256], F32, 3)
    pT = Ring(S, nc, st, "c_pT", [128, 384], BF16, 3)
    rc = Ring(S, nc, st, "c_rc", [128, 256], F32, 3)
    ps_s = PsRing(S, ps, [0, 1])
    ps_n = PsRing(S, ps, [2, 3])
    ps_d = PsRing(S, ps, [4, 5])
    for hl in range(2):
        P0 = 64 * hl
        sp_, sb_ = ps_s.next()
        pt_, ptb = pT.next()
        pn, bn = ps_n.next()
        pd, bd = ps_d.next()
        for kc in range(2):
            if kc == 1:
                sp_, sb_ = ps_s.next()
                pt_, ptb = pT.next()
            S.op("pe", lambda e, sp_=sp_, P0=P0, kc=kc, hl=hl: e.matmul(
                sp_[:, 0:256], lhsT=kb[:, kc * 128:(kc + 1) * 128], rhs=qb[:, hl, 0:256],
                start=True, stop=True), reads=[b_k, b_q], writes=[sb_])
            S.op("act", lambda e, sp_=sp_, pt_=pt_: e.activation(out=pt_[:, 0:256], in_=sp_[:, 0:256], func=AF.Exp,
                                                                 scale=0.125), reads=[sb_], writes=[ptb])
            S.op("pe", lambda e, pn=pn, pt_=pt_, kc=kc: e.matmul(pn[:, 0:256], lhsT=vte[:, kc, :], rhs=pt_[:, 0:256],
                                                                 start=(kc == 0), stop=(kc == 1)),
                 reads=[b_ve, ptb], writes=[bn])
            S.op("pe", lambda e, pd=pd, pt_=pt_, kc=kc: e.matmul(pd[:, 0:256], lhsT=ones_b[:], rhs=pt_[:, 0:256],
                                                                 start=(kc == 0), stop=(kc == 1)),
                 reads=[b_ob, ptb], writes=[bd])
        rt, rb = rc.next()
        S.op("dve", lambda e, rt=rt, pd=pd, P0=P0: e.reciprocal(out=rt[P0:P0 + 64, 0:256], in_=pd[P0:P0 + 64, 0:256]),
             reads=[bd], writes=[rb])
        S.op("dve", lambda e, rt=rt, pn=pn, P0=P0: e.tensor_tensor(
            out=oc[P0:P0 + 64, 0:256], in0=pn[P0:P0 + 64, 0:256], in1=rt[P0:P0 + 64, 0:256], op=ALU.mult),
            reads=[bn, rb], writes=[b_oc])
    for r in range(128):
        case, r0 = na_case(r)
        q0 = 256 + 64 * r
        if r0 % 2 == 0:
            vt, bv, cbase = vte, b_ve, (256 + 64 * r0) // 128
        else:
            vt, bv, cbase = vto, b_vo, (256 + 64 * r0 - 64) // 128
        k0 = 256 + 64 * r0
        for hl in range(2):
            P0 = 64 * hl
            sp_, sb_ = ps_s.next()
            for c in range(6):
                ks = k0 + 128 * c if c < 4 else 128 * (c - 4)
                S.op("pe", lambda e, sp_=sp_, P0=P0, ks=ks, c=c, q0=q0, hl=hl: e.matmul(
                    sp_[:, c * 64:(c + 1) * 64], lhsT=kb[:, ks:ks + 128], rhs=qb[:, hl, q0:q0 + 64],
                    start=True, stop=True), reads=[b_k, b_q], writes=[sb_])
            bt_, btb = sbias.next()
            S.op("dve", lambda e, sp_=sp_, bt_=bt_, hl=hl, case=case: e.scalar_tensor_tensor(
                out=bt_[:], in0=sp_[:, 0:256], scalar=0.125,
                in1=nabt[:, hl, case, :, :].rearrange("p c q -> p (c q)"), op0=ALU.mult, op1=ALU.add),
                reads=[sb_, b_bt], writes=[btb])
            pt_, ptb = pT.next()
            S.op("act", lambda e, pt_=pt_, bt_=bt_: e.activation(out=pt_[:, 0:256], in_=bt_[:], func=AF.Exp),
                 reads=[btb], writes=[ptb])
            S.op("act", lambda e, pt_=pt_, sp_=sp_: e.activation(out=pt_[:, 256:384], in_=sp_[:, 256:384], func=AF.Exp,
                                                                 scale=0.125), reads=[sb_], writes=[ptb])
            pn, bn = ps_n.next()
            pd, bd = ps_d.next()
            for c in range(6):
                if c < 4:
                    lhs, bl = vt[:, cbase + c, :], bv
                else:
                    lhs, bl = vte[:, c - 4, :], b_ve
                S.op("pe", lambda e, pn=pn, pt_=pt_, c=c, lhs=lhs: e.matmul(
                    pn[:, 0:64], lhsT=lhs, rhs=pt_[:, c * 64:(c + 1) * 64], start=(c == 0), stop=(c == 5)),
                    reads=[bl, ptb], writes=[bn])
                S.op("pe", lambda e, pd=pd, pt_=pt_, c=c: e.matmul(
                    pd[:, 0:64], lhsT=ones_b[:], rhs=pt_[:, c * 64:(c + 1) * 64], start=(c == 0), stop=(c == 5)),
                    reads=[b_ob, ptb], writes=[bd])
            rt, rb = rc.next()
            S.op("dve", lambda e, rt=rt, pd=pd, P0=P0: e.reciprocal(out=rt[P0:P0 + 64, 0:64], in_=pd[P0:P0 + 64, 0:64]),
                 reads=[bd], writes=[rb])
            S.op("dve", lambda e, rt=rt, pn=pn, P0=P0, q0=q0: e.tensor_tensor(
                out=oc[P0:P0 + 64, q0:q0 + 64], in0=pn[P0:P0 + 64, 0:64], in1=rt[P0:P0 + 64, 0:64], op=ALU.mult),
                reads=[bn, rb], writes=[b_oc])
    store_group(S, "sp", os_d, 2, oc, b_oc, 0, NG)
    return [b_oc]


def emit_final_norm(S, nc, st, ps, xT_d, b_xd, gvec, b_gvec, out_d, tag):
    xr = Ring(S, nc, st, f"{tag}_x", [128, NT], F32, 3)
    sq = Ring(S, nc, st, f"{tag}_sq", [128, NT], F32, 2)
    tm = Ring(S, nc, st, f"{tag}_tm", [128, NT], F32, 3)
    ones = st.enter_context(nc.sbuf_tensor(f"{tag}_ones", [128, 128], F32))
    rstd = st.enter_context(nc.sbuf_tensor(f"{tag}_rstd", [128, NT], F32))
    b_ones, b_rstd = S.bufs(2, tag)
    S.op("pool", lambda e: e.memset(ones[:], 1.0), writes=[b_ones])
    xv = xT_d.rearrange("(k p) t -> k p t", p=128)
    pss = [(ps[i], S.buf(f"{tag}_pss{i}")) for i in range(3)]
    for k in range(32):
        xt, xb = xr.next()
        S.dma("sp", lambda e, xt=xt, k=k: e.dma_start(out=xt[:], in_=xv[k]), reads=[b_xd], writes=[xb])
        qt, qb = sq.next()
        S.op("act", lambda e, xt=xt, qt=qt: e.activation(out=qt[:], in_=xt[:], func=AF.Square), reads=[xb], writes=[qb])
        for i, (o, n) in enumerate(TT):
            S.op("pe", lambda e, i=i, o=o, n=n, qt=qt, k=k: e.matmul(
                pss[i][0][:, 0:n], lhsT=ones[:], rhs=qt[:, o:o + n], start=(k == 0), stop=(k == 31)),
                reads=[qb, b_ones], writes=[pss[i][1]])
    for i, (o, n) in enumerate(TT):
        S.op("dve", lambda e, i=i, o=o, n=n: e.tensor_scalar(
            out=rstd[:, o:o + n], in0=pss[i][0][:, 0:n], scalar1=1.0 / D, scalar2=EPS, op0=ALU.mult, op1=ALU.add),
            reads=[pss[i][1]], writes=[b_rstd])
    S.op("act", lambda e: e.activation(out=rstd[:], in_=rstd[:], func=AF.Sqrt), reads=[b_rstd], writes=[b_rstd])
    S.op("dve", lambda e: e.reciprocal(out=rstd[:], in_=rstd[:]), reads=[b_rstd], writes=[b_rstd])
    for k in range(32):
        xt, xb = xr.next()
        S.dma("sp", lambda e, xt=xt, k=k: e.dma_start(out=xt[:], in_=xv[k]), reads=[b_xd], writes=[xb])
        tt, tb = tm.next()
        S.op("dve", lambda e, xt=xt, tt=tt, k=k: e.scalar_tensor_tensor(
            out=tt[:], in0=xt[:], scalar=gvec[:, k:k + 1], in1=rstd[:], op0=ALU.mult, op1=ALU.mult),
            reads=[xb, b_rstd, b_gvec], writes=[tb])
        S.dma("sp", lambda e, tt=tt, k=k: e.dma_start(out=out_d[k], in_=tt[:, NC_CTX:NT]), reads=[tb], sembuf=tb)
    return tm.b


def emit_p3(S, nc, ps, scr, layer, last, d):
    MB = layer * 192
    with contextlib.ExitStack() as st0:
        modv = st0.enter_context(nc.sbuf_tensor("p3_modv", [128, 384, 2], F32))
        b_modv = S.buf("p3modv")
        S.dma("sp", lambda e: e.dma_start(out=modv[:], in_=d["modv"]), writes=[b_modv])
        b_x1d, b_h2d, b_x2d = S.bufs(3, "p3d")
        with contextlib.ExitStack() as st:
            yT = st.enter_context(nc.sbuf_tensor("p3_yT", [128, 32, NT], BF16))
            b_y = S.buf("p3y")
            with contextlib.ExitStack() as sm:
                oT = sm.enter_context(nc.sbuf_tensor("p3_oT", [128, 3, 8, NT], BF16))
                b_o = S.buf("p3o")
                for j in range(8):
                    for br in range(3):
                        S.dma("pool", lambda e, j=j, br=br: e.dma_start(out=oT[:, br, j, :], in_=d["orecv"][j, br]),
                              writes=[b_o])
                wr = Ring(S, nc, sm, "p3_wbr", [128, 3, 8, 256], BF16, 2)
                gr = Ring(S, nc, sm, "p3_gs", [128, NT], F32, 4)
                ya = Ring(S, nc, sm, "p3_ya", [128, NT], F32, 2)
                tr = Ring(S, nc, sm, "p3_tm", [128, 352], F32, 3)
                pr = PsRing(S, ps, [0, 1, 2, 3, 4, 5])
                for wi in range(16):
                    wt, wb = wr.next()
                    for br in range(3):
                        S.dma("pool", lambda e, wt=wt, br=br, wi=wi: e.dma_start(
                            out=wt[:, br, :, :],
                            in_=d["wbr"][br].rearrange("(k p) f -> p k f", p=128)[:, :, wi * 256:(wi + 1) * 256]),
                            writes=[wb])
                    for c in range(2):
                        dc = wi * 2 + c
                        yt, yb = ya.next()
                        for br in range(3):
                            gt, gb = gr.next()
                            S.dma("sp", lambda e, gt=gt, br=br, dc=dc: e.dma_start(out=gt[:], in_=d["gsig"][br * 32 + dc]),
                                  writes=[gb])
                            for (o, n) in TT:
                                pt, pb = pr.next()
                                for k in range(8):
                                    S.op("pe", lambda e, pt=pt, wt=wt, br=br, k=k, c=c, o=o, n=n: e.matmul(
                                        pt[:, 0:n], lhsT=wt[:, br, k, c * 128:(c + 1) * 128], rhs=oT[:, br, k, o:o + n],
                                        start=(k == 0), stop=(k == 7)), reads=[wb, b_o], writes=[pb])
                                if br == 0:
                                    S.op("dve", lambda e, pt=pt, yt=yt, gt=gt, o=o, n=n: e.tensor_tensor(
                                        out=yt[:, o:o + n], in0=pt[:, 0:n], in1=gt[:, o:o + n], op=ALU.mult),
                                        reads=[pb, gb], writes=[yb])
                                else:
                                    t_, tb_ = tr.next()
                                    S.op("dve", lambda e, pt=pt, t_=t_, gt=gt, o=o, n=n: e.tensor_tensor(
                                        out=t_[:, 0:n], in0=pt[:, 0:n], in1=gt[:, o:o + n], op=ALU.mult),
                                        reads=[pb, gb], writes=[tb_])
                                    S.op("pool", lambda e, t_=t_, yt=yt, o=o, n=n: e.tensor_tensor(
                                        out=yt[:, o:o + n], in0=yt[:, o:o + n], in1=t_[:, 0:n], op=ALU.add),
                                        reads=[tb_, yb], writes=[yb])
                        S.op("act", lambda e, yt=yt, dc=dc: e.activation(out=yT[:, dc, :], in_=yt[:], func=AF.Copy),
                             reads=[yb], writes=[b_y])
                sched_barrier(S, scr)
            with contextlib.ExitStack() as so:
                wr = Ring(S, nc, so, "p3_wo", [128, 32, 256], BF16, 3)
                xr = Ring(S, nc, so, "p3_xi", [128, NT], F32, 3)
                xo = Ring(S, nc, so, "p3_xo", [128, NT], F32, 3)
                pr = PsRing(S, ps, [0, 1, 2, 3, 4, 5])
                wv = d["wout"].rearrange("(k p) f -> p k f", p=128)
                xv = d["xT"].rearrange("(k p) t -> k p t", p=128)
                x1v = d["x1T"].rearrange("(k p) t -> k p t", p=128)
                for wi in range(16):
                    wt, wb = wr.next()
                    S.dma("pool", lambda e, wt=wt, wi=wi: e.dma_start(out=wt[:], in_=wv[:, :, wi * 256:(wi + 1) * 256]),
                          writes=[wb])
                    for c in range(2):
                        dc = wi * 2 + c
                        xt, xb = xr.next()
                        S.dma("sp", lambda e, xt=xt, dc=dc: e.dma_start(out=xt[:], in_=xv[dc]), writes=[xb])
                        ot, ob = xo.next()
                        for (o, n) in TT:
                            pt, pb = pr.next()
                            for k in range(32):
                                S.op("pe", lambda e, pt=pt, wt=wt, k=k, c=c, o=o, n=n: e.matmul(
                                    pt[:, 0:n], lhsT=wt[:, k, c * 128:(c + 1) * 128], rhs=yT[:, k, o:o + n],
                                    start=(k == 0), stop=(k == 31)), reads=[wb, b_y], writes=[pb])
                            segs = [(0, NC_CTX, 1), (NC_CTX, n, 0)] if o == 0 else [(0, n, 0)]
                            for (a, b, col) in segs:
                                S.op("dve", lambda e, pt=pt, ot=ot, xt=xt, o=o, a=a, b=b, col=col, dc=dc: e.scalar_tensor_tensor(
                                    out=ot[:, o + a:o + b], in0=pt[:, a:b], scalar=modv[:, MB + 64 + dc, col:col + 1],
                                    in1=xt[:, o + a:o + b], op0=ALU.mult, op1=ALU.add),
                                    reads=[pb, xb, b_modv], writes=[ob])
                        S.dma("sp", lambda e, ot=ot, dc=dc: e.dma_start(out=x1v[dc], in_=ot[:]), reads=[ob], sembuf=ob)
                sched_barrier(S, scr)
        gT = st0.enter_context(nc.sbuf_tensor("p3_gT", [32, NT], F32))
        b_gT = S.buf("p3gT")
        with contextlib.ExitStack() as sn:
            gvec = sn.enter_context(nc.sbuf_tensor("p3_n2g", [128, 32], F32))
            h2T = sn.enter_context(nc.sbuf_tensor("p3_h2T", [128, 32, NT], BF16))
            wrt = sn.enter_context(nc.sbuf_tensor("p3_wrt", [128, 32, 32], BF16))
            brt = sn.enter_context(nc.sbuf_tensor("p3_brt", [128, 32], F32))
            ident = sn.enter_context(nc.sbuf_tensor("p3_ident", [128, 128], F32))
            b_gv, b_h2, b_wrt, b_brt, b_id = S.bufs(5, "p3n")
            S.dma("sp", lambda e: e.dma_start(out=gvec[:], in_=d["n2g"]), writes=[b_gv])
            S.dma("sp", lambda e: e.dma_start(out=brt[:], in_=d["brt"]), writes=[b_brt])
            S.dma("sp", lambda e: e.dma_start(out=ident[:], in_=d["ident"]), writes=[b_id])
            S.dma("pool", lambda e: e.dma_start(out=wrt[:], in_=d["wrt"].rearrange("(k p) f -> p k f", p=128)),
                  writes=[b_wrt])
            emit_norm_mod(S, nc, sn, ps, d["x1T"], b_x1d, modv, b_modv, gvec, b_gv, MB + 96, h2T, b_h2, "p3nm")
            S.dma("sp", lambda e: e.dma_start(out=d["h2d"], in_=h2T[:]), reads=[b_h2], sembuf=b_h2)
            lg = Ring(S, nc, sn, "p3_lg", [128, 3, 32], F32, 3)
            m8 = Ring(S, nc, sn, "p3_m8", [128, 12], F32, 3)
            pr = PsRing(S, ps, [3, 4, 5, 6])
            for i in range(9):
                t0 = i * 128
                nt_ = min(128, NT - t0)
                pt, pb = pr.next()
                for k in range(32):
                    S.op("pe", lambda e, pt=pt, k=k, t0=t0, nt_=nt_: e.matmul(
                        pt[0:nt_, 0:32], lhsT=h2T[:, k, t0:t0 + nt_], rhs=wrt[:, k, :], start=(k == 0), stop=(k == 31)),
                        reads=[b_h2, b_wrt], writes=[pb])
                lt, lb = lg.next()
                mt, mb = m8.next()
                S.op("dve", lambda e, pt=pt, lt=lt, nt_=nt_: e.tensor_tensor(
                    out=lt[0:nt_, 0, :], in0=pt[0:nt_, 0:32], in1=brt[0:nt_, :], op=ALU.add), reads=[pb, b_brt], writes=[lb])
                S.op("dve", lambda e, lt=lt, mt=mt, nt_=nt_: e.max(out=mt[0:nt_, 0:8], in_=lt[0:nt_, 0, :]),
                     reads=[lb], writes=[mb])
                S.op("dve", lambda e, mt=mt, nt_=nt_: e.tensor_scalar(
                    out=mt[0:nt_, 8:9], in0=mt[0:nt_, 0:1], scalar1=-1.0, scalar2=None, op0=ALU.mult),
                    reads=[mb], writes=[mb])
                S.op("act", lambda e, lt=lt, mt=mt, nt_=nt_: e.activation(
                    out=lt[0:nt_, 1, :], in_=lt[0:nt_, 0, :], func=AF.Exp, bias=mt[0:nt_, 8:9]), reads=[lb, mb], writes=[lb])
                S.op("dve", lambda e, lt=lt, mt=mt, nt_=nt_: e.tensor_scalar(
                    out=lt[0:nt_, 2, :], in0=lt[0:nt_, 0, :], scalar1=mt[0:nt_, 3:4], scalar2=None, op0=ALU.is_ge),
                    reads=[lb, mb], writes=[lb])
                S.op("dve", lambda e, lt=lt, nt_=nt_: e.tensor_tensor(
                    out=lt[0:nt_, 1, :], in0=lt[0:nt_, 1, :], in1=lt[0:nt_, 2, :], op=ALU.mult), reads=[lb], writes=[lb])
                S.op("dve", lambda e, lt=lt, mt=mt, nt_=nt_: e.tensor_reduce(
                    out=mt[0:nt_, 9:10], in_=lt[0:nt_, 1, :], axis=AX.X, op=ALU.add), reads=[lb], writes=[mb])
                S.op("dve", lambda e, mt=mt, nt_=nt_: e.reciprocal(out=mt[0:nt_, 10:11], in_=mt[0:nt_, 9:10]),
                     reads=[mb], writes=[mb])
                S.op("dve", lambda e, lt=lt, mt=mt, nt_=nt_: e.tensor_scalar(
                    out=lt[0:nt_, 2, :], in0=lt[0:nt_, 1, :], scalar1=mt[0:nt_, 10:11], scalar2=None, op0=ALU.mult),
                    reads=[lb, mb], writes=[lb])
                pt2, pb2 = pr.next()
                S.op("pe", lambda e, pt2=pt2, lt=lt, nt_=nt_: e.transpose(
                    out=pt2[0:32, 0:nt_], in_=lt[0:nt_, 2, :], identity=ident[0:nt_, 0:nt_]),
                    reads=[lb, b_id], writes=[pb2])
                S.op("act", lambda e, pt2=pt2, t0=t0, nt_=nt_: e.activation(
                    out=gT[:, t0:t0 + nt_], in_=pt2[0:32, 0:nt_], func=AF.Copy), reads=[pb2], writes=[b_gT])
            sched_barrier(S, scr)
        with contextlib.ExitStack() as se:
            sel = se.enter_context(nc.sbuf_tensor("p3_sel", [32, 32, 128], F32))
            bdn = se.enter_context(nc.sbuf_tensor("p3_bdn", [32, 4096], F32))
            bgu = se.enter_context(nc.sbuf_tensor("p3_bgu", [128, 32, 8], F32))
            b_sel, b_bdn, b_bgu = S.bufs(3, "p3e")
            S.dma("sp", lambda e: e.dma_start(out=sel[:], in_=d["sel"]), writes=[b_sel])
            S.dma("sp", lambda e: e.dma_start(out=bdn[:], in_=d["bdn"]), writes=[b_bdn])
            S.dma("sp", lambda e: e.dma_start(out=bgu[:], in_=d["bgu"]), writes=[b_bgu])
            h2r = Ring(S, nc, se, "p3_h2", [128, 32, 352], BF16, 1)
            accr = Ring(S, nc, se, "p3_acc", [128, 32, 352], F32, 1)
            wgr = Ring(S, nc, se, "p3_wg", [128, 32, 256], BF16, 2)
            wdr = Ring(S, nc, se, "p3_wd", [128, 4, 2048], BF16, 2)
            gbr = Ring(S, nc, se, "p3_gb", [128, 352], F32, 2)
            glr = Ring(S, nc, se, "p3_gl", [128, 4, 352], F32, 2)
            tmr = Ring(S, nc, se, "p3_t", [128, 352], F32, 3)
            acr = Ring(S, nc, se, "p3_ac", [128, 4, 352], BF16, 2)
            xir = Ring(S, nc, se, "p3_x1", [128, 352], F32, 3)
            xor_ = Ring(S, nc, se, "p3_x2", [128, 352], F32, 3)
            pr = PsRing(S, ps, [0, 1, 2, 3, 4, 5, 6, 7])
            x1v = d["x1T"].rearrange("(k p) t -> k p t", p=128)
            x2v = d["x2T"].rearrange("(k p) t -> k p t", p=128)
            for (o, n) in TT:
                ht, hb = h2r.next()
                S.dma("sp", lambda e, ht=ht, o=o, n=n: e.dma_start(out=ht[:], in_=d["h2d"][:, :, o:o + n]),
                      reads=[b_h2d], writes=[hb])
                at, ab = accr.next()
                for dc in range(32):
                    pt, pb = pr.next()
                    S.op("pe", lambda e, pt=pt, dc=dc, o=o, n=n: e.matmul(
                        pt[:, 0:n], lhsT=bdn[:, dc * 128:(dc + 1) * 128], rhs=gT[:, o:o + n], start=True, stop=True),
                        reads=[b_bdn, b_gT], writes=[pb])
                    S.op("act", lambda e, pt=pt, at=at, dc=dc, n=n: e.activation(out=at[:, dc, :], in_=pt[:, 0:n],
                                                                                func=AF.Copy), reads=[pb], writes=[ab])
                for ex in range(32):
                    pt, pb = pr.next()
                    S.op("pe", lambda e, pt=pt, ex=ex, o=o, n=n: e.matmul(
                        pt[:, 0:n], lhsT=sel[:, ex, :], rhs=gT[:, o:o + n], start=True, stop=True),
                        reads=[b_sel, b_gT], writes=[pb])
                    gbt, gbb = gbr.next()
                    S.op("act", lambda e, pt=pt, gbt=gbt, n=n: e.activation(out=gbt[:], in_=pt[:, 0:n], func=AF.Copy),
                         reads=[pb], writes=[gbb])
                    glt, glb = glr.next()
                    act_, actb = acr.next()
                    for wi in range(4):
                        wt, wb = wgr.next()
                        S.dma("sp" if wi % 2 == 0 else "act", lambda e, wt=wt, ex=ex, wi=wi: e.dma_start(
                            out=wt[:], in_=d["wgu"][ex, wi]),
                            writes=[wb])
                        for c2 in range(2):
                            c = wi * 2 + c2
                            pt, pb = pr.next()
                            for k in range(32):
                                S.op("pe", lambda e, pt=pt, wt=wt, k=k, c2=c2, ht=ht, n=n: e.matmul(
                                    pt[:, 0:n], lhsT=wt[:, k, c2 * 128:(c2 + 1) * 128], rhs=ht[:, k, :],
                                    start=(k == 0), stop=(k == 31)), reads=[wb, hb], writes=[pb])
                            if c < 4:
                                S.op("dve", lambda e, pt=pt, glt=glt, c=c, ex=ex, n=n: e.tensor_scalar(
                                    out=glt[:, c, :], in0=pt[:, 0:n], scalar1=bgu[:, ex, c:c + 1], scalar2=7.0,
                                    op0=ALU.add, op1=ALU.min), reads=[pb, b_bgu], writes=[glb])
                                t_, tb_ = tmr.next()
                                S.op("act", lambda e, t_=t_, glt=glt, c=c: e.activation(
                                    out=t_[:], in_=glt[:, c, :], func=AF.Sigmoid, scale=1.702), reads=[glb], writes=[tb_])
                                S.op("pool", lambda e, t_=t_, glt=glt, c=c: e.tensor_tensor(
                                    out=glt[:, c, :], in0=glt[:, c, :], in1=t_[:], op=ALU.mult), reads=[tb_, glb], writes=[glb])
                            else:
                                t_, tb_ = tmr.next()
                                S.op("dve", lambda e, pt=pt, t_=t_, c=c, ex=ex, n=n: e.tensor_scalar(
                                    out=t_[:], in0=pt[:, 0:n], scalar1=bgu[:, ex, c:c + 1], scalar2=7.0,
                                    op0=ALU.add, op1=ALU.min), reads=[pb, b_bgu], writes=[tb_])
                                S.op("dve", lambda e, t_=t_: e.tensor_scalar(
                                    out=t_[:], in0=t_[:], scalar1=-7.0, scalar2=1.0, op0=ALU.max, op1=ALU.add),
                                    reads=[tb_], writes=[tb_])
                                S.op("pool", lambda e, t_=t_, glt=glt, c=c: e.tensor_tensor(
                                    out=t_[:], in0=t_[:], in1=glt[:, c - 4, :], op=ALU.mult), reads=[tb_, glb], writes=[tb_])
                                S.op("dve", lambda e, t_=t_, act_=act_, gbt=gbt, c=c: e.tensor_tensor(
                                    out=act_[:, c - 4, :], in0=t_[:], in1=gbt[:], op=ALU.mult),
                                    reads=[tb_, gbb], writes=[actb])
                    for wj in range(2):
                        wt, wb = wdr.next()
                        S.dma("sp" if wj % 2 == 0 else "act", lambda e, wt=wt, ex=ex, wj=wj: e.dma_start(
                            out=wt[:], in_=d["wdn"][ex, wj]),
                            writes=[wb])
                        for dcl in range(16):
                            dc = wj * 16 + dcl
                            pt, pb = pr.next()
                            for k in range(4):
                                S.op("pe", lambda e, pt=pt, wt=wt, k=k, dcl=dcl, act_=act_, n=n: e.matmul(
                                    pt[:, 0:n], lhsT=wt[:, k, dcl * 128:(dcl + 1) * 128], rhs=act_[:, k, :],
                                    start=(k == 0), stop=(k == 3)), reads=[wb, actb], writes=[pb])
                            S.op("dve", lambda e, pt=pt, at=at, dc=dc, n=n: e.tensor_tensor(
                                out=at[:, dc, :], in0=pt[:, 0:n], in1=at[:, dc, :], op=ALU.add), reads=[pb, ab], writes=[ab])
                for dc in range(32):
                    xt, xb = xir.next()
                    S.dma("sp", lambda e, xt=xt, dc=dc, o=o, n=n: e.dma_start(out=xt[:], in_=x1v[dc][:, o:o + n]),
                          reads=[b_x1d], writes=[xb])
                    ot, ob = xor_.next()
                    segs = [(0, NC_CTX, 1), (NC_CTX, n, 0)] if o == 0 else [(0, n, 0)]
                    for (a, b, col) in segs:
                        S.op("dve", lambda e, at=at, ot=ot, xt=xt, a=a, b=b, col=col, dc=dc: e.scalar_tensor_tensor(
                            out=ot[:, a:b], in0=at[:, dc, a:b], scalar=modv[:, MB + 160 + dc, col:col + 1],
                            in1=xt[:, a:b], op0=ALU.mult, op1=ALU.add), reads=[ab, xb, b_modv], writes=[ob])
                    S.dma("sp", lambda e, ot=ot, dc=dc, o=o, n=n: e.dma_start(out=x2v[dc][:, o:o + n], in_=ot[:]),
                          reads=[ob], sembuf=ob)
            sched_barrier(S, scr)
        if last:
            with contextlib.ExitStack() as sf:
                fg = sf.enter_context(nc.sbuf_tensor("p3_fg", [128, 32], F32))
                b_fg = S.buf("p3fg")
                S.dma("sp", lambda e: e.dma_start(out=fg[:], in_=d["fing"]), writes=[b_fg])
                emit_final_norm(S, nc, sf, ps, d["x2T"], b_x2d, fg, b_fg, d["outT"], "p3f")
                sched_barrier(S, scr)
        sched_barrier(S, scr)


def emit_pw(S, nc, st, wgu_d, wdn_d, wgu_o, wdn_o):
    ring = Ring(S, nc, st, "pw_t", [128, 8, 1024], BF16, 6)
    gi = wgu_d.rearrange("e (k p) c -> p (e k) c", p=128)
    go = wgu_o.rearrange("e (k p) c -> p (e k) c", p=128)
    outs = []
    for r in range(32):
        t, b = ring.next()
        S.dma("pool", lambda e, t=t, r=r: e.dma_start(out=t[:], in_=gi[:, r * 8:(r + 1) * 8, :]), writes=[b])
        S.dma("sp", lambda e, t=t, r=r: e.dma_start(out=go[:, r * 8:(r + 1) * 8, :], in_=t[:]), reads=[b], sembuf=b)
    di = wdn_d.rearrange("e (k p) (h c) -> p (e k) h c", p=128, h=4)
    do = wdn_o.rearrange("e (k p) (h c) -> p (e k) h c", p=128, h=4)
    for r in range(16):
        t, b = ring.next()
        tv = t[:].rearrange("p (a h) c -> p a h c", h=4)
        S.dma("pool", lambda e, tv=tv, r=r: e.dma_start(out=tv, in_=di[:, r * 2:(r + 1) * 2, :, :]), writes=[b])
        S.dma("sp", lambda e, tv=tv, r=r: e.dma_start(out=do[:, r * 2:(r + 1) * 2, :, :], in_=tv), reads=[b], sembuf=b)
    return ring.b


GRID_W = 64
def fm(v):
    return np.ascontiguousarray(np.asarray(v, np.float32).reshape(-1, 128).T)

def rope_tables():
    t = np.arange(8192)
    row = (t // GRID_W).astype(np.float32); col = (t % GRID_W).astype(np.float32)
    inv = (np.float32(10000.0) ** (-np.arange(16, dtype=np.float32) / np.float32(16))).astype(np.float32)
    ang_r = (row[:, None] * inv).astype(np.float32); ang_c = (col[:, None] * inv).astype(np.float32)
    C = np.ones((128, 8448), np.float32); Sn = np.zeros((128, 8448), np.float32)
    for p in range(128):
        d = p % 64
        ang = ang_r if d < 32 else ang_c
        f = d % 16
        C[p, 256:] = np.cos(ang[:, f]); Sn[p, 256:] = np.sin(ang[:, f])
    return C, Sn

def rot_matrix_T():
    R = np.zeros((128, 128), np.float32)
    for m in range(128):
        if m % 32 < 16:
            R[m, m + 16] = -1.0
        else:
            R[m, m - 16] = 1.0
    return np.ascontiguousarray(R.T)

def na_bias_tables(rpb2):
    out = np.full((2, 8, 4, 128, 64), -30000.0, np.float32)
    qc = np.arange(64)
    c0 = np.clip(qc - 8, 0, 48)
    for case in range(8):
        for kr in range(8):
            if case < 4:
                row_rel = kr - case + 7
            elif case == 4:
                row_rel = kr + 3
            else:
                r = 125 + (case - 5)
                row_rel = 120 + kr - r + 7
            for kcol in range(64):
                valid = (kcol >= c0) & (kcol < c0 + 16)
                col_rel = kcol - qc + 15
                c, k128 = kr // 2, (kr % 2) * 64 + kcol
                for hl in range(2):
                    vals = rpb2[hl, row_rel, np.clip(col_rel, 0, 30)]
                    out[hl, case, c, k128, :] = np.where(valid, vals, np.float32(-30000.0))
    return np.ascontiguousarray(out.transpose(3, 0, 1, 2, 4))

def p2_consts(inp, l, j, ropeC, ropeS):
    gw = inp["lru_gate_w"][l][:, :, j]
    gb = inp["lru_gate_b"][l][:, :, j]
    return {
        "ropeC": ropeC, "ropeS": ropeS, "rmat": rot_matrix_T(), "ident": np.eye(128, dtype=np.float32),
        "lamqk": np.ascontiguousarray(np.broadcast_to(inp["lam_qk"][l].reshape(1, 256), (128, 256))).astype(np.float32),
        "subg": np.ascontiguousarray(inp["subln_g"][l].reshape(128, 1)).astype(np.float32),
        "convw": np.ascontiguousarray(inp["conv_w"][l][:, 128 * j:128 * j + 128].T).astype(np.float32),
        "convb": np.ascontiguousarray(inp["conv_b"][l][128 * j:128 * j + 128].reshape(128, 1)).astype(np.float32),
        "gatew": np.ascontiguousarray(gw.reshape(4, 128, 128).transpose(1, 0, 2)).astype(np.float32),
        "gateb": np.ascontiguousarray(gb.reshape(4, 128).T).astype(np.float32),
        "lrul": np.ascontiguousarray(inp["lru_lambda"][l][:, 128 * j:128 * j + 128].T).astype(np.float32),
        "nabt": na_bias_tables(np.asarray(inp["na_rpb"][l][2 * j:2 * j + 2], np.float32)),
    }

P2_CONST_SHAPES = {"ropeC": [128, 8448], "ropeS": [128, 8448], "rmat": [128, 128], "ident": [128, 128],
                   "lamqk": [128, 256], "subg": [128, 1], "convw": [128, 4], "convb": [128, 1],
                   "gatew": [128, 4, 128], "gateb": [128, 4], "lrul": [128, 2], "nabt": [128, 2, 8, 4, 64]}

GU_PERM = np.concatenate([np.arange(0, 1024, 2), np.arange(1, 1024, 2)])

def p3_consts(inp, l):
    wgu = np.ascontiguousarray(np.asarray(inp["w_gu"][l], np.float32)[:, :, GU_PERM])
    bgu = np.asarray(inp["b_gu"][l], np.float32)[:, GU_PERM].reshape(32, 8, 128).transpose(2, 0, 1)
    sel = np.zeros((32, 32, 128), np.float32)
    for e in range(32):
        sel[e, e, :] = 1.0
    return {
        "n2g": fm(inp["norm2_g"][l]), "wbr": np.ascontiguousarray(inp["w_branch"][l], dtype=np.float32),
        "wout": np.ascontiguousarray(inp["w_out"][l], dtype=np.float32),
        "wrt": np.ascontiguousarray(inp["w_router"][l], dtype=np.float32),
        "brt": np.ascontiguousarray(np.broadcast_to(np.asarray(inp["b_router"][l], np.float32).reshape(1, 32), (128, 32))),
        "wgu": wgu, "bgu": np.ascontiguousarray(bgu),
        "wdn": np.ascontiguousarray(inp["w_down"][l], dtype=np.float32),
        "bdn": np.ascontiguousarray(inp["b_down"][l], dtype=np.float32),
        "sel": sel, "ident": np.eye(128, dtype=np.float32), "fing": fm(inp["final_g"]),
    }

P3_IN_SHAPES = {"orecv": [8, 3, 128, 1056], "gsig": [96, 128, 1056], "xT": [4096, 1056], "modv": [128, 384, 2],
                "n2g": [128, 32], "wbr": [3, 1024, 4096], "wout": [4096, 4096], "wrt": [4096, 32], "brt": [128, 32],
                "wgu": [32, 4, 128, 32, 256], "bgu": [128, 32, 8], "wdn": [32, 2, 128, 4, 2048], "bdn": [32, 4096],
                "sel": [32, 32, 128], "ident": [128, 128], "fing": [128, 32]}


def tile_major_gu(w):
    return np.ascontiguousarray(w.reshape(32, 32, 128, 4, 256).transpose(0, 3, 2, 1, 4))

def tile_major_dn(w):
    return np.ascontiguousarray(w.reshape(32, 4, 128, 2, 2048).transpose(0, 3, 2, 1, 4))
def _run(nc, in_maps):
    from concourse.bass_utils import run_bass_kernel_spmd
    return run_bass_kernel_spmd(nc, in_maps, core_ids=list(range(len(in_maps)))).results


def _build_p0():
    nc = bass.Bass("TRN2", target_bir_lowering=False)
    cT_d = nc.dram_tensor("cT", [128, 32, 2], F32, kind="ExternalInput").ap()
    wada_d = nc.dram_tensor("wada", [4096, 6144], F32, kind="ExternalInput").ap()
    bada_d = nc.dram_tensor("bada", [128, 48], F32, kind="ExternalInput").ap()
    modv_d = nc.dram_tensor("modv", [128, 48, 2], F32, kind="ExternalOutput").ap()
    S = Sched(nc)
    with contextlib.ExitStack() as st:
        ps = alloc_psum(nc, st)
        outs = emit_p0(S, nc, st, ps, cT_d, wada_d, bada_d, modv_d)
        S.wait_all("sp", outs)
        S.emit(st)
    return nc


def _build_p1(layer):
    nc = bass.Bass("TRN2", target_bir_lowering=False)
    xT_d = nc.dram_tensor("xT", [4096, NT], F32, kind="ExternalInput").ap()
    modv_d = nc.dram_tensor("modv", [128, 384, 2], F32, kind="ExternalInput").ap()
    n1g_d = nc.dram_tensor("n1g", [128, 32], F32, kind="ExternalInput").ap()
    win_d = nc.dram_tensor("win", [4096, 20480], F32, kind="ExternalInput").ap()
    zs_d = nc.dram_tensor("zsend", [8, 8, 128, NT], F32, kind="ExternalOutput").ap()
    gs_d = nc.dram_tensor("gsig", [96, 128, NT], F32, kind="ExternalOutput").ap()
    S = Sched(nc)
    with contextlib.ExitStack() as st:
        ps = alloc_psum(nc, st)
        outs = emit_p1(S, nc, st, ps, layer, xT_d, S.buf("xd"), modv_d, n1g_d, win_d, zs_d, None, gs_d, None)
        S.wait_all("sp", outs)
        S.emit(st)
    return nc


def _build_p2(lambda_init):
    nc = bass.Bass("TRN2", target_bir_lowering=False)
    zr_d = nc.dram_tensor("zr", [8, 8, 128, NT], F32, kind="ExternalInput").ap()
    os_d = nc.dram_tensor("os", [8, 3, 128, NT], F32, kind="ExternalOutput").ap()
    cd = {k: nc.dram_tensor(k, v, F32, kind="ExternalInput").ap() for k, v in P2_CONST_SHAPES.items()}
    S = Sched(nc)
    with contextlib.ExitStack() as st:
        ps = alloc_psum(nc, st)
        scr = st.enter_context(nc.sbuf_tensor("p2_scr", [128, 1], F32))
        b_zr = S.buf("zr")
        with contextlib.ExitStack() as sa:
            emit_p2a(S, nc, sa, ps, zr_d, b_zr, os_d, cd, lambda_init)
            sched_barrier(S, scr[:])
        with contextlib.ExitStack() as sb:
            emit_p2b(S, nc, sb, ps, zr_d, b_zr, os_d, cd)
            sched_barrier(S, scr[:])
        with contextlib.ExitStack() as sc:
            emit_p2c(S, nc, sc, ps, zr_d, b_zr, os_d, cd)
            sched_barrier(S, scr[:])
        S.emit(st)
    return nc


def _build_pw():
    nc = bass.Bass("TRN2", target_bir_lowering=False)
    a = nc.dram_tensor("wgu32", [8, 4096, 1024], F32, kind="ExternalInput").ap()
    b = nc.dram_tensor("wdn32", [8, 512, 4096], F32, kind="ExternalInput").ap()
    ao = nc.dram_tensor("wgub", [8, 4096, 1024], BF16, kind="ExternalOutput").ap()
    bo = nc.dram_tensor("wdnb", [8, 512, 4096], BF16, kind="ExternalOutput").ap()
    S = Sched(nc)
    with contextlib.ExitStack() as st:
        outs = emit_pw(S, nc, st, a, b, ao, bo)
        S.wait_all("sp", outs)
        S.emit(st)
    return nc


def _build_p3(layer, last):
    nc = bass.Bass("TRN2", target_bir_lowering=False)
    dd = {k: nc.dram_tensor(k, v, BF16 if k in ("wgu", "wdn") else F32, kind="ExternalInput").ap()
          for k, v in P3_IN_SHAPES.items()}
    dd["x1T"] = nc.dram_tensor("x1T", [4096, NT], F32, kind="Internal").ap()
    dd["h2d"] = nc.dram_tensor("h2d", [128, 32, NT], BF16, kind="Internal").ap()
    dd["x2T"] = nc.dram_tensor("x2T", [4096, NT], F32, kind="ExternalOutput").ap()
    if last:
        dd["outT"] = nc.dram_tensor("outT", [32, 128, 1024], F32, kind="ExternalOutput").ap()
    S = Sched(nc)
    with contextlib.ExitStack() as st:
        ps = alloc_psum(nc, st)
        scr = st.enter_context(nc.sbuf_tensor("p3_scr", [128, 1], F32))
        emit_p3(S, nc, ps, scr[:], layer, last, dd)
        S.emit(st)
    return nc


def kernel(x, c, ctx, c_ctx, w_ada, b_ada, norm1_g, norm2_g, w_in, lam_qk, subln_g, conv_w, conv_b,
           lru_gate_w, lru_gate_b, lru_lambda, na_rpb, w_branch, w_out, w_router, b_router,
           w_gu, b_gu, w_down, b_down, final_g):
    import math
    A = lambda v: np.asarray(v, dtype=np.float32)
    x = A(x); ctx = A(ctx)
    inp = dict(lam_qk=A(lam_qk), subln_g=A(subln_g), conv_w=A(conv_w), conv_b=A(conv_b), lru_gate_w=A(lru_gate_w),
               lru_gate_b=A(lru_gate_b), lru_lambda=A(lru_lambda), na_rpb=A(na_rpb), norm2_g=A(norm2_g),
               w_branch=A(w_branch), w_out=A(w_out), w_router=A(w_router), b_router=A(b_router), w_gu=A(w_gu),
               b_gu=A(b_gu), w_down=A(w_down), b_down=A(b_down), final_g=A(final_g))
    w_ada = A(w_ada); b_ada = A(b_ada); w_in = A(w_in); norm1_g = A(norm1_g)
    cT = np.stack([fm(A(c)[0]), fm(A(c_ctx))], axis=-1)
    ims = []
    for j in range(8):
        l, q = j // 4, j % 4
        ims.append({"cT": cT, "wada": np.ascontiguousarray(w_ada[l][:, q * 6144:(q + 1) * 6144]),
                    "bada": fm(b_ada[l][q * 6144:(q + 1) * 6144])})
    res = _run(_build_p0(), ims)
    modv = np.ascontiguousarray(np.concatenate([r["modv"] for r in res], axis=1))
    del ims
    xTs = []
    for i in range(8):
        xc = np.concatenate([ctx[0, 32 * i:32 * i + 32], x[0, 1024 * i:1024 * i + 1024]], 0)
        xTs.append(np.ascontiguousarray(xc.T))
    ropeC, ropeS = rope_tables()
    csts = [p3_consts(inp, l) for l in range(2)]
    ims = [{"wgu32": np.ascontiguousarray(np.concatenate([csts[0]["wgu"][4 * j:4 * j + 4], csts[1]["wgu"][4 * j:4 * j + 4]])),
            "wdn32": np.ascontiguousarray(np.concatenate([csts[0]["wdn"][4 * j:4 * j + 4], csts[1]["wdn"][4 * j:4 * j + 4]]))}
           for j in range(8)]
    res = _run(_build_pw(), ims)
    for l in range(2):
        csts[l]["wgu"] = tile_major_gu(np.concatenate([res[j]["wgub"][4 * l:4 * l + 4] for j in range(8)]))
        csts[l]["wdn"] = tile_major_dn(np.concatenate([res[j]["wdnb"][4 * l:4 * l + 4] for j in range(8)]))
    del ims, res
    outT = None
    for l in range(2):
        last = (l == 1)
        n1g = fm(norm1_g[l])
        ims = [{"xT": xTs[i], "modv": modv, "n1g": n1g, "win": w_in[l]} for i in range(8)]
        res = _run(_build_p1(l), ims)
        zs = [r["zsend"] for r in res]
        gs = [r["gsig"] for r in res]
        del ims, res
        lambda_init = 0.8 - 0.6 * math.exp(-0.3 * l)
        ims = []
        for j in range(8):
            im = {"zr": np.ascontiguousarray(np.stack([zs[s][j] for s in range(8)]))}
            im.update(p2_consts(inp, l, j, ropeC, ropeS))
            ims.append(im)
        del zs
        res = _run(_build_p2(lambda_init), ims)
        osr = [r["os"] for r in res]
        del ims, res
        cst = csts[l]
        ims = []
        for i in range(8):
            im = {"orecv": np.ascontiguousarray(np.stack([osr[j][i] for j in range(8)])), "gsig": gs[i],
                  "xT": xTs[i], "modv": modv}
            im.update(cst)
            ims.append(im)
        del osr, gs
        res = _run(_build_p3(l, last), ims)
        xTs = [r["x2T"] for r in res]
        if last:
            outT = [r["outT"] for r in res]
        del ims, res, cst
        csts[l] = None
    out = np.empty((1, 8192, 4096), np.float32)
    for i in range(8):
        out[0, 1024 * i:1024 * (i + 1), :] = outT[i].reshape(4096, 1024).T
    return out
```

```python
import contextlib
import numpy as np
import concourse.bass as bass
import concourse.mybir as mybir

F32 = mybir.dt.float32
BF16 = mybir.dt.bfloat16
I32 = mybir.dt.int32
U32 = mybir.dt.uint32
AF = mybir.ActivationFunctionType
ALU = mybir.AluOpType
AX = mybir.AxisListType

ENGS = ("pe", "act", "dve", "pool", "sp")


class Buf:
    __slots__ = ("name", "w", "r", "sem", "semval")

    def __init__(self, name):
        self.name = name
        self.w = None
        self.r = []
        self.sem = None
        self.semval = 0


class Sched:
    def __init__(self, nc):
        self.nc = nc
        self.stream = {e: [] for e in ENGS}
        self.seen = {e: {} for e in ENGS}
        self.ndma = 0
        self._allbufs = []

    def buf(self, name="b"):
        b = Buf(name)
        self._allbufs.append(b)
        return b

    def bufs(self, n, name="b"):
        return [self.buf(f"{name}{i}") for i in range(n)]

    def _need(self, eng, ev, waits, same_engine_ok):
        if ev is None:
            return
        kind = ev[0]
        if kind == "E":
            _, src, idx = ev
            if src == eng and same_engine_ok:
                return
            key = ("E", src)
            if self.seen[eng].get(key, -1) >= idx:
                return
            waits[key] = max(waits.get(key, -1), idx)
        else:
            _, semid, val = ev
            key = ("D", semid)
            if self.seen[eng].get(key, -1) >= val:
                return
            waits[key] = max(waits.get(key, -1), val)

    def _collect(self, eng, reads, writes):
        waits = {}
        for b in reads:
            self._need(eng, b.w, waits, False)
        for b in writes:
            self._need(eng, b.w, waits, True)
            for r in b.r:
                self._need(eng, r, waits, True)
        wl = []
        for key, v in waits.items():
            self.seen[eng][key] = v
            if key[0] == "E":
                self.stream[key[1]][v]["sig"] = True
            wl.append((key, v))
        return wl

    def op(self, eng, fn, reads=(), writes=()):
        wl = self._collect(eng, reads, writes)
        idx = len(self.stream[eng])
        self.stream[eng].append(dict(fn=fn, waits=wl, sig=False, dma=None))
        ev = ("E", eng, idx)
        for b in reads:
            b.r.append(ev)
        for b in writes:
            b.w = ev
            b.r = []
        return ev

    def dma(self, q, fn, reads=(), writes=(), sembuf=None):
        wl = self._collect(q, reads, writes)
        if sembuf is None:
            sembuf = writes[0] if writes else reads[0]
        if sembuf.sem is None:
            sembuf.sem = self.ndma
            self.ndma += 1
        sembuf.semval += 16
        ev = ("D", sembuf.sem, sembuf.semval)
        self.stream[q].append(dict(fn=fn, waits=wl, sig=False, dma=sembuf.sem))
        for b in reads:
            b.r.append(ev)
        for b in writes:
            b.w = ev
            b.r = []
        return ev

    def wait_all(self, eng, bufs):
        wl = self._collect(eng, [], bufs)
        self.stream[eng].append(dict(fn=None, waits=wl, sig=False, dma=None))

    def emit(self, stack):
        nc = self.nc
        esem = {e: stack.enter_context(nc.semaphore(f"es_{e}")) for e in ENGS}
        dsem = [stack.enter_context(nc.semaphore(f"ds_{i}")) for i in range(self.ndma)]
        vals = {}
        for e in ENGS:
            c = 0
            v = []
            for ent in self.stream[e]:
                if ent["sig"]:
                    c += 1
                v.append(c)
            vals[e] = v
        self.maxval = {e: (vals[e][-1] if vals[e] else 0) for e in ENGS}
        engobj = {"pe": "tensor", "act": "scalar", "dve": "vector", "pool": "gpsimd", "sp": "sync"}

        def replay(ename, eng):
            for ent in self.stream[ename]:
                for key, v in ent["waits"]:
                    if key[0] == "E":
                        eng.wait_ge(esem[key[1]], vals[key[1]][v])
                    else:
                        eng.wait_ge(dsem[key[1]], v)
                if ent["fn"] is None:
                    continue
                ins = ent["fn"](eng)
                if ent["dma"] is not None:
                    ins.then_inc(dsem[ent["dma"]], 16)
                elif ent["sig"]:
                    ins.then_inc(esem[ename], 1)

        import os
        if os.environ.get("MK_CLEAR", "0") == "1":
          with nc.Block() as blk0:
            @blk0.vector
            def _(v):
                for e in ENGS:
                    v.sem_clear(esem[e])
                for s in dsem:
                    v.sem_clear(s)
            del _
        with nc.Block() as blk:
            @blk.tensor
            def _(e):
                replay("pe", e)

            @blk.scalar
            def _(e):
                replay("act", e)

            @blk.vector
            def _(e):
                replay("dve", e)

            @blk.gpsimd
            def _(e):
                replay("pool", e)

            @blk.sync
            def _(e):
                replay("sp", e)


def sched_fence(S, eng, bufs, out, scratch_ap):
    S.wait_all(eng, bufs)
    S.op(eng, lambda e: e.memset(scratch_ap, 0.0), writes=[out])


def sched_barrier(S, scratch_ap):
    allb = list(S._allbufs)
    S.wait_all("dve", allb)
    bar = Buf("bar")
    S.op("dve", lambda e: e.memset(scratch_ap, 0.0), writes=[bar])
    for e in ENGS:
        if e != "dve":
            S.wait_all(e, [bar])
    S._allbufs.append(bar)


NT = 1056
NC_CTX = 32
D = 4096
KC = 32
TT = [(0, 352), (352, 352), (704, 352)]
EPS = 1e-6


class Ring:
    def __init__(self, S, nc, st, name, shape, dtype, n):
        self.t = [st.enter_context(nc.sbuf_tensor(f"{name}{i}", shape, dtype)) for i in range(n)]
        self.b = S.bufs(n, name)
        self.i = 0
        self.n = n

    def next(self):
        k = self.i % self.n
        self.i += 1
        return self.t[k], self.b[k]


class PsRing:
    def __init__(self, S, ps, idxs):
        self.t = [ps[i] for i in idxs]
        self.b = S.bufs(len(idxs), "psr")
        self.i = 0

    def next(self):
        k = self.i % len(self.t)
        self.i += 1
        return self.t[k], self.b[k]


def alloc_psum(nc, st):
    return [st.enter_context(nc.psum_tensor(f"psb{i}", [128, 512], F32)) for i in range(8)]


def emit_p0(S, nc, st, ps, cT_d, wada_d, bada_d, modv_d):
    cT = st.enter_context(nc.sbuf_tensor("p0_cT", [128, 32, 2], F32))
    sT = st.enter_context(nc.sbuf_tensor("p0_sT", [128, 32, 2], F32))
    bada = st.enter_context(nc.sbuf_tensor("p0_bada", [2, 6144], F32))
    modv = st.enter_context(nc.sbuf_tensor("p0_modv", [2, 6144], F32))
    b_c, b_s, b_b, b_m = S.bufs(4, "p0")
    wr = Ring(S, nc, st, "p0_w", [128, 32, 512], F32, 2)
    pr = PsRing(S, ps, [0, 1, 2, 3])
    S.dma("sp", lambda e: e.dma_start(out=cT[:], in_=cT_d), writes=[b_c])
    S.dma("sp", lambda e: e.dma_start(out=bada[:], in_=bada_d), writes=[b_b])
    S.op("act", lambda e: e.activation(out=sT[:], in_=cT[:], func=AF.Silu), reads=[b_c], writes=[b_s])
    wv = wada_d.rearrange("(k p) f -> p k f", p=128)
    for g in range(12):
        wt, wb = wr.next()
        for h in range(2):
            S.dma("sp" if h == 0 else "act",
                  lambda e, wt=wt, g=g, h=h: e.dma_start(out=wt[:, h * 16:(h + 1) * 16, :],
                                                         in_=wv[:, h * 16:(h + 1) * 16, g * 512:(g + 1) * 512]),
                  writes=[wb])
        pt, pb = pr.next()
        for k in range(32):
            S.op("pe", lambda e, pt=pt, wt=wt, k=k: e.matmul(
                pt[0:2, 0:512], lhsT=sT[:, k, :], rhs=wt[:, k, :], start=(k == 0), stop=(k == 31)),
                reads=[wb, b_s], writes=[pb])
        S.op("dve", lambda e, pt=pt, g=g: e.tensor_tensor(
            out=modv[:, g * 512:(g + 1) * 512], in0=pt[0:2, 0:512], in1=bada[:, g * 512:(g + 1) * 512], op=ALU.add),
            reads=[pb, b_b], writes=[b_m])
    S.dma("sp", lambda e: e.dma_start(out=modv_d, in_=modv[:]), reads=[b_m], sembuf=b_m)
    return [b_m]


def emit_norm_mod(S, nc, st, ps, xT_d, b_xd, modv, b_modv, gvec, b_gvec, mod_base, hT, b_h, tag,
                  h32_cb=None):
    xr = Ring(S, nc, st, f"{tag}_x", [128, NT], F32, 3)
    sq = Ring(S, nc, st, f"{tag}_sq", [128, NT], F32, 2)
    tm = Ring(S, nc, st, f"{tag}_tm", [128, NT], F32, 2)
    ones = st.enter_context(nc.sbuf_tensor(f"{tag}_ones", [128, 128], F32))
    rstd = st.enter_context(nc.sbuf_tensor(f"{tag}_rstd", [128, NT], F32))
    Ab = st.enter_context(nc.sbuf_tensor(f"{tag}_A", [128, 32, 2], F32))
    b_ones, b_rstd, b_A = S.bufs(3, tag)
    S.op("pool", lambda e: e.memset(ones[:], 1.0), writes=[b_ones])
    S.op("dve", lambda e: e.tensor_scalar(out=Ab[:], in0=modv[:, mod_base + 32:mod_base + 64, :], scalar1=1.0,
                                          scalar2=None, op0=ALU.add), reads=[b_modv], writes=[b_A])
    for t in range(2):
        S.op("dve", lambda e, t=t: e.tensor_tensor(out=Ab[:, :, t], in0=Ab[:, :, t], in1=gvec[:], op=ALU.mult),
             reads=[b_A, b_gvec], writes=[b_A])
    xv = xT_d.rearrange("(k p) t -> k p t", p=128)
    pss = [(ps[i], S.buf(f"{tag}_pss{i}")) for i in range(3)]
    for k in range(32):
        xt, xb = xr.next()
        S.dma("sp", lambda e, xt=xt, k=k: e.dma_start(out=xt[:], in_=xv[k]), reads=[b_xd], writes=[xb])
        qt, qb = sq.next()
        S.op("act", lambda e, xt=xt, qt=qt: e.activation(out=qt[:], in_=xt[:], func=AF.Square),
             reads=[xb], writes=[qb])
        for i, (o, n) in enumerate(TT):
            S.op("pe", lambda e, i=i, o=o, n=n, qt=qt, k=k: e.matmul(
                pss[i][0][:, 0:n], lhsT=ones[:], rhs=qt[:, o:o + n], start=(k == 0), stop=(k == 31)),
                reads=[qb, b_ones], writes=[pss[i][1]])
    for i, (o, n) in enumerate(TT):
        S.op("dve", lambda e, i=i, o=o, n=n: e.tensor_scalar(
            out=rstd[:, o:o + n], in0=pss[i][0][:, 0:n], scalar1=1.0 / D, scalar2=EPS, op0=ALU.mult, op1=ALU.add),
            reads=[pss[i][1]], writes=[b_rstd])
    S.op("act", lambda e: e.activation(out=rstd[:], in_=rstd[:], func=AF.Sqrt), reads=[b_rstd], writes=[b_rstd])
    S.op("dve", lambda e: e.reciprocal(out=rstd[:], in_=rstd[:]), reads=[b_rstd], writes=[b_rstd])
    for k in range(32):
        xt, xb = xr.next()
        S.dma("sp", lambda e, xt=xt, k=k: e.dma_start(out=xt[:], in_=xv[k]), reads=[b_xd], writes=[xb])
        tt, tb = tm.next()
        S.op("dve", lambda e, xt=xt, tt=tt: e.tensor_tensor(out=tt[:], in0=xt[:], in1=rstd[:], op=ALU.mult),
             reads=[xb, b_rstd], writes=[tb])
        if h32_cb is None:
            S.op("act", lambda e, tt=tt, k=k: e.activation(
                out=hT[:, k, 0:NC_CTX], in_=tt[:, 0:NC_CTX], func=AF.Identity,
                scale=Ab[:, k, 1:2], bias=modv[:, mod_base + k, 1:2]), reads=[tb, b_A, b_modv], writes=[b_h])
            S.op("act", lambda e, tt=tt, k=k: e.activation(
                out=hT[:, k, NC_CTX:NT], in_=tt[:, NC_CTX:NT], func=AF.Identity,
                scale=Ab[:, k, 0:1], bias=modv[:, mod_base + k, 0:1]), reads=[tb, b_A, b_modv], writes=[b_h])
        else:
            S.op("act", lambda e, tt=tt, k=k: e.activation(
                out=tt[:, 0:NC_CTX], in_=tt[:, 0:NC_CTX], func=AF.Identity,
                scale=Ab[:, k, 1:2], bias=modv[:, mod_base + k, 1:2]), reads=[tb, b_A, b_modv], writes=[tb])
            S.op("act", lambda e, tt=tt, k=k: e.activation(
                out=tt[:, NC_CTX:NT], in_=tt[:, NC_CTX:NT], func=AF.Identity,
                scale=Ab[:, k, 0:1], bias=modv[:, mod_base + k, 0:1]), reads=[tb, b_A, b_modv], writes=[tb])
            S.op("dve", lambda e, tt=tt, k=k: e.tensor_copy(out=hT[:, k, :], in_=tt[:]), reads=[tb], writes=[b_h])
            h32_cb(k, tt, tb)


def emit_p1(S, nc, st, ps, layer, xT_d, b_xd, modv_d, n1g_d, win_d, zsend_d, b_zd, gsig_d, b_gd):
    modv = st.enter_context(nc.sbuf_tensor("p1_modv", [128, 384, 2], F32))
    gvec = st.enter_context(nc.sbuf_tensor("p1_g", [128, 32], F32))
    hT = st.enter_context(nc.sbuf_tensor("p1_hT", [128, 32, NT], BF16))
    b_modv, b_gvec, b_h = S.bufs(3, "p1")
    S.dma("sp", lambda e: e.dma_start(out=modv[:], in_=modv_d), writes=[b_modv])
    S.dma("sp", lambda e: e.dma_start(out=gvec[:], in_=n1g_d), writes=[b_gvec])
    emit_norm_mod(S, nc, st, ps, xT_d, b_xd, modv, b_modv, gvec, b_gvec, layer * 192 + 0, hT, b_h, "p1n")
    WCOL = 256
    wr = Ring(S, nc, st, "p1_w", [128, 32, WCOL], BF16, 3)
    osr = Ring(S, nc, st, "p1_os", [128, NT], F32, 3)
    pr = PsRing(S, ps, [0, 1, 2, 3, 4, 5])
    wv = win_d.rearrange("(k p) f -> p k f", p=128)
    nfc = 160
    for wi in range(nfc * 128 // WCOL):
        wt, wb = wr.next()
        S.dma("pool", lambda e, wt=wt, wi=wi: e.dma_start(out=wt[:], in_=wv[:, :, wi * WCOL:(wi + 1) * WCOL]),
              writes=[wb])
        for c in range(WCOL // 128):
            fc = wi * (WCOL // 128) + c
            ot, ob = osr.next()
            for i, (o, n) in enumerate(TT):
                pt, pb = pr.next()
                for k in range(32):
                    S.op("pe", lambda e, pt=pt, wt=wt, c=c, k=k, o=o, n=n: e.matmul(
                        pt[:, 0:n], lhsT=wt[:, k, c * 128:(c + 1) * 128], rhs=hT[:, k, o:o + n],
                        start=(k == 0), stop=(k == 31)), reads=[wb, b_h], writes=[pb])
                if fc < 64:
                    S.op("dve", lambda e, pt=pt, ot=ot, o=o, n=n: e.tensor_copy(out=ot[:, o:o + n], in_=pt[:, 0:n]),
                         reads=[pb], writes=[ob])
                else:
                    S.op("act", lambda e, pt=pt, ot=ot, o=o, n=n: e.activation(
                        out=ot[:, o:o + n], in_=pt[:, 0:n], func=AF.Sigmoid), reads=[pb], writes=[ob])
            if fc < 64:
                S.dma("sp", lambda e, ot=ot, fc=fc: e.dma_start(out=zsend_d[fc % 8, fc // 8], in_=ot[:]),
                      reads=[ob], sembuf=ob)
            else:
                S.dma("sp", lambda e, ot=ot, fc=fc: e.dma_start(out=gsig_d[fc - 64], in_=ot[:]),
                      reads=[ob], sembuf=ob)
    return osr.b


NG = 8448
import os as _os
_QW = int(_os.environ.get("MK_QW", "512"))
QT = [(0, 256)] + [(256 + _QW * m, _QW) for m in range(8192 // _QW)]


def g_load_pieces(zr_d, grp):
    out = []
    for s in range(8):
        out.append((32 * s, 32, zr_d[s, grp, :, 0:32]))
        out.append((256 + 1024 * s, 1024, zr_d[s, grp, :, 32:1056]))
    return out


def g_store_pieces(os_d, br, g0, n):
    out = []
    g = g0
    while g < g0 + n:
        if g < 256:
            dest, col, m = g // 32, g % 32, min(32 - g % 32, g0 + n - g)
        else:
            l = g - 256
            dest, col = l // 1024, 32 + l % 1024
            m = min(1024 - l % 1024, g0 + n - g)
        out.append((g - g0, m, os_d[dest, br, :, col:col + m]))
        g += m
    return out


def load_group(S, nc, q, zr_d, b_zr, grp, tile, buf):
    for (off, n, src) in g_load_pieces(zr_d, grp):
        S.dma(q, lambda e, off=off, n=n, src=src: e.dma_start(out=tile[:, off:off + n], in_=src),
              reads=[b_zr], writes=[buf])


def store_group(S, q, os_d, br, tile, buf, g0, n, t0=0):
    for (off, m, dst) in g_store_pieces(os_d, br, g0, n):
        S.dma(q, lambda e, off=off, m=m, dst=dst: e.dma_start(out=dst, in_=tile[:, t0 + off:t0 + off + m]),
              reads=[buf], sembuf=buf)


def build_vtok(S, nc, ps_ring, vs, b_vs, ident, b_id, vtok, b_vtok, shift, nchunk):
    c = 0
    while c < nchunk:
        nb = min(4, nchunk - c)
        pt, pb = ps_ring.next()
        for j in range(nb):
            S.op("pe", lambda e, pt=pt, j=j, c=c: e.transpose(
                out=pt[:, j * 128:(j + 1) * 128], in_=vs[:, shift + (c + j) * 128: shift + (c + j + 1) * 128],
                identity=ident[:]), reads=[b_vs, b_id], writes=[pb])
        eng = "dve" if (c // 4) % 2 == 0 else "act"
        if eng == "dve":
            S.op("dve", lambda e, pt=pt, c=c, nb=nb: e.tensor_copy(
                out=vtok[:, c:c + nb, :], in_=pt[:, 0:nb * 128].rearrange("p (c e) -> p c e", e=128)),
                reads=[pb], writes=[b_vtok])
        else:
            S.op("act", lambda e, pt=pt, c=c, nb=nb: e.activation(
                out=vtok[:, c:c + nb, :], in_=pt[:, 0:nb * 128].rearrange("p (c e) -> p c e", e=128),
                func=AF.Copy), reads=[pb], writes=[b_vtok])
        c += nb


def emit_p2a(S, nc, st, ps, zr_d, b_zr, os_d, consts, lambda_init):
    stg = Ring(S, nc, st, "a_stg", [128, NG], F32, 2)
    qTb = st.enter_context(nc.sbuf_tensor("a_q", [128, 2, NG], BF16))
    kTb = st.enter_context(nc.sbuf_tensor("a_k", [128, NG], BF16))
    vtok = st.enter_context(nc.sbuf_tensor("a_v", [128, 66, 128], BF16))
    rmat = st.enter_context(nc.sbuf_tensor("a_rmat", [128, 128], F32))
    ident = st.enter_context(nc.sbuf_tensor("a_ident", [128, 128], F32))
    ones_b = st.enter_context(nc.sbuf_tensor("a_onesb", [128, 128], BF16))
    ones_f = st.enter_context(nc.sbuf_tensor("a_onesf", [128, 128], F32))
    lamqk = st.enter_context(nc.sbuf_tensor("a_lamqk", [128, 256], F32))
    lamt = st.enter_context(nc.sbuf_tensor("a_lamt", [128, 128], F32))
    lamv = st.enter_context(nc.sbuf_tensor("a_lamv", [128, 4], F32))
    subg = st.enter_context(nc.sbuf_tensor("a_subg", [128, 1], F32))
    b_q, b_k, b_v, b_rm, b_id, b_ob, b_of, b_lq, b_lt, b_lv, b_sg = S.bufs(11, "a")
    S.dma("sp", lambda e: e.dma_start(out=rmat[:], in_=consts["rmat"]), writes=[b_rm])
    S.dma("sp", lambda e: e.dma_start(out=ident[:], in_=consts["ident"]), writes=[b_id])
    S.dma("sp", lambda e: e.dma_start(out=lamqk[:], in_=consts["lamqk"]), writes=[b_lq])
    S.dma("sp", lambda e: e.dma_start(out=subg[:], in_=consts["subg"]), writes=[b_sg])
    S.op("pool", lambda e: e.memset(ones_b[:], 1.0), writes=[b_ob])
    S.op("pool", lambda e: e.memset(ones_f[:], 1.0), writes=[b_of])
    S.op("pool", lambda e: e.memset(qTb[:, 0, :], 0.0), writes=[b_q])
    S.op("pool", lambda e: e.memset(qTb[:, 1, :], 0.0), writes=[b_q])
    S.op("dve", lambda e: e.tensor_tensor(out=lamt[:, 0:64], in0=lamqk[:, 0:64], in1=lamqk[:, 64:128], op=ALU.mult),
         reads=[b_lq], writes=[b_lt])
    S.op("dve", lambda e: e.tensor_tensor(out=lamt[:, 64:128], in0=lamqk[:, 128:192], in1=lamqk[:, 192:256],
                                          op=ALU.mult), reads=[b_lq], writes=[b_lt])
    S.op("dve", lambda e: e.tensor_reduce(out=lamv[:, 0:2], in_=lamt[:].rearrange("p (a b) -> p a b", b=64),
                                          axis=AX.X, op=ALU.add), reads=[b_lt], writes=[b_lv])
    S.op("act", lambda e: e.activation(out=lamv[:, 0:2], in_=lamv[:, 0:2], func=AF.Exp), reads=[b_lv], writes=[b_lv])
    S.op("dve", lambda e: e.tensor_tensor(out=lamv[:, 2:3], in0=lamv[:, 1:2], in1=lamv[:, 0:1], op=ALU.subtract),
         reads=[b_lv], writes=[b_lv])
    S.op("dve", lambda e: e.tensor_scalar(out=lamv[:, 3:4], in0=lamv[:, 2:3], scalar1=-float(lambda_init),
                                          scalar2=None, op0=ALU.add), reads=[b_lv], writes=[b_lv])
    S.op("dve", lambda e: e.tensor_scalar(out=subg[:], in0=subg[:], scalar1=float(1.0 - lambda_init), scalar2=None,
                                          op0=ALU.mult), reads=[b_sg], writes=[b_sg])
    pr = PsRing(S, ps, [0, 1, 2, 3, 4, 5, 6, 7])
    cs = Ring(S, nc, st, "a_cs", [128, 2, 352], F32, 3)
    t1r = Ring(S, nc, st, "a_t1", [128, 352], F32, 2)
    t2r = Ring(S, nc, st, "a_t2", [128, 352], F32, 2)
    for grp, dstT, b_dst in ((0, qTb, b_q), (1, kTb, b_k)):
        xs, xb = stg.next()
        load_group(S, nc, "sp", zr_d, b_zr, grp, xs, xb)
        for i in range(NG // 352):
            o = i * 352
            ct, cb = cs.next()
            S.dma("act", lambda e, ct=ct, o=o: e.dma_start(out=ct[:, 0, :], in_=consts["ropeC"][:, o:o + 352]),
                  writes=[cb])
            S.dma("act", lambda e, ct=ct, o=o: e.dma_start(out=ct[:, 1, :], in_=consts["ropeS"][:, o:o + 352]),
                  writes=[cb])
            pt, pb = pr.next()
            S.op("pe", lambda e, pt=pt, xs=xs, o=o: e.matmul(pt[:, 0:352], lhsT=rmat[:], rhs=xs[:, o:o + 352],
                                                            start=True, stop=True), reads=[xb, b_rm], writes=[pb])
            t1, t1b = t1r.next()
            t2, t2b = t2r.next()
            S.op("pool", lambda e, t1=t1, xs=xs, ct=ct, o=o: e.tensor_tensor(
                out=t1[:], in0=xs[:, o:o + 352], in1=ct[:, 0, :], op=ALU.mult), reads=[xb, cb], writes=[t1b])
            S.op("dve", lambda e, t2=t2, pt=pt, ct=ct: e.tensor_tensor(
                out=t2[:], in0=pt[:, 0:352], in1=ct[:, 1, :], op=ALU.mult), reads=[pb, cb], writes=[t2b])
            if grp == 0:
                for c in range(2):
                    S.op("dve", lambda e, t1=t1, t2=t2, o=o, c=c: e.tensor_tensor(
                        out=qTb[64 * c:64 * c + 64, c, o:o + 352], in0=t1[64 * c:64 * c + 64, :],
                        in1=t2[64 * c:64 * c + 64, :], op=ALU.add), reads=[t1b, t2b], writes=[b_dst])
            else:
                S.op("dve", lambda e, t1=t1, t2=t2, dstT=dstT, o=o: e.tensor_tensor(
                    out=dstT[:, o:o + 352], in0=t1[:], in1=t2[:], op=ALU.add), reads=[t1b, t2b], writes=[b_dst])
    import os as _os
    if _os.environ.get("MK_DEBUG") == "rope":
        outs = []
        for br, (srcT, b_src) in enumerate(((qTb, b_q), (kTb, b_k))):
            xs, xb = stg.next()
            S.op("dve", lambda e, xs=xs, srcT=srcT: e.tensor_copy(out=xs[:], in_=srcT[:]), reads=[b_src], writes=[xb])
            for i in range(8):
                S.dma("sp", lambda e, xs=xs, i=i, br=br: e.dma_start(out=os_d[i, br], in_=xs[:, 1056 * i:1056 * (i + 1)]),
                      reads=[xb], sembuf=xb)
            outs.append(xb)
        return outs
    xs, xb = stg.next()
    load_group(S, nc, "sp", zr_d, b_zr, 2, xs, xb)
    build_vtok(S, nc, pr, xs, xb, ident, b_id, vtok, b_v, 0, 66)
    scr = st.enter_context(nc.sbuf_tensor("a_scr", [128, 1], F32))
    sched_barrier(S, scr[:])
    pT = Ring(S, nc, st, "a_pT", [128, 512], BF16, 8)
    epi = Ring(S, nc, st, "a_epi", [128, 4, 512], F32, 2)
    ost = Ring(S, nc, st, "a_ost", [128, 512], F32, 2)
    ps_s = PsRing(S, ps, [0, 1, 4, 5, 7])
    ps_nd = [(ps[2], ps[3]), (ps[2], ps[3])]
    _bn, _bd = S.buf("n0"), S.buf("d0")
    b_nd = [(_bn, _bd), (_bn, _bd)]
    ps_ss, b_ss = ps[6], S.buf("ss")
    if _os.environ.get("MK_DEBUG") == "att64":
        dbg = st.enter_context(nc.sbuf_tensor("a_dbg", [128, 3, 512], F32))
        b_dbg = S.buf("dbg")
        sp_, sb_ = ps_s.next()
        S.op("pe", lambda e: e.matmul(sp_[:, 0:512], lhsT=kTb[0:64, 8192:8320], rhs=qTb[0:64, 256:768],
                                      start=True, stop=True), reads=[b_k, b_q], writes=[sb_])
        pt_, ptb = pT.next()
        S.op("act", lambda e: e.activation(out=pt_[:, 0:512], in_=sp_[:, 0:512], func=AF.Exp, scale=0.125),
             reads=[sb_], writes=[ptb])
        S.op("dve", lambda e: e.tensor_copy(out=dbg[:, 0, :], in_=pt_[:, 0:512]), reads=[ptb], writes=[b_dbg])
        S.op("dve", lambda e: e.tensor_copy(out=dbg[:, 1, 0:128], in_=vtok[:, 64, :]), reads=[b_v], writes=[b_dbg])
        S.op("dve", lambda e: e.tensor_copy(out=dbg[:, 1, 128:256], in_=vtok[:, 65, :]), reads=[b_v], writes=[b_dbg])
        S.op("dve", lambda e: e.tensor_copy(out=dbg[:, 2, 0:256], in_=kTb[:, 8192:8448]), reads=[b_k], writes=[b_dbg])
        for i in range(3):
            S.dma("sp", lambda e, i=i: e.dma_start(out=os_d[i, 0, :, 0:512], in_=dbg[:, i, :]), reads=[b_dbg], sembuf=b_dbg)
        return [b_dbg]
    HALF = int(_os.environ.get("MK_HALF", "66"))
    acc = Ring(S, nc, st, "a_acc", [128, 4, 512], F32, 2)
    for (q0, qn) in QT:
        nkc = 2 if q0 == 0 else int(_os.environ.get('MK_NKC', '66'))
        at, ab = acc.next()
        kc0 = 0 if q0 == 0 else int(_os.environ.get('MK_KC0', '0'))
        groups = [(g0, min(g0 + HALF, nkc)) for g0 in range(kc0, nkc, HALF)]
        steps = [(c, gi, g0, g1, kc) for c in range(2) for gi, (g0, g1) in enumerate(groups) for kc in range(g0, g1)]
        pending = []

        def emit_qk(step, q0=q0, qn=qn):
            c, gi, g0, g1, kc = step
            sp_, sb_ = ps_s.next()
            S.op("pe", lambda e, sp_=sp_, c=c, kc=kc, q0=q0, qn=qn: e.matmul(
                sp_[:, 0:qn], lhsT=kTb[:, kc * 128:(kc + 1) * 128],
                rhs=qTb[:, c, q0:q0 + qn], start=True, stop=True),
                reads=[b_k, b_q], writes=[sb_])
            pt_, ptb = pT.next()
            S.op("act", lambda e, sp_=sp_, pt_=pt_, qn=qn: e.activation(
                out=pt_[:, 0:qn], in_=sp_[:, 0:qn], func=AF.Exp, scale=0.125), reads=[sb_], writes=[ptb])
            return (step, pt_, ptb)

        def emit_pv(item, qn=qn, at=at, ab=ab):
            (c, gi, g0, g1, kc), pt_, ptb = item
            (pn, pd), (bn, bd) = ps_nd[c], b_nd[c]
            S.op("pe", lambda e, pn=pn, pt_=pt_, kc=kc, qn=qn, g0=g0, g1=g1: e.matmul(
                pn[:, 0:qn], lhsT=vtok[:, kc, :], rhs=pt_[:, 0:qn], start=(kc == g0), stop=(kc == g1 - 1)),
                reads=[b_v, ptb], writes=[bn])
            S.op("pe", lambda e, pd=pd, pt_=pt_, kc=kc, qn=qn, g0=g0, g1=g1: e.matmul(
                pd[:, 0:qn], lhsT=ones_b[:], rhs=pt_[:, 0:qn], start=(kc == g0), stop=(kc == g1 - 1)),
                reads=[b_ob, ptb], writes=[bd])
            if kc == g1 - 1:
                if gi == 0:
                    S.op("dve", lambda e, pn=pn, c=c: e.tensor_copy(out=at[:, c, 0:qn], in_=pn[:, 0:qn]),
                         reads=[bn], writes=[ab])
                    S.op("dve", lambda e, pd=pd, c=c: e.tensor_copy(out=at[:, 2 + c, 0:qn], in_=pd[:, 0:qn]),
                         reads=[bd], writes=[ab])
                else:
                    S.op("dve", lambda e, pn=pn, c=c: e.tensor_tensor(
                        out=at[:, c, 0:qn], in0=pn[:, 0:qn], in1=at[:, c, 0:qn], op=ALU.add), reads=[bn, ab], writes=[ab])
                    S.op("dve", lambda e, pd=pd, c=c: e.tensor_tensor(
                        out=at[:, 2 + c, 0:qn], in0=pd[:, 0:qn], in1=at[:, 2 + c, 0:qn], op=ALU.add),
                        reads=[bd, ab], writes=[ab])

        for step in steps:
            pending.append(emit_qk(step))
            if len(pending) > 4:
                emit_pv(pending.pop(0))
        while pending:
            emit_pv(pending.pop(0))
        et, eb = epi.next()
        for c in range(2):
            S.op("dve", lambda e, et=et, at=at, c=c, qn=qn: e.reciprocal(out=et[:, 2 + c, 0:qn], in_=at[:, 2 + c, 0:qn]),
                 reads=[ab], writes=[eb])
            S.op("dve", lambda e, et=et, at=at, c=c, qn=qn: e.tensor_tensor(
                out=et[:, c, 0:qn], in0=at[:, c, 0:qn], in1=et[:, 2 + c, 0:qn], op=ALU.mult), reads=[ab, eb], writes=[eb])
        S.op("dve", lambda e, et=et, qn=qn: e.scalar_tensor_tensor(
            out=et[:, 0, 0:qn], in0=et[:, 1, 0:qn], scalar=lamv[:, 3:4], in1=et[:, 0, 0:qn], op0=ALU.mult, op1=ALU.add),
            reads=[eb, b_lv], writes=[eb])
        S.op("act", lambda e, et=et, qn=qn: e.activation(out=et[:, 1, 0:qn], in_=et[:, 0, 0:qn], func=AF.Square),
             reads=[eb], writes=[eb])
        S.op("pe", lambda e, et=et, qn=qn: e.matmul(ps_ss[:, 0:qn], lhsT=ones_f[:], rhs=et[:, 1, 0:qn],
                                                    start=True, stop=True), reads=[eb, b_of], writes=[b_ss])
        S.op("dve", lambda e, et=et, qn=qn: e.tensor_scalar(
            out=et[:, 2, 0:qn], in0=ps_ss[:, 0:qn], scalar1=1.0 / 128, scalar2=EPS, op0=ALU.mult, op1=ALU.add),
            reads=[b_ss], writes=[eb])
        S.op("act", lambda e, et=et, qn=qn: e.activation(out=et[:, 2, 0:qn], in_=et[:, 2, 0:qn], func=AF.Sqrt),
             reads=[eb], writes=[eb])
        S.op("dve", lambda e, et=et, qn=qn: e.reciprocal(out=et[:, 2, 0:qn], in_=et[:, 2, 0:qn]),
             reads=[eb], writes=[eb])
        ot, ob = ost.next()
        S.op("dve", lambda e, et=et, ot=ot, qn=qn: e.scalar_tensor_tensor(
            out=ot[:, 0:qn], in0=et[:, 0, 0:qn], scalar=subg[:, 0:1], in1=et[:, 2, 0:qn], op0=ALU.mult, op1=ALU.mult),
            reads=[eb, b_sg], writes=[ob])
        store_group(S, "sp", os_d, 0, ot, ob, q0, qn)
    return ost.b


def emit_p2b(S, nc, st, ps, zr_d, b_zr, os_d, consts):
    xs = st.enter_context(nc.sbuf_tensor("b_xs", [128, NG], F32))
    u = st.enter_context(nc.sbuf_tensor("b_u", [128, NG], F32))
    aa = st.enter_context(nc.sbuf_tensor("b_a", [128, NG], F32))
    bb = st.enter_context(nc.sbuf_tensor("b_b", [128, NG], F32))
    hf = st.enter_context(nc.sbuf_tensor("b_hf", [128, NG], F32))
    convw = st.enter_context(nc.sbuf_tensor("b_cw", [128, 4], F32))
    convb = st.enter_context(nc.sbuf_tensor("b_cb", [128, 1], F32))
    gatew = st.enter_context(nc.sbuf_tensor("b_gw", [128, 4, 128], F32))
    gateb = st.enter_context(nc.sbuf_tensor("b_gb", [128, 4], F32))
    lrul = st.enter_context(nc.sbuf_tensor("b_ll", [128, 2], F32))
    cneg = st.enter_context(nc.sbuf_tensor("b_cn", [128, 2], F32))
    b_xs, b_u, b_a, b_b, b_hf, b_cw, b_cb, b_gw, b_gb, b_ll, b_cn = S.bufs(11, "bb")
    for t, d_, b_ in ((convw, "convw", b_cw), (convb, "convb", b_cb), (gatew, "gatew", b_gw), (gateb, "gateb", b_gb),
                      (lrul, "lrul", b_ll)):
        S.dma("sp", lambda e, t=t, d_=d_: e.dma_start(out=t[:], in_=consts[d_]), writes=[b_])
    S.op("act", lambda e: e.activation(out=cneg[:], in_=lrul[:], func=AF.Exp, scale=-1.0), reads=[b_ll], writes=[b_cn])
    S.op("act", lambda e: e.activation(out=cneg[:], in_=cneg[:], func=AF.Ln, bias=1.0), reads=[b_cn], writes=[b_cn])
    S.op("dve", lambda e: e.tensor_scalar(out=cneg[:], in0=cneg[:], scalar1=-8.0, scalar2=None, op0=ALU.mult),
         reads=[b_cn], writes=[b_cn])
    load_group(S, nc, "sp", zr_d, b_zr, 3, xs, b_xs)
    SEG = [(0, 256), (256, 8192)]
    for (o, n) in SEG:
        S.op("dve", lambda e, o=o, n=n: e.tensor_scalar(out=u[:, o:o + n], in0=xs[:, o:o + n], scalar1=convw[:, 2:3],
                                                        scalar2=convb[:, 0:1], op0=ALU.mult, op1=ALU.add),
             reads=[b_xs, b_cw, b_cb], writes=[b_u])
        for (j, sh) in ((0, -2), (1, -1), (3, 1)):
            if sh < 0:
                oo, io, m = o - sh, o, n + sh
            else:
                oo, io, m = o, o + sh, n - sh
            S.op("dve", lambda e, oo=oo, io=io, m=m, j=j: e.scalar_tensor_tensor(
                out=u[:, oo:oo + m], in0=xs[:, io:io + m], scalar=convw[:, j:j + 1], in1=u[:, oo:oo + m],
                op0=ALU.mult, op1=ALU.add), reads=[b_xs, b_cw, b_u], writes=[b_u])
    ri = Ring(S, nc, st, "b_ri", [128, 3, 512], F32, 3)
    pr = PsRing(S, ps, [0, 1, 2, 3])
    TL = [(0, 256)] + [(256 + 512 * m, 512) for m in range(16)]
    for d in range(2):
        for (o, n) in TL:
            rt, rb = ri.next()
            for g in range(2):
                pt, pb = pr.next()
                S.op("pe", lambda e, pt=pt, d=d, g=g, o=o, n=n: e.matmul(
                    pt[:, 0:n], lhsT=gatew[:, d * 2 + g, :], rhs=u[:, o:o + n], start=True, stop=True),
                    reads=[b_gw, b_u], writes=[pb])
                S.op("act", lambda e, pt=pt, rt=rt, d=d, g=g, n=n: e.activation(
                    out=rt[:, g, 0:n], in_=pt[:, 0:n], func=AF.Sigmoid, bias=gateb[:, d * 2 + g:d * 2 + g + 1]),
                    reads=[pb, b_gb], writes=[rb])
            S.op("act", lambda e, rt=rt, d=d, o=o, n=n: e.activation(
                out=aa[:, o:o + n], in_=rt[:, 0, 0:n], func=AF.Exp, scale=cneg[:, d:d + 1]),
                reads=[rb, b_cn], writes=[b_a])
            S.op("dve", lambda e, rt=rt, o=o, n=n: e.tensor_tensor(
                out=rt[:, 2, 0:n], in0=aa[:, o:o + n], in1=aa[:, o:o + n], op=ALU.mult), reads=[b_a], writes=[rb])
            S.op("dve", lambda e, rt=rt, n=n: e.tensor_scalar(
                out=rt[:, 2, 0:n], in0=rt[:, 2, 0:n], scalar1=-1.0, scalar2=1.0, op0=ALU.mult, op1=ALU.add),
                reads=[rb], writes=[rb])
            S.op("act", lambda e, rt=rt, n=n: e.activation(out=rt[:, 2, 0:n], in_=rt[:, 2, 0:n], func=AF.Sqrt),
                 reads=[rb], writes=[rb])
            S.op("pool", lambda e, rt=rt, o=o, n=n: e.tensor_tensor(
                out=rt[:, 1, 0:n], in0=rt[:, 1, 0:n], in1=u[:, o:o + n], op=ALU.mult), reads=[rb, b_u], writes=[rb])
            S.op("dve", lambda e, rt=rt, o=o, n=n: e.tensor_tensor(
                out=bb[:, o:o + n], in0=rt[:, 2, 0:n], in1=rt[:, 1, 0:n], op=ALU.mult), reads=[rb], writes=[b_b])
        if d == 0:
            S.op("dve", lambda e: e.tensor_tensor_scan(out=hf[:, 0:256], data0=aa[:, 0:256], data1=bb[:, 0:256],
                                                       initial=0.0, op0=ALU.mult, op1=ALU.add),
                 reads=[b_a, b_b], writes=[b_hf])
            S.op("dve", lambda e: e.tensor_tensor_scan(out=hf[:, 256:NG], data0=aa[:, 256:NG], data1=bb[:, 256:NG],
                                                       initial=hf[:, 255:256], op0=ALU.mult, op1=ALU.add),
                 reads=[b_a, b_b, b_hf], writes=[b_hf])
        else:
            S.op("dve", lambda e: e.tensor_tensor_scan(out=xs[:, 255::-1], data0=aa[:, 255::-1], data1=bb[:, 255::-1],
                                                       initial=0.0, op0=ALU.mult, op1=ALU.add),
                 reads=[b_a, b_b, b_u], writes=[b_xs])
            S.op("dve", lambda e: e.tensor_tensor_scan(out=xs[:, NG - 1:255:-1], data0=aa[:, NG - 1:255:-1],
                                                       data1=bb[:, NG - 1:255:-1], initial=xs[:, 0:1],
                                                       op0=ALU.mult, op1=ALU.add),
                 reads=[b_a, b_b, b_xs], writes=[b_xs])
    S.op("pool", lambda e: e.tensor_tensor(out=hf[:], in0=hf[:], in1=xs[:], op=ALU.add), reads=[b_hf, b_xs],
         writes=[b_hf])
    load_group(S, nc, "sp", zr_d, b_zr, 4, u, b_u)
    for (o, n) in TL:
        S.op("dve", lambda e, o=o, n=n: e.tensor_tensor(out=aa[:, o:o + n], in0=u[:, o:o + n], in1=u[:, o:o + n],
                                                        op=ALU.mult), reads=[b_u], writes=[b_a])
        S.op("dve", lambda e, o=o, n=n: e.tensor_scalar(out=aa[:, o:o + n], in0=aa[:, o:o + n], scalar1=0.044715,
                                                        scalar2=1.0, op0=ALU.mult, op1=ALU.add),
             reads=[b_a], writes=[b_a])
        S.op("pool", lambda e, o=o, n=n: e.tensor_tensor(out=aa[:, o:o + n], in0=aa[:, o:o + n], in1=u[:, o:o + n],
                                                         op=ALU.mult), reads=[b_a, b_u], writes=[b_a])
        S.op("act", lambda e, o=o, n=n: e.activation(out=aa[:, o:o + n], in_=aa[:, o:o + n], func=AF.Sigmoid,
                                                     scale=1.5957691216057308), reads=[b_a], writes=[b_a])
        S.op("pool", lambda e, o=o, n=n: e.tensor_tensor(out=aa[:, o:o + n], in0=aa[:, o:o + n], in1=u[:, o:o + n],
                                                         op=ALU.mult), reads=[b_a, b_u], writes=[b_a])
        S.op("dve", lambda e, o=o, n=n: e.tensor_tensor(out=bb[:, o:o + n], in0=aa[:, o:o + n], in1=hf[:, o:o + n],
                                                        op=ALU.mult), reads=[b_a, b_hf], writes=[b_b])
    store_group(S, "sp", os_d, 1, bb, b_b, 0, NG)
    return [b_b]


def na_case(r):
    if r < 4:
        return r, 0
    if r <= 124:
        return 4, r - 4
    return 5 + (r - 125), 120


def emit_p2c(S, nc, st, ps, zr_d, b_zr, os_d, consts):
    stg = Ring(S, nc, st, "c_stg", [128, NG], F32, 2)
    qb = st.enter_context(nc.sbuf_tensor("c_q", [128, 2, NG], BF16))
    kb = st.enter_context(nc.sbuf_tensor("c_k", [128, NG], BF16))
    vte = st.enter_context(nc.sbuf_tensor("c_ve", [128, 66, 128], BF16))
    vto = st.enter_context(nc.sbuf_tensor("c_vo", [128, 65, 128], BF16))
    oc = st.enter_context(nc.sbuf_tensor("c_o", [128, NG], F32))
    nabt = st.enter_context(nc.sbuf_tensor("c_bt", [128, 2, 8, 4, 64], F32))
    ident = st.enter_context(nc.sbuf_tensor("c_ident", [128, 128], F32))
    ones_b = st.enter_context(nc.sbuf_tensor("c_onesb", [128, 128], BF16))
    b_q, b_k, b_ve, b_vo, b_oc, b_bt, b_id, b_ob = S.bufs(8, "cc")
    S.dma("sp", lambda e: e.dma_start(out=nabt[:], in_=consts["nabt"]), writes=[b_bt])
    S.dma("sp", lambda e: e.dma_start(out=ident[:], in_=consts["ident"]), writes=[b_id])
    S.op("pool", lambda e: e.memset(ones_b[:], 1.0), writes=[b_ob])
    pr = PsRing(S, ps, [0, 1, 2, 3, 4, 5, 6, 7])
    S.op("pool", lambda e: e.memset(qb[:, 0, :], 0.0), writes=[b_q])
    S.op("pool", lambda e: e.memset(qb[:, 1, :], 0.0), writes=[b_q])
    for grp, dst, b_dst in ((5, qb, b_q), (6, kb, b_k)):
        xs, xb = stg.next()
        load_group(S, nc, "sp", zr_d, b_zr, grp, xs, xb)
        for i in range(4):
            o = i * (NG // 4)
            eng = "dve" if i % 2 == 0 else "pool"
            if grp == 5:
                for hl in range(2):
                    S.op(eng, lambda e, xs=xs, o=o, hl=hl: e.tensor_copy(
                        out=qb[64 * hl:64 * hl + 64, hl, o:o + NG // 4], in_=xs[64 * hl:64 * hl + 64, o:o + NG // 4]),
                        reads=[xb], writes=[b_q])
            else:
                S.op(eng, lambda e, xs=xs, dst=dst, o=o: e.tensor_copy(out=dst[:, o:o + NG // 4], in_=xs[:, o:o + NG // 4]),
                     reads=[xb], writes=[b_dst])
    xs, xb = stg.next()
    load_group(S, nc, "sp", zr_d, b_zr, 7, xs, xb)
    build_vtok(S, nc, pr, xs, xb, ident, b_id, vte, b_ve, 0, 66)
    build_vtok(S, nc, pr, xs, xb, ident, b_id, vto, b_vo, 64, 65)
    scr = st.enter_context(nc.sbuf_tensor("c_scr", [128, 1], F32))
    sched_barrier(S, scr[:])
    sbias = Ring(S, nc, st, "c_sb", [128, 256], F32, 3)
    pT = Ring(S, nc, st, "c_pT", [128, 384], BF16, 3)
    rc = Ring(S, nc, st, "c_rc", [128, 256], F32, 3)
    ps_s = PsRing(S, ps, [0, 1])
    ps_n = PsRing(S, ps, [2, 3])
    ps_d = PsRing(S, ps, [4, 5])
    for hl in range(2):
        P0 = 64 * hl
        sp_, sb_ = ps_s.next()
        pt_, ptb = pT.next()
        pn, bn = ps_n.next()
        pd, bd = ps_d.next()
        for kc in range(2):
            if kc == 1:
                sp_, sb_ = ps_s.next()
                pt_, ptb = pT.next()
            S.op("pe", lambda e, sp_=sp_, P0=P0, kc=kc, hl=hl: e.matmul(
                sp_[:, 0:256], lhsT=kb[:, kc * 128:(kc + 1) * 128], rhs=qb[:, hl, 0:256],
                start=True, stop=True), reads=[b_k, b_q], writes=[sb_])
            S.op("act", lambda e, sp_=sp_, pt_=pt_: e.activation(out=pt_[:, 0:256], in_=sp_[:, 0:256], func=AF.Exp,
                                                                 scale=0.125), reads=[sb_], writes=[ptb])
            S.op("pe", lambda e, pn=pn, pt_=pt_, kc=kc: e.matmul(pn[:, 0:256], lhsT=vte[:, kc, :], rhs=pt_[:, 0:256],
                                                                 start=(kc == 0), stop=(kc == 1)),
                 reads=[b_ve, ptb], writes=[bn])
            S.op("pe", lambda e, pd=pd, pt_=pt_, kc=kc: e.matmul(pd[:, 0:256], lhsT=ones_b[:], rhs=pt_[:, 0:256],
                                                                 start=(kc == 0), stop=(kc == 1)),
                 reads=[b_ob, ptb], writes=[bd])
        rt, rb = rc.next()
        S.op("dve", lambda e, rt=rt, pd=pd, P0=P0: e.reciprocal(out=rt[P0:P0 + 64, 0:256], in_=pd[P0:P0 + 64, 0:256]),
             reads=[bd], writes=[rb])
        S.op("dve", lambda e, rt=rt, pn=pn, P0=P0: e.tensor_tensor(
            out=oc[P0:P0 + 64, 0:256], in0=pn[P0:P0 + 64, 0:256], in1=rt[P0:P0 + 64, 0:256], op=ALU.mult),
            reads=[bn, rb], writes=[b_oc])
    for r in range(128):
        case, r0 = na_case(r)
        q0 = 256 + 64 * r
        if r0 % 2 == 0:
            vt, bv, cbase = vte, b_ve, (256 + 64 * r0) // 128
        else:
            vt, bv, cbase = vto, b_vo, (256 + 64 * r0 - 64) // 128
        k0 = 256 + 64 * r0
        for hl in range(2):
            P0 = 64 * hl
            sp_, sb_ = ps_s.next()
            for c in range(6):
                ks = k0 + 128 * c if c < 4 else 128 * (c - 4)
                S.op("pe", lambda e, sp_=sp_, P0=P0, ks=ks, c=c, q0=q0, hl=hl: e.matmul(
                    sp_[:, c * 64:(c + 1) * 64], lhsT=kb[:, ks:ks + 128], rhs=qb[:, hl, q0:q0 + 64],
                    start=True, stop=True), reads=[b_k, b_q], writes=[sb_])
            bt_, btb = sbias.next()
            S.op("dve", lambda e, sp_=sp_, bt_=bt_, hl=hl, case=case: e.scalar_tensor_tensor(
                out=bt_[:], in0=sp_[:, 0:256], scalar=0.125,
                in1=nabt[:, hl, case, :, :].rearrange("p c q -> p (c q)"), op0=ALU.mult, op1=ALU.add),
                reads=[sb_, b_bt], writes=[btb])
            pt_, ptb = pT.next()
            S.op("act", lambda e, pt_=pt_, bt_=bt_: e.activation(out=pt_[:, 0:256], in_=bt_[:], func=AF.Exp),
                 reads=[btb], writes=[ptb])
            S.op("act", lambda e, pt_=pt_, sp_=sp_: e.activation(out=pt_[:, 256:384], in_=sp_[:, 256:384], func=AF.Exp,
                                                                 scale=0.125), reads=[sb_], writes=[ptb])
            pn, bn = ps_n.next()
            pd, bd = ps_d.next()
            for c in range(6):
                if c < 4:
                    lhs, bl = vt[:, cbase + c, :], bv
                else:
                    lhs, bl = vte[:, c - 4, :], b_ve
                S.op("pe", lambda e, pn=pn, pt_=pt_, c=c, lhs=lhs: e.matmul(
                    pn[:, 0:64], lhsT=lhs, rhs=pt_[:, c * 64:(c + 1) * 64], start=(c == 0), stop=(c == 5)),
                    reads=[bl, ptb], writes=[bn])
                S.op("pe", lambda e, pd=pd, pt_=pt_, c=c: e.matmul(
                    pd[:, 0:64], lhsT=ones_b[:], rhs=pt_[:, c * 64:(c + 1) * 64], start=(c == 0), stop=(c == 5)),
                    reads=[b_ob, ptb], writes=[bd])
            rt, rb = rc.next()
            S.op("dve", lambda e, rt=rt, pd=pd, P0=P0: e.reciprocal(out=rt[P0:P0 + 64, 0:64], in_=pd[P0:P0 + 64, 0:64]),
                 reads=[bd], writes=[rb])
            S.op("dve", lambda e, rt=rt, pn=pn, P0=P0, q0=q0: e.tensor_tensor(
                out=oc[P0:P0 + 64, q0:q0 + 64], in0=pn[P0:P0 + 64, 0:64], in1=rt[P0:P0 + 64, 0:64], op=ALU.mult),
                reads=[bn, rb], writes=[b_oc])
    store_group(S, "sp", os_d, 2, oc, b_oc, 0, NG)
    return [b_oc]


def emit_final_norm(S, nc, st, ps, xT_d, b_xd, gvec, b_gvec, out_d, tag):
    xr = Ring(S, nc, st, f"{tag}_x", [128, NT], F32, 3)
    sq = Ring(S, nc, st, f"{tag}_sq", [128, NT], F32, 2)
    tm = Ring(S, nc, st, f"{tag}_tm", [128, NT], F32, 3)
    ones = st.enter_context(nc.sbuf_tensor(f"{tag}_ones", [128, 128], F32))
    rstd = st.enter_context(nc.sbuf_tensor(f"{tag}_rstd", [128, NT], F32))
    b_ones, b_rstd = S.bufs(2, tag)
    S.op("pool", lambda e: e.memset(ones[:], 1.0), writes=[b_ones])
    xv = xT_d.rearrange("(k p) t -> k p t", p=128)
    pss = [(ps[i], S.buf(f"{tag}_pss{i}")) for i in range(3)]
    for k in range(32):
        xt, xb = xr.next()
        S.dma("sp", lambda e, xt=xt, k=k: e.dma_start(out=xt[:], in_=xv[k]), reads=[b_xd], writes=[xb])
        qt, qb = sq.next()
        S.op("act", lambda e, xt=xt, qt=qt: e.activation(out=qt[:], in_=xt[:], func=AF.Square), reads=[xb], writes=[qb])
        for i, (o, n) in enumerate(TT):
            S.op("pe", lambda e, i=i, o=o, n=n, qt=qt, k=k: e.matmul(
                pss[i][0][:, 0:n], lhsT=ones[:], rhs=qt[:, o:o + n], start=(k == 0), stop=(k == 31)),
                reads=[qb, b_ones], writes=[pss[i][1]])
    for i, (o, n) in enumerate(TT):
        S.op("dve", lambda e, i=i, o=o, n=n: e.tensor_scalar(
            out=rstd[:, o:o + n], in0=pss[i][0][:, 0:n], scalar1=1.0 / D, scalar2=EPS, op0=ALU.mult, op1=ALU.add),
            reads=[pss[i][1]], writes=[b_rstd])
    S.op("act", lambda e: e.activation(out=rstd[:], in_=rstd[:], func=AF.Sqrt), reads=[b_rstd], writes=[b_rstd])
    S.op("dve", lambda e: e.reciprocal(out=rstd[:], in_=rstd[:]), reads=[b_rstd], writes=[b_rstd])
    for k in range(32):
        xt, xb = xr.next()
        S.dma("sp", lambda e, xt=xt, k=k: e.dma_start(out=xt[:], in_=xv[k]), reads=[b_xd], writes=[xb])
        tt, tb = tm.next()
        S.op("dve", lambda e, xt=xt, tt=tt, k=k: e.scalar_tensor_tensor(
            out=tt[:], in0=xt[:], scalar=gvec[:, k:k + 1], in1=rstd[:], op0=ALU.mult, op1=ALU.mult),
            reads=[xb, b_rstd, b_gvec], writes=[tb])
        S.dma("sp", lambda e, tt=tt, k=k: e.dma_start(out=out_d[k], in_=tt[:, NC_CTX:NT]), reads=[tb], sembuf=tb)
    return tm.b


def emit_p3(S, nc, ps, scr, layer, last, d):
    MB = layer * 192
    with contextlib.ExitStack() as st0:
        modv = st0.enter_context(nc.sbuf_tensor("p3_modv", [128, 384, 2], F32))
        b_modv = S.buf("p3modv")
        S.dma("sp", lambda e: e.dma_start(out=modv[:], in_=d["modv"]), writes=[b_modv])
        b_x1d, b_h2d, b_x2d = S.bufs(3, "p3d")
        with contextlib.ExitStack() as st:
            yT = st.enter_context(nc.sbuf_tensor("p3_yT", [128, 32, NT], BF16))
            b_y = S.buf("p3y")
            with contextlib.ExitStack() as sm:
                oT = sm.enter_context(nc.sbuf_tensor("p3_oT", [128, 3, 8, NT], BF16))
                b_o = S.buf("p3o")
                for j in range(8):
                    for br in range(3):
                        S.dma("pool", lambda e, j=j, br=br: e.dma_start(out=oT[:, br, j, :], in_=d["orecv"][j, br]),
                              writes=[b_o])
                wr = Ring(S, nc, sm, "p3_wbr", [128, 3, 8, 256], BF16, 2)
                gr = Ring(S, nc, sm, "p3_gs", [128, NT], F32, 4)
                ya = Ring(S, nc, sm, "p3_ya", [128, NT], F32, 2)
                tr = Ring(S, nc, sm, "p3_tm", [128, 352], F32, 3)
                pr = PsRing(S, ps, [0, 1, 2, 3, 4, 5])
                for wi in range(16):
                    wt, wb = wr.next()
                    for br in range(3):
                        S.dma("pool", lambda e, wt=wt, br=br, wi=wi: e.dma_start(
                            out=wt[:, br, :, :],
                            in_=d["wbr"][br].rearrange("(k p) f -> p k f", p=128)[:, :, wi * 256:(wi + 1) * 256]),
                            writes=[wb])
                    for c in range(2):
                        dc = wi * 2 + c
                        yt, yb = ya.next()
                        for br in range(3):
                            gt, gb = gr.next()
                            S.dma("sp", lambda e, gt=gt, br=br, dc=dc: e.dma_start(out=gt[:], in_=d["gsig"][br * 32 + dc]),
                                  writes=[gb])
                            for (o, n) in TT:
                                pt, pb = pr.next()
                                for k in range(8):
                                    S.op("pe", lambda e, pt=pt, wt=wt, br=br, k=k, c=c, o=o, n=n: e.matmul(
                                        pt[:, 0:n], lhsT=wt[:, br, k, c * 128:(c + 1) * 128], rhs=oT[:, br, k, o:o + n],
                                        start=(k == 0), stop=(k == 7)), reads=[wb, b_o], writes=[pb])
                                if br == 0:
                                    S.op("dve", lambda e, pt=pt, yt=yt, gt=gt, o=o, n=n: e.tensor_tensor(
                                        out=yt[:, o:o + n], in0=pt[:, 0:n], in1=gt[:, o:o + n], op=ALU.mult),
                                        reads=[pb, gb], writes=[yb])
                                else:
                                    t_, tb_ = tr.next()
                                    S.op("dve", lambda e, pt=pt, t_=t_, gt=gt, o=o, n=n: e.tensor_tensor(
                                        out=t_[:, 0:n], in0=pt[:, 0:n], in1=gt[:, o:o + n], op=ALU.mult),
                                        reads=[pb, gb], writes=[tb_])
                                    S.op("pool", lambda e, t_=t_, yt=yt, o=o, n=n: e.tensor_tensor(
                                        out=yt[:, o:o + n], in0=yt[:, o:o + n], in1=t_[:, 0:n], op=ALU.add),
                                        reads=[tb_, yb], writes=[yb])
                        S.op("act", lambda e, yt=yt, dc=dc: e.activation(out=yT[:, dc, :], in_=yt[:], func=AF.Copy),
                             reads=[yb], writes=[b_y])
                sched_barrier(S, scr)
            with contextlib.ExitStack() as so:
                wr = Ring(S, nc, so, "p3_wo", [128, 32, 256], BF16, 3)
                xr = Ring(S, nc, so, "p3_xi", [128, NT], F32, 3)
                xo = Ring(S, nc, so, "p3_xo", [128, NT], F32, 3)
                pr = PsRing(S, ps, [0, 1, 2, 3, 4, 5])
                wv = d["wout"].rearrange("(k p) f -> p k f", p=128)
                xv = d["xT"].rearrange("(k p) t -> k p t", p=128)
                x1v = d["x1T"].rearrange("(k p) t -> k p t", p=128)
                for wi in range(16):
                    wt, wb = wr.next()
                    S.dma("pool", lambda e, wt=wt, wi=wi: e.dma_start(out=wt[:], in_=wv[:, :, wi * 256:(wi + 1) * 256]),
                          writes=[wb])
                    for c in range(2):
                        dc = wi * 2 + c
                        xt, xb = xr.next()
                        S.dma("sp", lambda e, xt=xt, dc=dc: e.dma_start(out=xt[:], in_=xv[dc]), writes=[xb])
                        ot, ob = xo.next()
                        for (o, n) in TT:
                            pt, pb = pr.next()
                            for k in range(32):
                                S.op("pe", lambda e, pt=pt, wt=wt, k=k, c=c, o=o, n=n: e.matmul(
                                    pt[:, 0:n], lhsT=wt[:, k, c * 128:(c + 1) * 128], rhs=yT[:, k, o:o + n],
                                    start=(k == 0), stop=(k == 31)), reads=[wb, b_y], writes=[pb])
                            segs = [(0, NC_CTX, 1), (NC_CTX, n, 0)] if o == 0 else [(0, n, 0)]
                            for (a, b, col) in segs:
                                S.op("dve", lambda e, pt=pt, ot=ot, xt=xt, o=o, a=a, b=b, col=col, dc=dc: e.scalar_tensor_tensor(
                                    out=ot[:, o + a:o + b], in0=pt[:, a:b], scalar=modv[:, MB + 64 + dc, col:col + 1],
                                    in1=xt[:, o + a:o + b], op0=ALU.mult, op1=ALU.add),
                                    reads=[pb, xb, b_modv], writes=[ob])
                        S.dma("sp", lambda e, ot=ot, dc=dc: e.dma_start(out=x1v[dc], in_=ot[:]), reads=[ob], sembuf=ob)
                sched_barrier(S, scr)
        gT = st0.enter_context(nc.sbuf_tensor("p3_gT", [32, NT], F32))
        b_gT = S.buf("p3gT")
        with contextlib.ExitStack() as sn:
            gvec = sn.enter_context(nc.sbuf_tensor("p3_n2g", [128, 32], F32))
            h2T = sn.enter_context(nc.sbuf_tensor("p3_h2T", [128, 32, NT], BF16))
            wrt = sn.enter_context(nc.sbuf_tensor("p3_wrt", [128, 32, 32], BF16))
            brt = sn.enter_context(nc.sbuf_tensor("p3_brt", [128, 32], F32))
            ident = sn.enter_context(nc.sbuf_tensor("p3_ident", [128, 128], F32))
            b_gv, b_h2, b_wrt, b_brt, b_id = S.bufs(5, "p3n")
            S.dma("sp", lambda e: e.dma_start(out=gvec[:], in_=d["n2g"]), writes=[b_gv])
            S.dma("sp", lambda e: e.dma_start(out=brt[:], in_=d["brt"]), writes=[b_brt])
            S.dma("sp", lambda e: e.dma_start(out=ident[:], in_=d["ident"]), writes=[b_id])
            S.dma("pool", lambda e: e.dma_start(out=wrt[:], in_=d["wrt"].rearrange("(k p) f -> p k f", p=128)),
                  writes=[b_wrt])
            emit_norm_mod(S, nc, sn, ps, d["x1T"], b_x1d, modv, b_modv, gvec, b_gv, MB + 96, h2T, b_h2, "p3nm")
            S.dma("sp", lambda e: e.dma_start(out=d["h2d"], in_=h2T[:]), reads=[b_h2], sembuf=b_h2)
            lg = Ring(S, nc, sn, "p3_lg", [128, 3, 32], F32, 3)
            m8 = Ring(S, nc, sn, "p3_m8", [128, 12], F32, 3)
            pr = PsRing(S, ps, [3, 4, 5, 6])
            for i in range(9):
                t0 = i * 128
                nt_ = min(128, NT - t0)
                pt, pb = pr.next()
                for k in range(32):
                    S.op("pe", lambda e, pt=pt, k=k, t0=t0, nt_=nt_: e.matmul(
                        pt[0:nt_, 0:32], lhsT=h2T[:, k, t0:t0 + nt_], rhs=wrt[:, k, :], start=(k == 0), stop=(k == 31)),
                        reads=[b_h2, b_wrt], writes=[pb])
                lt, lb = lg.next()
                mt, mb = m8.next()
                S.op("dve", lambda e, pt=pt, lt=lt, nt_=nt_: e.tensor_tensor(
                    out=lt[0:nt_, 0, :], in0=pt[0:nt_, 0:32], in1=brt[0:nt_, :], op=ALU.add), reads=[pb, b_brt], writes=[lb])
                S.op("dve", lambda e, lt=lt, mt=mt, nt_=nt_: e.max(out=mt[0:nt_, 0:8], in_=lt[0:nt_, 0, :]),
                     reads=[lb], writes=[mb])
                S.op("dve", lambda e, mt=mt, nt_=nt_: e.tensor_scalar(
                    out=mt[0:nt_, 8:9], in0=mt[0:nt_, 0:1], scalar1=-1.0, scalar2=None, op0=ALU.mult),
                    reads=[mb], writes=[mb])
                S.op("act", lambda e, lt=lt, mt=mt, nt_=nt_: e.activation(
                    out=lt[0:nt_, 1, :], in_=lt[0:nt_, 0, :], func=AF.Exp, bias=mt[0:nt_, 8:9]), reads=[lb, mb], writes=[lb])
                S.op("dve", lambda e, lt=lt, mt=mt, nt_=nt_: e.tensor_scalar(
                    out=lt[0:nt_, 2, :], in0=lt[0:nt_, 0, :], scalar1=mt[0:nt_, 3:4], scalar2=None, op0=ALU.is_ge),
                    reads=[lb, mb], writes=[lb])
                S.op("dve", lambda e, lt=lt, nt_=nt_: e.tensor_tensor(
                    out=lt[0:nt_, 1, :], in0=lt[0:nt_, 1, :], in1=lt[0:nt_, 2, :], op=ALU.mult), reads=[lb], writes=[lb])
                S.op("dve", lambda e, lt=lt, mt=mt, nt_=nt_: e.tensor_reduce(
                    out=mt[0:nt_, 9:10], in_=lt[0:nt_, 1, :], axis=AX.X, op=ALU.add), reads=[lb], writes=[mb])
                S.op("dve", lambda e, mt=mt, nt_=nt_: e.reciprocal(out=mt[0:nt_, 10:11], in_=mt[0:nt_, 9:10]),
                     reads=[mb], writes=[mb])
                S.op("dve", lambda e, lt=lt, mt=mt, nt_=nt_: e.tensor_scalar(
                    out=lt[0:nt_, 2, :], in0=lt[0:nt_, 1, :], scalar1=mt[0:nt_, 10:11], scalar2=None, op0=ALU.mult),
                    reads=[lb, mb], writes=[lb])
                pt2, pb2 = pr.next()
                S.op("pe", lambda e, pt2=pt2, lt=lt, nt_=nt_: e.transpose(
                    out=pt2[0:32, 0:nt_], in_=lt[0:nt_, 2, :], identity=ident[0:nt_, 0:nt_]),
                    reads=[lb, b_id], writes=[pb2])
                S.op("act", lambda e, pt2=pt2, t0=t0, nt_=nt_: e.activation(
                    out=gT[:, t0:t0 + nt_], in_=pt2[0:32, 0:nt_], func=AF.Copy), reads=[pb2], writes=[b_gT])
            sched_barrier(S, scr)
        with contextlib.ExitStack() as se:
            sel = se.enter_context(nc.sbuf_tensor("p3_sel", [32, 32, 128], F32))
            bdn = se.enter_context(nc.sbuf_tensor("p3_bdn", [32, 4096], F32))
            bgu = se.enter_context(nc.sbuf_tensor("p3_bgu", [128, 32, 8], F32))
            b_sel, b_bdn, b_bgu = S.bufs(3, "p3e")
            S.dma("sp", lambda e: e.dma_start(out=sel[:], in_=d["sel"]), writes=[b_sel])
            S.dma("sp", lambda e: e.dma_start(out=bdn[:], in_=d["bdn"]), writes=[b_bdn])
            S.dma("sp", lambda e: e.dma_start(out=bgu[:], in_=d["bgu"]), writes=[b_bgu])
            h2r = Ring(S, nc, se, "p3_h2", [128, 32, 352], BF16, 1)
            accr = Ring(S, nc, se, "p3_acc", [128, 32, 352], F32, 1)
            wgr = Ring(S, nc, se, "p3_wg", [128, 32, 256], BF16, 2)
            wdr = Ring(S, nc, se, "p3_wd", [128, 4, 2048], BF16, 2)
            gbr = Ring(S, nc, se, "p3_gb", [128, 352], F32, 2)
            glr = Ring(S, nc, se, "p3_gl", [128, 4, 352], F32, 2)
            tmr = Ring(S, nc, se, "p3_t", [128, 352], F32, 3)
            acr = Ring(S, nc, se, "p3_ac", [128, 4, 352], BF16, 2)
            xir = Ring(S, nc, se, "p3_x1", [128, 352], F32, 3)
            xor_ = Ring(S, nc, se, "p3_x2", [128, 352], F32, 3)
            pr = PsRing(S, ps, [0, 1, 2, 3, 4, 5, 6, 7])
            x1v = d["x1T"].rearrange("(k p) t -> k p t", p=128)
            x2v = d["x2T"].rearrange("(k p) t -> k p t", p=128)
            for (o, n) in TT:
                ht, hb = h2r.next()
                S.dma("sp", lambda e, ht=ht, o=o, n=n: e.dma_start(out=ht[:], in_=d["h2d"][:, :, o:o + n]),
                      reads=[b_h2d], writes=[hb])
                at, ab = accr.next()
                for dc in range(32):
                    pt, pb = pr.next()
                    S.op("pe", lambda e, pt=pt, dc=dc, o=o, n=n: e.matmul(
                        pt[:, 0:n], lhsT=bdn[:, dc * 128:(dc + 1) * 128], rhs=gT[:, o:o + n], start=True, stop=True),
                        reads=[b_bdn, b_gT], writes=[pb])
                    S.op("act", lambda e, pt=pt, at=at, dc=dc, n=n: e.activation(out=at[:, dc, :], in_=pt[:, 0:n],
                                                                                func=AF.Copy), reads=[pb], writes=[ab])
                for ex in range(32):
                    pt, pb = pr.next()
                    S.op("pe", lambda e, pt=pt, ex=ex, o=o, n=n: e.matmul(
                        pt[:, 0:n], lhsT=sel[:, ex, :], rhs=gT[:, o:o + n], start=True, stop=True),
                        reads=[b_sel, b_gT], writes=[pb])
                    gbt, gbb = gbr.next()
                    S.op("act", lambda e, pt=pt, gbt=gbt, n=n: e.activation(out=gbt[:], in_=pt[:, 0:n], func=AF.Copy),
                         reads=[pb], writes=[gbb])
                    glt, glb = glr.next()
                    act_, actb = acr.next()
                    for wi in range(4):
                        wt, wb = wgr.next()
                        S.dma("sp" if wi % 2 == 0 else "act", lambda e, wt=wt, ex=ex, wi=wi: e.dma_start(
                            out=wt[:], in_=d["wgu"][ex, wi]),
                            writes=[wb])
                        for c2 in range(2):
                            c = wi * 2 + c2
                            pt, pb = pr.next()
                            for k in range(32):
                                S.op("pe", lambda e, pt=pt, wt=wt, k=k, c2=c2, ht=ht, n=n: e.matmul(
                                    pt[:, 0:n], lhsT=wt[:, k, c2 * 128:(c2 + 1) * 128], rhs=ht[:, k, :],
                                    start=(k == 0), stop=(k == 31)), reads=[wb, hb], writes=[pb])
                            if c < 4:
                                S.op("dve", lambda e, pt=pt, glt=glt, c=c, ex=ex, n=n: e.tensor_scalar(
                                    out=glt[:, c, :], in0=pt[:, 0:n], scalar1=bgu[:, ex, c:c + 1], scalar2=7.0,
                                    op0=ALU.add, op1=ALU.min), reads=[pb, b_bgu], writes=[glb])
                                t_, tb_ = tmr.next()
                                S.op("act", lambda e, t_=t_, glt=glt, c=c: e.activation(
                                    out=t_[:], in_=glt[:, c, :], func=AF.Sigmoid, scale=1.702), reads=[glb], writes=[tb_])
                                S.op("pool", lambda e, t_=t_, glt=glt, c=c: e.tensor_tensor(
                                    out=glt[:, c, :], in0=glt[:, c, :], in1=t_[:], op=ALU.mult), reads=[tb_, glb], writes=[glb])
                            else:
                                t_, tb_ = tmr.next()
                                S.op("dve", lambda e, pt=pt, t_=t_, c=c, ex=ex, n=n: e.tensor_scalar(
                                    out=t_[:], in0=pt[:, 0:n], scalar1=bgu[:, ex, c:c + 1], scalar2=7.0,
                                    op0=ALU.add, op1=ALU.min), reads=[pb, b_bgu], writes=[tb_])
                                S.op("dve", lambda e, t_=t_: e.tensor_scalar(
                                    out=t_[:], in0=t_[:], scalar1=-7.0, scalar2=1.0, op0=ALU.max, op1=ALU.add),
                                    reads=[tb_], writes=[tb_])
                                S.op("pool", lambda e, t_=t_, glt=glt, c=c: e.tensor_tensor(
                                    out=t_[:], in0=t_[:], in1=glt[:, c - 4, :], op=ALU.mult), reads=[tb_, glb], writes=[tb_])
                                S.op("dve", lambda e, t_=t_, act_=act_, gbt=gbt, c=c: e.tensor_tensor(
                                    out=act_[:, c - 4, :], in0=t_[:], in1=gbt[:], op=ALU.mult),
                                    reads=[tb_, gbb], writes=[actb])
                    for wj in range(2):
                        wt, wb = wdr.next()
                        S.dma("sp" if wj % 2 == 0 else "act", lambda e, wt=wt, ex=ex, wj=wj: e.dma_start(
                            out=wt[:], in_=d["wdn"][ex, wj]),
                            writes=[wb])
                        for dcl in range(16):
                            dc = wj * 16 + dcl
                            pt, pb = pr.next()
                            for k in range(4):
                                S.op("pe", lambda e, pt=pt, wt=wt, k=k, dcl=dcl, act_=act_, n=n: e.matmul(
                                    pt[:, 0:n], lhsT=wt[:, k, dcl * 128:(dcl + 1) * 128], rhs=act_[:, k, :],
                                    start=(k == 0), stop=(k == 3)), reads=[wb, actb], writes=[pb])
                            S.op("dve", lambda e, pt=pt, at=at, dc=dc, n=n: e.tensor_tensor(
                                out=at[:, dc, :], in0=pt[:, 0:n], in1=at[:, dc, :], op=ALU.add), reads=[pb, ab], writes=[ab])
                for dc in range(32):
                    xt, xb = xir.next()
                    S.dma("sp", lambda e, xt=xt, dc=dc, o=o, n=n: e.dma_start(out=xt[:], in_=x1v[dc][:, o:o + n]),
                          reads=[b_x1d], writes=[xb])
                    ot, ob = xor_.next()
                    segs = [(0, NC_CTX, 1), (NC_CTX, n, 0)] if o == 0 else [(0, n, 0)]
                    for (a, b, col) in segs:
                        S.op("dve", lambda e, at=at, ot=ot, xt=xt, a=a, b=b, col=col, dc=dc: e.scalar_tensor_tensor(
                            out=ot[:, a:b], in0=at[:, dc, a:b], scalar=modv[:, MB + 160 + dc, col:col + 1],
                            in1=xt[:, a:b], op0=ALU.mult, op1=ALU.add), reads=[ab, xb, b_modv], writes=[ob])
                    S.dma("sp", lambda e, ot=ot, dc=dc, o=o, n=n: e.dma_start(out=x2v[dc][:, o:o + n], in_=ot[:]),
                          reads=[ob], sembuf=ob)
            sched_barrier(S, scr)
        if last:
            with contextlib.ExitStack() as sf:
                fg = sf.enter_context(nc.sbuf_tensor("p3_fg", [128, 32], F32))
                b_fg = S.buf("p3fg")
                S.dma("sp", lambda e: e.dma_start(out=fg[:], in_=d["fing"]), writes=[b_fg])
                emit_final_norm(S, nc, sf, ps, d["x2T"], b_x2d, fg, b_fg, d["outT"], "p3f")
                sched_barrier(S, scr)
        sched_barrier(S, scr)


def emit_pw(S, nc, st, wgu_d, wdn_d, wgu_o, wdn_o):
    ring = Ring(S, nc, st, "pw_t", [128, 8, 1024], BF16, 6)
    gi = wgu_d.rearrange("e (k p) c -> p (e k) c", p=128)
    go = wgu_o.rearrange("e (k p) c -> p (e k) c", p=128)
    outs = []
    for r in range(32):
        t, b = ring.next()
        S.dma("pool", lambda e, t=t, r=r: e.dma_start(out=t[:], in_=gi[:, r * 8:(r + 1) * 8, :]), writes=[b])
        S.dma("sp", lambda e, t=t, r=r: e.dma_start(out=go[:, r * 8:(r + 1) * 8, :], in_=t[:]), reads=[b], sembuf=b)
    di = wdn_d.rearrange("e (k p) (h c) -> p (e k) h c", p=128, h=4)
    do = wdn_o.rearrange("e (k p) (h c) -> p (e k) h c", p=128, h=4)
    for r in range(16):
        t, b = ring.next()
        tv = t[:].rearrange("p (a h) c -> p a h c", h=4)
        S.dma("pool", lambda e, tv=tv, r=r: e.dma_start(out=tv, in_=di[:, r * 2:(r + 1) * 2, :, :]), writes=[b])
        S.dma("sp", lambda e, tv=tv, r=r: e.dma_start(out=do[:, r * 2:(r + 1) * 2, :, :], in_=tv), reads=[b], sembuf=b)
    return ring.b


GRID_W = 64
def fm(v):
    return np.ascontiguousarray(np.asarray(v, np.float32).reshape(-1, 128).T)

def rope_tables():
    t = np.arange(8192)
    row = (t // GRID_W).astype(np.float32); col = (t % GRID_W).astype(np.float32)
    inv = (np.float32(10000.0) ** (-np.arange(16, dtype=np.float32) / np.float32(16))).astype(np.float32)
    ang_r = (row[:, None] * inv).astype(np.float32); ang_c = (col[:, None] * inv).astype(np.float32)
    C = np.ones((128, 8448), np.float32); Sn = np.zeros((128, 8448), np.float32)
    for p in range(128):
        d = p % 64
        ang = ang_r if d < 32 else ang_c
        f = d % 16
        C[p, 256:] = np.cos(ang[:, f]); Sn[p, 256:] = np.sin(ang[:, f])
    return C, Sn

def rot_matrix_T():
    R = np.zeros((128, 128), np.float32)
    for m in range(128):
        if m % 32 < 16:
            R[m, m + 16] = -1.0
        else:
            R[m, m - 16] = 1.0
    return np.ascontiguousarray(R.T)

def na_bias_tables(rpb2):
    out = np.full((2, 8, 4, 128, 64), -30000.0, np.float32)
    qc = np.arange(64)
    c0 = np.clip(qc - 8, 0, 48)
    for case in range(8):
        for kr in range(8):
            if case < 4:
                row_rel = kr - case + 7
            elif case == 4:
                row_rel = kr + 3
            else:
                r = 125 + (case - 5)
                row_rel = 120 + kr - r + 7
            for kcol in range(64):
                valid = (kcol >= c0) & (kcol < c0 + 16)
                col_rel = kcol - qc + 15
                c, k128 = kr // 2, (kr % 2) * 64 + kcol
                for hl in range(2):
                    vals = rpb2[hl, row_rel, np.clip(col_rel, 0, 30)]
                    out[hl, case, c, k128, :] = np.where(valid, vals, np.float32(-30000.0))
    return np.ascontiguousarray(out.transpose(3, 0, 1, 2, 4))

def p2_consts(inp, l, j, ropeC, ropeS):
    gw = inp["lru_gate_w"][l][:, :, j]
    gb = inp["lru_gate_b"][l][:, :, j]
    return {
        "ropeC": ropeC, "ropeS": ropeS, "rmat": rot_matrix_T(), "ident": np.eye(128, dtype=np.float32),
        "lamqk": np.ascontiguousarray(np.broadcast_to(inp["lam_qk"][l].reshape(1, 256), (128, 256))).astype(np.float32),
        "subg": np.ascontiguousarray(inp["subln_g"][l].reshape(128, 1)).astype(np.float32),
        "convw": np.ascontiguousarray(inp["conv_w"][l][:, 128 * j:128 * j + 128].T).astype(np.float32),
        "convb": np.ascontiguousarray(inp["conv_b"][l][128 * j:128 * j + 128].reshape(128, 1)).astype(np.float32),
        "gatew": np.ascontiguousarray(gw.reshape(4, 128, 128).transpose(1, 0, 2)).astype(np.float32),
        "gateb": np.ascontiguousarray(gb.reshape(4, 128).T).astype(np.float32),
        "lrul": np.ascontiguousarray(inp["lru_lambda"][l][:, 128 * j:128 * j + 128].T).astype(np.float32),
        "nabt": na_bias_tables(np.asarray(inp["na_rpb"][l][2 * j:2 * j + 2], np.float32)),
    }

P2_CONST_SHAPES = {"ropeC": [128, 8448], "ropeS": [128, 8448], "rmat": [128, 128], "ident": [128, 128],
                   "lamqk": [128, 256], "subg": [128, 1], "convw": [128, 4], "convb": [128, 1],
                   "gatew": [128, 4, 128], "gateb": [128, 4], "lrul": [128, 2], "nabt": [128, 2, 8, 4, 64]}

GU_PERM = np.concatenate([np.arange(0, 1024, 2), np.arange(1, 1024, 2)])

def p3_consts(inp, l):
    wgu = np.ascontiguousarray(np.asarray(inp["w_gu"][l], np.float32)[:, :, GU_PERM])
    bgu = np.asarray(inp["b_gu"][l], np.float32)[:, GU_PERM].reshape(32, 8, 128).transpose(2, 0, 1)
    sel = np.zeros((32, 32, 128), np.float32)
    for e in range(32):
        sel[e, e, :] = 1.0
    return {
        "n2g": fm(inp["norm2_g"][l]), "wbr": np.ascontiguousarray(inp["w_branch"][l], dtype=np.float32),
        "wout": np.ascontiguousarray(inp["w_out"][l], dtype=np.float32),
        "wrt": np.ascontiguousarray(inp["w_router"][l], dtype=np.float32),
        "brt": np.ascontiguousarray(np.broadcast_to(np.asarray(inp["b_router"][l], np.float32).reshape(1, 32), (128, 32))),
        "wgu": wgu, "bgu": np.ascontiguousarray(bgu),
        "wdn": np.ascontiguousarray(inp["w_down"][l], dtype=np.float32),
        "bdn": np.ascontiguousarray(inp["b_down"][l], dtype=np.float32),
        "sel": sel, "ident": np.eye(128, dtype=np.float32), "fing": fm(inp["final_g"]),
    }

P3_IN_SHAPES = {"orecv": [8, 3, 128, 1056], "gsig": [96, 128, 1056], "xT": [4096, 1056], "modv": [128, 384, 2],
                "n2g": [128, 32], "wbr": [3, 1024, 4096], "wout": [4096, 4096], "wrt": [4096, 32], "brt": [128, 32],
                "wgu": [32, 4, 128, 32, 256], "bgu": [128, 32, 8], "wdn": [32, 2, 128, 4, 2048], "bdn": [32, 4096],
                "sel": [32, 32, 128], "ident": [128, 128], "fing": [128, 32]}


def tile_major_gu(w):
    return np.ascontiguousarray(w.reshape(32, 32, 128, 4, 256).transpose(0, 3, 2, 1, 4))

def tile_major_dn(w):
    return np.ascontiguousarray(w.reshape(32, 4, 128, 2, 2048).transpose(0, 3, 2, 1, 4))
def _run(nc, in_maps):
    from concourse.bass_utils import run_bass_kernel_spmd
    return run_bass_kernel_spmd(nc, in_maps, core_ids=list(range(len(in_maps)))).results


def _build_p0():
    nc = bass.Bass("TRN2", target_bir_lowering=False)
    cT_d = nc.dram_tensor("cT", [128, 32, 2], F32, kind="ExternalInput").ap()
    wada_d = nc.dram_tensor("wada", [4096, 6144], F32, kind="ExternalInput").ap()
    bada_d = nc.dram_tensor("bada", [2, 6144], F32, kind="ExternalInput").ap()
    modv_d = nc.dram_tensor("modv", [2, 6144], F32, kind="ExternalOutput").ap()
    S = Sched(nc)
    with contextlib.ExitStack() as st:
        ps = alloc_psum(nc, st)
        outs = emit_p0(S, nc, st, ps, cT_d, wada_d, bada_d, modv_d)
        S.wait_all("sp", outs)
        S.emit(st)
    return nc


def _build_p1(layer):
    nc = bass.Bass("TRN2", target_bir_lowering=False)
    xT_d = nc.dram_tensor("xT", [4096, NT], F32, kind="ExternalInput").ap()
    modv_d = nc.dram_tensor("modv", [128, 384, 2], F32, kind="ExternalInput").ap()
    n1g_d = nc.dram_tensor("n1g", [128, 32], F32, kind="ExternalInput").ap()
    win_d = nc.dram_tensor("win", [4096, 20480], F32, kind="ExternalInput").ap()
    zs_d = nc.dram_tensor("zsend", [8, 8, 128, NT], F32, kind="ExternalOutput").ap()
    gs_d = nc.dram_tensor("gsig", [96, 128, NT], F32, kind="ExternalOutput").ap()
    S = Sched(nc)
    with contextlib.ExitStack() as st:
        ps = alloc_psum(nc, st)
        outs = emit_p1(S, nc, st, ps, layer, xT_d, S.buf("xd"), modv_d, n1g_d, win_d, zs_d, None, gs_d, None)
        S.wait_all("sp", outs)
        S.emit(st)
    return nc


def _build_p2(lambda_init):
    nc = bass.Bass("TRN2", target_bir_lowering=False)
    zr_d = nc.dram_tensor("zr", [8, 8, 128, NT], F32, kind="ExternalInput").ap()
    os_d = nc.dram_tensor("os", [8, 3, 128, NT], F32, kind="ExternalOutput").ap()
    cd = {k: nc.dram_tensor(k, v, F32, kind="ExternalInput").ap() for k, v in P2_CONST_SHAPES.items()}
    S = Sched(nc)
    with contextlib.ExitStack() as st:
        ps = alloc_psum(nc, st)
        scr = st.enter_context(nc.sbuf_tensor("p2_scr", [128, 1], F32))
        b_zr = S.buf("zr")
        with contextlib.ExitStack() as sa:
            emit_p2a(S, nc, sa, ps, zr_d, b_zr, os_d, cd, lambda_init)
            sched_barrier(S, scr[:])
        with contextlib.ExitStack() as sb:
            emit_p2b(S, nc, sb, ps, zr_d, b_zr, os_d, cd)
            sched_barrier(S, scr[:])
        with contextlib.ExitStack() as sc:
            emit_p2c(S, nc, sc, ps, zr_d, b_zr, os_d, cd)
            sched_barrier(S, scr[:])
        S.emit(st)
    return nc


def _build_pw():
    nc = bass.Bass("TRN2", target_bir_lowering=False)
    a = nc.dram_tensor("wgu32", [8, 4096, 1024], F32, kind="ExternalInput").ap()
    b = nc.dram_tensor("wdn32", [8, 512, 4096], F32, kind="ExternalInput").ap()
    ao = nc.dram_tensor("wgub", [8, 4096, 1024], BF16, kind="ExternalOutput").ap()
    bo = nc.dram_tensor("wdnb", [8, 512, 4096], BF16, kind="ExternalOutput").ap()
    S = Sched(nc)
    with contextlib.ExitStack() as st:
        outs = emit_pw(S, nc, st, a, b, ao, bo)
        S.wait_all("sp", outs)
        S.emit(st)
    return nc


def _build_p3(layer, last):
    nc = bass.Bass("TRN2", target_bir_lowering=False)
    dd = {k: nc.dram_tensor(k, v, BF16 if k in ("wgu", "wdn") else F32, kind="ExternalInput").ap()
          for k, v in P3_IN_SHAPES.items()}
    dd["x1T"] = nc.dram_tensor("x1T", [4096, NT], F32, kind="Internal").ap()
    dd["h2d"] = nc.dram_tensor("h2d", [128, 32, NT], BF16, kind="Internal").ap()
    dd["x2T"] = nc.dram_tensor("x2T", [4096, NT], F32, kind="ExternalOutput").ap()
    if last:
        dd["outT"] = nc.dram_tensor("outT", [32, 128, 1024], F32, kind="ExternalOutput").ap()
    S = Sched(nc)
    with contextlib.ExitStack() as st:
        ps = alloc_psum(nc, st)
        scr = st.enter_context(nc.sbuf_tensor("p3_scr", [128, 1], F32))
        emit_p3(S, nc, ps, scr[:], layer, last, dd)
        S.emit(st)
    return nc


def kernel(x, c, ctx, c_ctx, w_ada, b_ada, norm1_g, norm2_g, w_in, lam_qk, subln_g, conv_w, conv_b,
           lru_gate_w, lru_gate_b, lru_lambda, na_rpb, w_branch, w_out, w_router, b_router,
           w_gu, b_gu, w_down, b_down, final_g):
    import math
    A = lambda v: np.asarray(v, dtype=np.float32)
    x = A(x); ctx = A(ctx)
    inp = dict(lam_qk=A(lam_qk), subln_g=A(subln_g), conv_w=A(conv_w), conv_b=A(conv_b), lru_gate_w=A(lru_gate_w),
               lru_gate_b=A(lru_gate_b), lru_lambda=A(lru_lambda), na_rpb=A(na_rpb), norm2_g=A(norm2_g),
               w_branch=A(w_branch), w_out=A(w_out), w_router=A(w_router), b_router=A(b_router), w_gu=A(w_gu),
               b_gu=A(b_gu), w_down=A(w_down), b_down=A(b_down), final_g=A(final_g))
    w_ada = A(w_ada); b_ada = A(b_ada); w_in = A(w_in); norm1_g = A(norm1_g)
    cT = np.stack([fm(A(c)[0]), fm(A(c_ctx))], axis=-1)
    ims = []
    for j in range(8):
        l, q = j // 4, j % 4
        ims.append({"cT": cT, "wada": np.ascontiguousarray(w_ada[l][:, q * 6144:(q + 1) * 6144]),
                    "bada": np.ascontiguousarray(np.broadcast_to(b_ada[l][q * 6144:(q + 1) * 6144][None], (2, 6144)))})
    res = _run(_build_p0(), ims)
    modv = np.ascontiguousarray(np.concatenate([r["modv"].reshape(2, 48, 128).transpose(2, 1, 0) for r in res], axis=1))
    del ims
    xTs = []
    for i in range(8):
        xc = np.concatenate([ctx[0, 32 * i:32 * i + 32], x[0, 1024 * i:1024 * i + 1024]], 0)
        xTs.append(np.ascontiguousarray(xc.T))
    ropeC, ropeS = rope_tables()
    csts = [p3_consts(inp, l) for l in range(2)]
    ims = [{"wgu32": np.ascontiguousarray(np.concatenate([csts[0]["wgu"][4 * j:4 * j + 4], csts[1]["wgu"][4 * j:4 * j + 4]])),
            "wdn32": np.ascontiguousarray(np.concatenate([csts[0]["wdn"][4 * j:4 * j + 4], csts[1]["wdn"][4 * j:4 * j + 4]]))}
           for j in range(8)]
    res = _run(_build_pw(), ims)
    for l in range(2):
        csts[l]["wgu"] = tile_major_gu(np.concatenate([res[j]["wgub"][4 * l:4 * l + 4] for j in range(8)]))
        csts[l]["wdn"] = tile_major_dn(np.concatenate([res[j]["wdnb"][4 * l:4 * l + 4] for j in range(8)]))
    del ims, res
    outT = None
    for l in range(2):
        last = (l == 1)
        n1g = fm(norm1_g[l])
        ims = [{"xT": xTs[i], "modv": modv, "n1g": n1g, "win": w_in[l]} for i in range(8)]
        res = _run(_build_p1(l), ims)
        zs = [r["zsend"] for r in res]
        gs = [r["gsig"] for r in res]
        del ims, res
        lambda_init = 0.8 - 0.6 * math.exp(-0.3 * l)
        ims = []
        for j in range(8):
            im = {"zr": np.ascontiguousarray(np.stack([zs[s][j] for s in range(8)]))}
            im.update(p2_consts(inp, l, j, ropeC, ropeS))
            ims.append(im)
        del zs
        res = _run(_build_p2(lambda_init), ims)
        osr = [r["os"] for r in res]
        del ims, res
        cst = csts[l]
        ims = []
        for i in range(8):
            im = {"orecv": np.ascontiguousarray(np.stack([osr[j][i] for j in range(8)])), "gsig": gs[i],
                  "xT": xTs[i], "modv": modv}
            im.update(cst)
            ims.append(im)
        del osr, gs
        res = _run(_build_p3(l, last), ims)
        xTs = [r["x2T"] for r in res]
        if last:
            outT = [r["outT"] for r in res]
        del ims, res, cst
        csts[l] = None
    out = np.empty((1, 8192, 4096), np.float32)
    for i in range(8):
        out[0, 1024 * i:1024 * (i + 1), :] = outT[i].reshape(4096, 1024).T
    return out
```
